# Optimizing a Trainium2 kernel written in Bass

```python
import jax
import jax.numpy as jnp
from jax import lax
import numpy as np

D_MODEL = 1024
BATCH = 16
SEQ = 4096
DEPTH = 4

GRID_W = 64
CTX_LEN = 256
EPS = 1e-6
W_A = D_MODEL
H_A = 8
DH_A = W_A // H_A
CONV_W = 4
LRU_C = 8.0
W_B = D_MODEL // 2
POOL_WINDOWS = (2, 4, 8, 16)
G_B = len(POOL_WINDOWS)
DG_B = W_B // G_B
W_C = D_MODEL // 2
CHUNK = 128
G_C = 4
DG_C = W_C // G_C
N_BRANCH = 3
OFF_GA = W_A
OFF_B = 2 * W_A
OFF_U = OFF_B + W_B
OFF_V = OFF_U + W_C
OFF_G = OFF_V + W_C
IN_COLS = OFF_G + N_BRANCH * D_MODEL
N_EXPERTS = 32
TOP_K = 4
D_FF = D_MODEL
SWIGLU_LIMIT = 7.0
SWIGLU_ALPHA = 1.702
MOE_BLOCK = 512

kernel_name = 'hybrid_lru_pool_sgu_moe_prefix_trunk'


def _rmsnorm(x, g):
    xf = x.astype(jnp.float32)
    y = xf * lax.rsqrt(jnp.mean(xf * xf, axis=-1, keepdims=True) + EPS)
    return (y * g.astype(jnp.float32)).astype(x.dtype)


def _layernorm(x, g, b):
    xf = x.astype(jnp.float32)
    mu = jnp.mean(xf, axis=-1, keepdims=True)
    var = jnp.mean(jnp.square(xf - mu), axis=-1, keepdims=True)
    return ((xf - mu) * lax.rsqrt(var + EPS)).astype(x.dtype) * g + b


def _dwconv(x, w, b):
    y = lax.conv_general_dilated(
        x, w[:, None, :], window_strides=(1,),
        padding=[(CONV_W // 2, CONV_W - 1 - CONV_W // 2)],
        dimension_numbers=('NWC', 'WIO', 'NWC'),
        feature_group_count=x.shape[-1])
    return y + b


def _rglru_scan(xc, wa, ba, wx, bx, lam, h0, reverse, return_seq):
    xf = xc.astype(jnp.float32)
    bsz, t = xf.shape[0], xf.shape[1]
    xh = xf.reshape(bsz, t, H_A, DH_A)
    r = jax.nn.sigmoid(jnp.einsum('bthi,hij->bthj', xh, wa.astype(jnp.float32)).reshape(bsz, t, W_A) + ba)
    gi = jax.nn.sigmoid(jnp.einsum('bthi,hij->bthj', xh, wx.astype(jnp.float32)).reshape(bsz, t, W_A) + bx)
    log_a = -LRU_C * r * jax.nn.softplus(-lam.astype(jnp.float32))
    a = jnp.exp(log_a)
    inp = jnp.sqrt(-jnp.expm1(2.0 * log_a)) * gi * xf
    a_t = jnp.swapaxes(a, 0, 1)
    b_t = jnp.swapaxes(inp, 0, 1)
    if return_seq:
        def step(h, ab):
            h = ab[0] * h + ab[1]
            return h, h
        h_last, hs = lax.scan(step, h0, (a_t, b_t), reverse=reverse)
        return jnp.swapaxes(hs, 0, 1), h_last

    def step_state(h, ab):
        return ab[0] * h + ab[1], None
    h_last, _ = lax.scan(step_state, h0, (a_t, b_t), reverse=reverse)
    return None, h_last


def _lru_branch(xa_lat, xa_ctx, lp, ctx_seq):
    xcl = _dwconv(xa_lat, lp['conv_w'], lp['conv_b'])
    xcc = _dwconv(xa_ctx, lp['conv_w'], lp['conv_b'])
    h0 = jnp.zeros((xa_ctx.shape[0], W_A), jnp.float32)
    out_lat = None
    out_ctx = None
    for d in range(2):
        rev = d == 1
        args = (lp['lru_wa'][d], lp['lru_ba'][d], lp['lru_wx'][d], lp['lru_bx'][d], lp['lru_lambda'][d])
        hs_c, h_ctx_last = _rglru_scan(xcc, *args, h0, rev, ctx_seq)
        hs_l, _ = _rglru_scan(xcl, *args, h_ctx_last, rev, True)
        out_lat = hs_l if out_lat is None else out_lat + hs_l
        if ctx_seq:
            out_ctx = hs_c if out_ctx is None else out_ctx + hs_c
    out_lat = out_lat.astype(xa_lat.dtype)
    if ctx_seq:
        out_ctx = out_ctx.astype(xa_ctx.dtype)
    return out_lat, out_ctx


def _window_mean(x, k, axis):
    t = x.shape[axis]
    cs = jnp.cumsum(x.astype(jnp.float32), axis=axis)
    pad = [(0, 0)] * x.ndim
    pad[axis] = (1, 0)
    cs = jnp.pad(cs, pad)
    pos = jnp.arange(t)
    lo = jnp.clip(pos - k // 2, 0, t)
    hi = jnp.clip(pos + (k - k // 2), 0, t)
    s = jnp.take(cs, hi, axis=axis) - jnp.take(cs, lo, axis=axis)
    shape = [1] * x.ndim
    shape[axis] = t
    cnt = (hi - lo).astype(jnp.float32).reshape(shape)
    return (s / cnt).astype(x.dtype)


def _pool_mix(xb, rows, w, b, scale):
    bsz, t = xb.shape[0], xb.shape[1]
    outs = []
    for g, k in enumerate(POOL_WINDOWS):
        seg = xb[..., g * DG_B:(g + 1) * DG_B]
        if rows is None:
            pooled = _window_mean(seg, k, 1)
        else:
            sg = seg.reshape(bsz, rows, GRID_W, DG_B)
            pooled = _window_mean(_window_mean(sg, k, 2), k, 1).reshape(bsz, t, DG_B)
        outs.append(pooled - seg)
    p = jnp.stack(outs, axis=2)
    y = jnp.einsum('btgi,gij->btgj', p, w).reshape(bsz, t, W_B) + b
    return y * scale


def _spatial_gate(u, v, ln_g, ln_b, sg_w, sg_b):
    bsz, t = v.shape[0], v.shape[1]
    vn = _layernorm(v, ln_g, ln_b)
    vc = vn.reshape(bsz, t // CHUNK, CHUNK, G_C, DG_C)
    s = jnp.einsum('gpq,bnqgd->bnpgd', sg_w, vc) + jnp.swapaxes(sg_b, 0, 1)[:, :, None]
    return u * s.reshape(bsz, t, W_C)


def _branches_out(z, lru_out, rows, lp):
    y_a = (jax.nn.gelu(z[..., OFF_GA:OFF_B]) * lru_out) @ lp['out_a']
    y_b = _pool_mix(z[..., OFF_B:OFF_U], rows, lp['pool_w'], lp['pool_b'], lp['pool_scale']) @ lp['out_b']
    uv = jax.nn.gelu(z[..., OFF_U:OFF_G])
    y_c = _spatial_gate(uv[..., :W_C], uv[..., W_C:], lp['sg_ln_g'], lp['sg_ln_b'], lp['sg_w'], lp['sg_b']) @ lp['out_c']
    gates = jax.nn.sigmoid(z[..., OFF_G:])
    merged = (gates[..., :D_MODEL] * y_a + gates[..., D_MODEL:2 * D_MODEL] * y_b
              + gates[..., 2 * D_MODEL:] * y_c)
    return merged @ lp['w_o'] + lp['b_o']


def _mixer(h, hc, rows, lp, ctx_out):
    z = h @ lp['w_in'] + lp['b_in']
    if ctx_out:
        zc = hc @ lp['w_in'] + lp['b_in']
        xa_c = zc[..., :W_A]
    else:
        xa_c = hc @ lp['w_in'][:, :W_A] + lp['b_in'][:W_A]
    lru_lat, lru_ctx = _lru_branch(z[..., :W_A], xa_c, lp, ctx_out)
    y = _branches_out(z, lru_lat, rows, lp)
    yc = _branches_out(zc, lru_ctx, None, lp) if ctx_out else None
    return y, yc


def _moe_ffn(h, router_w, router_b, w_gate, b_gate, w_up, b_up, w_down, b_down):
    shp = h.shape
    x2 = h.reshape(-1, D_MODEL)
    n_tok = x2.shape[0]
    logits = (x2 @ router_w + router_b).astype(jnp.float32)
    top_vals, top_idx = lax.top_k(logits, TOP_K)
    probs = jax.nn.softmax(top_vals, axis=-1)
    n_assign = n_tok * TOP_K
    flat_e = top_idx.reshape(-1)
    order = jnp.argsort(flat_e)
    sorted_e = flat_e[order]
    sorted_tok = order // TOP_K
    counts = jnp.bincount(flat_e, length=N_EXPERTS)
    padded = (counts + MOE_BLOCK - 1) // MOE_BLOCK * MOE_BLOCK
    pad_end = jnp.cumsum(padded)
    pad_start = pad_end - padded
    start = jnp.cumsum(counts) - counts
    dest = pad_start[sorted_e] + jnp.arange(n_assign) - start[sorted_e]
    n_blocks = (n_assign + N_EXPERTS * (MOE_BLOCK - 1) + MOE_BLOCK - 1) // MOE_BLOCK
    n_rows = n_blocks * MOE_BLOCK
    row_tok = jnp.full((n_rows,), n_tok, jnp.int32).at[dest].set(sorted_tok.astype(jnp.int32))
    row_w = jnp.zeros((n_rows,), x2.dtype).at[dest].set(probs.reshape(-1)[order].astype(x2.dtype))
    block_e = jnp.minimum(jnp.searchsorted(pad_end, jnp.arange(n_blocks) * MOE_BLOCK, side='right'), N_EXPERTS - 1)
    x_ext = jnp.concatenate([x2, jnp.zeros((1, D_MODEL), x2.dtype)], axis=0)

    def expert_block(args):
        rows_i, e, rw = args
        xb = x_ext[rows_i]
        gate = jnp.minimum(xb @ w_gate[e] + b_gate[e], SWIGLU_LIMIT)
        up = jnp.clip(xb @ w_up[e] + b_up[e], -SWIGLU_LIMIT, SWIGLU_LIMIT)
        glu = gate * jax.nn.sigmoid(gate * SWIGLU_ALPHA)
        out = ((up + 1.0) * glu) @ w_down[e] + b_down[e]
        return out * rw[:, None]

    yp = lax.map(expert_block, (row_tok.reshape(n_blocks, MOE_BLOCK), block_e,
                                row_w.reshape(n_blocks, MOE_BLOCK))).reshape(n_rows, D_MODEL)
    y = jax.ops.segment_sum(yp, row_tok, num_segments=n_tok + 1)[:n_tok]
    return y.reshape(shp)


def setup_inputs(seed: int = 0) -> dict:
    key = jax.random.key(seed)
    ks = iter(jax.random.split(key, 64))
    f32 = jnp.float32
    L = DEPTH

    def nrm(shape, scale):
        return jax.random.normal(next(ks), shape, f32) * scale

    u = jax.random.uniform(next(ks), (L, 2, W_A), f32, 0.9, 0.999)
    base = u ** (1.0 / LRU_C)
    lru_lambda = jnp.log(base) - jnp.log1p(-base)
    return {
        'x': nrm((BATCH, SEQ, D_MODEL), 1.0),
        'c': nrm((BATCH, D_MODEL), 1.0),
        'ctx': nrm((BATCH, CTX_LEN, D_MODEL), 1.0),
        'c_ctx': nrm((D_MODEL,), 1.0),
        'ada_w': nrm((L, D_MODEL, 6 * D_MODEL), 0.5 * D_MODEL ** -0.5),
        'ada_b': nrm((L, 6 * D_MODEL), 0.02),
        'norm1_g': 1.0 + nrm((L, D_MODEL), 0.05),
        'norm2_g': 1.0 + nrm((L, D_MODEL), 0.05),
        'w_in': nrm((L, D_MODEL, IN_COLS), D_MODEL ** -0.5),
        'b_in': nrm((L, IN_COLS), 0.02),
        'conv_w': nrm((L, CONV_W, W_A), CONV_W ** -0.5),
        'conv_b': nrm((L, W_A), 0.02),
        'lru_wa': nrm((L, 2, H_A, DH_A, DH_A), DH_A ** -0.5),
        'lru_ba': nrm((L, 2, W_A), 0.02),
        'lru_wx': nrm((L, 2, H_A, DH_A, DH_A), DH_A ** -0.5),
        'lru_bx': nrm((L, 2, W_A), 0.02),
        'lru_lambda': lru_lambda,
        'out_a': nrm((L, W_A, D_MODEL), W_A ** -0.5),
        'pool_w': nrm((L, G_B, DG_B, DG_B), DG_B ** -0.5),
        'pool_b': nrm((L, W_B), 0.02),
        'pool_scale': 1.0 + nrm((L, W_B), 0.1),
        'out_b': nrm((L, W_B, D_MODEL), W_B ** -0.5),
        'sg_ln_g': 1.0 + nrm((L, W_C), 0.05),
        'sg_ln_b': nrm((L, W_C), 0.02),
        'sg_w': nrm((L, G_C, CHUNK, CHUNK), CHUNK ** -0.5),
        'sg_b': 1.0 + nrm((L, G_C, CHUNK), 0.02),
        'out_c': nrm((L, W_C, D_MODEL), W_C ** -0.5),
        'w_o': nrm((L, D_MODEL, D_MODEL), D_MODEL ** -0.5),
        'b_o': nrm((L, D_MODEL), 0.02),
        'router_w': nrm((L, D_MODEL, N_EXPERTS), D_MODEL ** -0.5),
        'router_b': nrm((L, N_EXPERTS), 0.01),
        'w_gate': nrm((L, N_EXPERTS, D_MODEL, D_FF), D_MODEL ** -0.5),
        'b_gate': nrm((L, N_EXPERTS, D_FF), 0.02),
        'w_up': nrm((L, N_EXPERTS, D_MODEL, D_FF), D_MODEL ** -0.5),
        'b_up': nrm((L, N_EXPERTS, D_FF), 0.02),
        'w_down': nrm((L, N_EXPERTS, D_FF, D_MODEL), D_FF ** -0.5),
        'b_down': nrm((L, N_EXPERTS, D_MODEL), 0.02),
        'final_g': 1.0 + nrm((D_MODEL,), 0.05),
    }


def reference(x, c, ctx, c_ctx, ada_w, ada_b, norm1_g, norm2_g, w_in, b_in, conv_w, conv_b,
              lru_wa, lru_ba, lru_wx, lru_bx, lru_lambda, out_a, pool_w, pool_b, pool_scale, out_b,
              sg_ln_g, sg_ln_b, sg_w, sg_b, out_c, w_o, b_o, router_w, router_b,
              w_gate, b_gate, w_up, b_up, w_down, b_down, final_g):
    rows = x.shape[1] // GRID_W
    n_ctx = ctx.shape[1]
    silu_c = jax.nn.silu(c)
    silu_cc = jax.nn.silu(c_ctx)
    for l in range(DEPTH):
        last = l == DEPTH - 1
        lp = {
            'w_in': w_in[l], 'b_in': b_in[l], 'conv_w': conv_w[l], 'conv_b': conv_b[l],
            'lru_wa': lru_wa[l], 'lru_ba': lru_ba[l], 'lru_wx': lru_wx[l], 'lru_bx': lru_bx[l],
            'lru_lambda': lru_lambda[l], 'out_a': out_a[l],
            'pool_w': pool_w[l], 'pool_b': pool_b[l], 'pool_scale': pool_scale[l], 'out_b': out_b[l],
            'sg_ln_g': sg_ln_g[l], 'sg_ln_b': sg_ln_b[l], 'sg_w': sg_w[l], 'sg_b': sg_b[l], 'out_c': out_c[l],
            'w_o': w_o[l], 'b_o': b_o[l],
        }
        mod = (silu_c @ ada_w[l] + ada_b[l])[:, None, :]
        sh1, sc1, g1, sh2, sc2, g2 = jnp.split(mod, 6, axis=-1)
        modc = silu_cc @ ada_w[l] + ada_b[l]
        sh1c, sc1c, g1c, sh2c, sc2c, g2c = jnp.split(modc, 6, axis=-1)

        h = _rmsnorm(x, norm1_g[l]) * (1.0 + sc1) + sh1
        hc = _rmsnorm(ctx, norm1_g[l]) * (1.0 + sc1c) + sh1c
        y, yc = _mixer(h, hc, rows, lp, not last)
        x = x + g1 * y
        h2 = _rmsnorm(x, norm2_g[l]) * (1.0 + sc2) + sh2
        moe_args = (router_w[l], router_b[l], w_gate[l], b_gate[l], w_up[l], b_up[l], w_down[l], b_down[l])
        if last:
            f = _moe_ffn(h2, *moe_args)
        else:
            ctx = ctx + g1c * yc
            h2c = _rmsnorm(ctx, norm2_g[l]) * (1.0 + sc2c) + sh2c
            both = _moe_ffn(jnp.concatenate([h2c, h2], axis=1), *moe_args)
            ctx = ctx + g2c * both[:, :n_ctx]
            f = both[:, n_ctx:]
        x = x + g2 * f
    return _rmsnorm(x, final_g)
```

```python
import contextlib
import numpy as np
import ml_dtypes
import concourse.bass as bass
import concourse.mybir as mybir
from concourse.bass_utils import run_bass_kernel_spmd

F32 = mybir.dt.float32
BF16 = mybir.dt.bfloat16
I32 = mybir.dt.int32
AF = mybir.ActivationFunctionType
ALU = mybir.AluOpType
AX = mybir.AxisListType

D = 1024
NE = 32
EPS = 1e-6
POOL_K = (2, 4, 8, 16)
GRID_W = 64
MOE_BLOCK = 512


class TT:
    __slots__ = ("name", "w", "r", "rd")

    def __init__(self, name=""):
        self.name = name
        self.w = None
        self.r = {}
        self.rd = []


class Prog:
    ENGS = ("pe", "act", "dve", "pool", "sp")
    NDMASEM = 24

    profile = False

    def __init__(self, nc):
        self.nc = nc
        self.ops = []

    def add(self, eng, fn, reads=(), writes=(), dma=False):
        i = len(self.ops)
        deps = {}

        def dep(j, kind):
            if j is None:
                return
            if deps.get(j) != "raw":
                deps[j] = kind

        for t in reads:
            dep(t.w, "raw")
        for t in writes:
            dep(t.w, "waw")
            for j in t.r.values():
                dep(j, "war")
            for j in t.rd:
                dep(j, "war")
        for t in reads:
            if dma:
                t.rd.append(i)
            else:
                t.r[eng] = i
        for t in writes:
            t.w = i
            t.r = {}
            t.rd = []
        self.ops.append(dict(eng=eng, fn=fn, deps=deps, dma=dma, sig=False, ph=getattr(self, "phase", None)))
        return i

    def barrier(self):
        bt = [TT("bar_" + e) for e in self.ENGS]
        pend = TT("bar_dma")
        pend.rd = [i for i, op in enumerate(self.ops) if op["dma"] and i >= getattr(self, "_bar_from", 0)]
        self.add("sp", lambda e: e.nop(), writes=[pend, bt[4]])
        for k, e in enumerate(self.ENGS[:4]):
            self.add(e, self.bar_fn[e], writes=[bt[k]] + self.bar_tt.get(e, []))
        for k, e in enumerate(self.ENGS):
            self.add(e, (lambda ee: ee.nop()), reads=bt)
        self._bar_from = len(self.ops)

    def emit(self):
        nc = self.nc
        ops = self.ops
        for i, op in enumerate(ops):
            need = []
            for j, kind in op["deps"].items():
                pj = ops[j]
                if pj["dma"]:
                    need.append(j)
                    continue
                if pj["eng"] == op["eng"] and not op["dma"]:
                    if op["eng"] == "pe":
                        continue
                    if kind != "raw":
                        continue
                need.append(j)
                pj["sig"] = True
            op["need"] = need
        st = contextlib.ExitStack()
        esem = {e: st.enter_context(nc.semaphore("S_" + e)) for e in self.ENGS}
        dsem = {e: [st.enter_context(nc.semaphore("D_%s%d" % (e, k))) for k in range(self.NDMASEM)]
                for e in ("sp", "act", "pool")}
        ecount = {e: 0 for e in self.ENGS}
        dcount = {e: [0] * self.NDMASEM for e in dsem}
        dnext = {e: 0 for e in dsem}
        for op in ops:
            e = op["eng"]
            if op["dma"]:
                k = dnext[e] % self.NDMASEM
                dnext[e] += 1
                op["prev"] = (dsem[e][k], dcount[e][k])
                dcount[e][k] += 16
                op["done"] = (dsem[e][k], dcount[e][k])
            elif op["sig"]:
                ecount[e] += 1
                op["done"] = (esem[e], ecount[e])
        self.stats = dict(n_ops=len(ops), sig=dict(ecount), ndma=dict(dnext))
        block = st.enter_context(nc.Block())

        def run(ename):
            def body(eng):
                waited = {}
                for op in ops:
                    if op["eng"] != ename:
                        continue
                    ws = []
                    if op["dma"] and op["prev"][1] > 0:
                        ws.append(op["prev"])
                    for j in op["need"]:
                        ws.append(ops[j]["done"])
                    best = {}
                    for s, c in ws:
                        if c > best.get(s.name, (None, 0))[1]:
                            best[s.name] = (s, c)
                    for s, c in best.values():
                        if waited.get(s.name, 0) >= c:
                            continue
                        eng.wait_ge(s, c)
                        waited[s.name] = c
                    if self.profile and op["ph"]:
                        with nc.named_scope(op["ph"]):
                            ins = op["fn"](eng)
                    else:
                        ins = op["fn"](eng)
                    if op["dma"]:
                        ins.then_inc(op["done"][0], 16)
                    elif op["sig"]:
                        ins.then_inc(op["done"][0], 1)
            return body

        block.tensor(run("pe"))
        block.scalar(run("act"))
        block.vector(run("dve"))
        block.gpsimd(run("pool"))
        block.sync(run("sp"))
        st.close()


class Ring:
    def __init__(self, items):
        self.items = items
        self.i = 0

    def next(self):
        it = self.items[self.i % len(self.items)]
        self.i += 1
        return it


class Cfg:
    def __init__(self, NS=2, TC=256, TL=4096, L=4, debug=False, stop=None):
        self.NS, self.TC, self.TL, self.L, self.debug, self.stop = NS, TC, TL, L, debug, stop
        self.TS = TC + TL
        self.NT = NS * self.TS
        assert self.NT % 512 == 0 and TC % 128 == 0 and TL % 128 == 0
        self.NTILE = self.NT // 128
        self.NST = self.NT // 512
        self.R = TL // GRID_W
        self.SEG = 1024 if TL % 1024 == 0 else TL
        self.NBLK = (self.NT * 4 + NE * (MOE_BLOCK - 1) + MOE_BLOCK - 1) // MOE_BLOCK
        self.NROWS = self.NBLK * MOE_BLOCK

    def tile_r(self, ti):
        s = (ti * 128) // self.TS
        off = ti * 128 - s * self.TS
        return 2 if off < self.TC else s


OFF_GA, OFF_B, OFF_U, OFF_V, OFF_G, IN_COLS = 1024, 2048, 2560, 3072, 3584, 6656


def build(cfg):
    nc = bass.Bass("TRN2", target_bir_lowering=False)
    P = Prog(nc)
    NS, TC, TL, L, TS, NT, NTILE, NST = cfg.NS, cfg.TC, cfg.TL, cfg.L, cfg.TS, cfg.NT, cfg.NTILE, cfg.NST
    NBLK, NROWS, R, SEG = cfg.NBLK, cfg.NROWS, cfg.R, cfg.SEG

    def din(name, shape, dt=F32):
        return nc.dram_tensor(name, list(shape), dt, kind="ExternalInput").ap()

    def dscr(name, shape, dt):
        kind = "ExternalOutput" if cfg.debug else "Internal"
        return nc.dram_tensor(name, list(shape), dt, kind=kind).ap()

    xin = din("xin", [NT, D])
    cT = din("cT", [128, 8 * 3])
    ada_w = din("ada_w", [L, D, 6 * D])
    ada_b = din("ada_b", [L, 6 * D])
    n1gT = din("n1gT", [L, 128, 8])
    n2g = din("n2g", [L, D])
    final_g = din("final_g", [1, D])
    w_in = din("w_in", [L, D, IN_COLS])
    b_inT = din("b_inT", [L, 128, 52])
    b_in = din("b_in", [L, IN_COLS])
    convT = din("convT", [L, 128, 8 * 5])
    lruT = din("lruT", [L, 128, 2 * 8 * 3])
    lru_wa = din("lru_wa", [L, 2, 8, 128, 128])
    lru_wx = din("lru_wx", [L, 2, 8, 128, 128])
    out_a = din("out_a", [L, D, D])
    out_b = din("out_b", [L, 512, D])
    out_c = din("out_c", [L, 512, D])
    w_o = din("w_o", [L, D, D])
    b_o = din("b_o", [L, D])
    pool_w = din("pool_w", [L, 4, 128, 128])
    poolT = din("poolT", [L, 128, 8])
    sg_ln = din("sg_ln", [L, 2, 512])
    sg_wT = din("sg_wT", [L, 4, 128, 128])
    sg_b = din("sg_b", [L, 512])
    router_w = din("router_w", [L, D, NE])
    router_b = din("router_b", [L, NE])
    w_gate = din("w_gate", [L, NE, D, D])
    w_up = din("w_up", [L, NE, D, D])
    w_down = din("w_down", [L, NE, D, D])
    b_gateT = din("b_gateT", [L * NE * 128, 8])
    b_upT = din("b_upT", [L * NE * 128, 8])
    b_down = din("b_down", [L * NE, D])
    c_ident = din("c_ident", [128, 128], BF16)
    c_ltri = din("c_ltri", [128, 128], BF16)
    c_ones = din("c_ones", [128, 128], BF16)
    c_pinv = din("c_pinv", [4, 2 * 64 + TC])
    c_pidx = din("c_pidx", [128, 1])
    c_pl = din("c_pl", [1, 8])
    c_blk = din("c_blk", [1, NBLK])
    c_tok = din("c_tok", [128, NTILE * 2], I32)
    c_meta0 = din("c_meta0", [128, 2 * (NROWS // 128)], I32)
    out = nc.dram_tensor("out", [NS * TL, D], F32, kind="ExternalOutput").ap()

    xres = dscr("xres", [NT, D], F32)
    mod_d = dscr("mod_d", [3, 6 * D], F32)
    xaT_d = dscr("xaT_d", [D, NT], F32)
    gaT_d = dscr("gaT_d", [D, NT], BF16)
    zbT_d = dscr("zbT_d", [512, NT], F32)
    ucT_d = dscr("ucT_d", [512, NT], BF16)
    gT_d = dscr("gT_d", [3 * D, NT], BF16)
    uaT_d = dscr("uaT_d", [D, NT], BF16)
    pmT_d = dscr("pmT_d", [512, NT], BF16)
    h2_d = dscr("h2_d", [NT + 128, D], BF16)
    meta_d = dscr("meta_d", [NROWS, 2], I32)
    yp_d = dscr("yp_d", [NROWS, D], F32)
    T_xres_all, T_mod, T_xaT, T_gaT, T_zbT, T_ucT, T_gT, T_uaT, T_pmT, T_h2, T_meta, T_yp, T_out = [
        TT(n) for n in "xres mod xaT gaT zbT ucT gT uaT pmT h2 meta yp out".split()]

    T_xres = [TT("xres%d" % i) for i in range(NTILE)]
    top = contextlib.ExitStack()

    def mk_sb(stack):
        cnt = [0]

        def sb(shape, dt, name=None):
            cnt[0] += 1
            return stack.enter_context(nc.sbuf_tensor("%s_%d_%d" % (name or "t", id(stack) % 10007, cnt[0]), list(shape), dt))
        return sb

    sbp = mk_sb(top)
    psf = [top.enter_context(nc.psum_tensor("psf%d" % i, [128, 512], F32)) for i in range(6)]
    psb = [top.enter_context(nc.psum_tensor("psb%d" % i, [128, 1024], BF16)) for i in range(2)]
    PSF = Ring([(psf[i], TT("psf%d" % i)) for i in range(6)])
    PSB = Ring([(psb[i], TT("psb%d" % i)) for i in range(2)])

    bscr = sbp([128, 8], F32, "bscr")
    ident = sbp([128, 128], BF16, "ident")
    ltri = sbp([128, 128], BF16, "ltri")
    ones = sbp([128, 128], BF16, "ones")
    T_const = TT("const")
    P.bar_fn = {
        "pe": lambda e: e.matmul(psf[0][0:1, 0:1], lhsT=ident[:, 0:1], rhs=ident[:, 0:1], start=True, stop=True),
        "act": lambda e: e.activation(out=bscr[0:1, 0:1], in_=bscr[0:1, 1:2], func=AF.Copy),
        "dve": lambda e: e.memset(bscr[0:1, 2:3], 0.0),
        "pool": lambda e: e.memset(bscr[0:1, 4:5], 0.0),
    }
    P.bar_tt = {"pe": [PSF.items[0][1]]}
    P.add("dve", lambda e: e.memset(bscr[:], 0.0), writes=[TT("bscr")])
    siluT = sbp([128, 8, 3], F32, "siluT")
    T_silu = TT("silu")
    for tl, src in ((ident, c_ident), (ltri, c_ltri), (ones, c_ones)):
        P.add("sp", lambda e, tl=tl, src=src: e.dma_start(out=tl[:], in_=src), writes=[T_const], dma=True)
    P.add("sp", lambda e: e.dma_start(out=siluT[:].rearrange("p a b -> p (a b)"), in_=cT), writes=[T_silu], dma=True)
    P.add("act", lambda e: e.activation(out=siluT[:], in_=siluT[:], func=AF.Silu), reads=[T_silu], writes=[T_silu])
    for i in range(NST):
        P.add("sp", lambda e, i=i: e.dma_start(out=xres[i * 512:(i + 1) * 512, :], in_=xin[i * 512:(i + 1) * 512, :]),
              writes=T_xres[4 * i:4 * i + 4], dma=True)

    def sl(i, n=128):
        return slice(i * n, (i + 1) * n)

    def phase_mod(l):
        with contextlib.ExitStack() as st:
            sb = mk_sb(st)
            wr = Ring([(sb([128, 8, 512], F32, "adaw"), TT("adaw%d" % i)) for i in range(2)])
            modrow = sb([3, 6 * D], F32, "modrow")
            adab = sb([3, 6 * D], F32, "adab")
            T_modrow, T_adab = TT("modrow"), TT("adab")
            P.add("sp", lambda e: e.dma_start(out=adab[:], in_=ada_b[l:l + 1, :].to_broadcast([3, 6 * D])),
                  writes=[T_adab], dma=True)
            for cb in range(12):
                wt, Tw = wr.next()
                P.add("sp", lambda e, wt=wt, cb=cb: e.dma_start(
                    out=wt[:], in_=ada_w[l].rearrange("(kc p) n -> p kc n", p=128)[:, :, sl(cb, 512)]), writes=[Tw], dma=True)
                ps, Tp = PSF.next()
                for kc in range(8):
                    P.add("pe", lambda e, ps=ps, wt=wt, kc=kc: e.matmul(ps[0:3, :], lhsT=siluT[:, kc, :], rhs=wt[:, kc, :],
                                                                      start=(kc == 0), stop=(kc == 7)),
                          reads=[T_silu, Tw], writes=[Tp])
                P.add("dve", lambda e, ps=ps, cb=cb: e.tensor_tensor(out=modrow[:, sl(cb, 512)], in0=ps[0:3, :], in1=adab[:, sl(cb, 512)], op=ALU.add),
                      reads=[Tp, T_adab], writes=[T_modrow])
            P.add("sp", lambda e: e.dma_start(out=mod_d, in_=modrow[:]), reads=[T_modrow], writes=[T_mod], dma=True)
        P.barrier()

    def phase_A(l):
        with contextlib.ExitStack() as st:
            sb = mk_sb(st)
            win = sb([128, 8, IN_COLS], BF16, "win")
            T_win = [TT("win%d" % i) for i in range(13)]
            for cb in range(13):
                P.add("pool", lambda e, cb=cb: e.dma_start(out=win[:, :, sl(cb, 512)],
                                                            in_=w_in[l].rearrange("(kc p) n -> p kc n", p=128)[:, :, sl(cb, 512)]),
                      writes=[T_win[cb]], dma=True)

            def Twin(c0, c1):
                return T_win[c0 // 512:(c1 - 1) // 512 + 1]
            binT = sb([128, 52], F32, "binT")
            bv_bc = sb([128, 512], F32, "bv")
            lng = sb([128, 512], F32, "lng")
            lnb = sb([128, 512], F32, "lnb")
            sgb = sb([128, 512], F32, "sgb")
            sgw = sb([128, 4, 128], BF16, "sgw")
            n1g = sb([128, 8], F32, "n1g")
            modT = sb([128, 16, 3], F32, "modT")
            gm1 = sb([128, 8, 3], F32, "gm1")
            T_par, T_modT, T_gm1 = TT("parA"), TT("modT"), TT("gm1")
            P.add("sp", lambda e: e.dma_start(out=binT[:], in_=b_inT[l]), writes=[T_par], dma=True)
            P.add("sp", lambda e: e.dma_start(out=bv_bc[:], in_=b_in[l:l + 1, OFF_V:OFF_G].to_broadcast([128, 512])), writes=[T_par], dma=True)
            P.add("sp", lambda e: e.dma_start(out=lng[:], in_=sg_ln[l, 0:1, :].to_broadcast([128, 512])), writes=[T_par], dma=True)
            P.add("sp", lambda e: e.dma_start(out=lnb[:], in_=sg_ln[l, 1:2, :].to_broadcast([128, 512])), writes=[T_par], dma=True)
            P.add("sp", lambda e: e.dma_start(out=sgb[:], in_=sg_b[l:l + 1, :].to_broadcast([128, 512])), writes=[T_par], dma=True)
            P.add("pool", lambda e: e.dma_start(out=sgw[:], in_=sg_wT[l].rearrange("g q p -> q g p")), writes=[T_par], dma=True)
            P.add("sp", lambda e: e.dma_start(out=n1g[:], in_=n1gT[l]), writes=[T_par], dma=True)
            for r in range(3):
                P.add("sp", lambda e, r=r: e.dma_start(out=modT[:, :, r:r + 1], in_=mod_d[r, 0:2048].rearrange("(c p o) -> p c o", p=128, o=1),
                                                       allow_slow_non_contiguous=True),
                      reads=[T_mod], writes=[T_modT], dma=True)
            for r in range(3):
                P.add("dve", lambda e, r=r: e.scalar_tensor_tensor(out=gm1[:, :, r], in0=modT[:, 8:16, r], scalar=1.0, in1=n1g[:],
                                                                  op0=ALU.add, op1=ALU.mult), reads=[T_modT, T_par], writes=[T_gm1])
            XT = Ring([(sb([128, D], F32, "xt"), TT("xt%d" % i)) for i in range(4)])
            XN = Ring([(sb([128, D], BF16, "xn"), TT("xn%d" % i)) for i in range(4)])
            HT = Ring([(sb([128, 8, 512], BF16, "hT"), TT("hT%d" % i)) for i in range(2)])
            junk = sb([128, D], BF16, "junk")
            T_junk = TT("junk")
            stat = Ring([(sb([128, 4], F32, "stat"), TT("stat%d" % i)) for i in range(4)])
            SF = Ring([(sb([128, 512], F32, "sf"), TT("sf%d" % i)) for i in range(4)])
            SH = Ring([(sb([128, 512], BF16, "sh"), TT("sh%d" % i)) for i in range(6)])
            UT = Ring([(sb([128, 4, 512], BF16, "uT"), TT("uT%d" % i)) for i in range(1)])
            UC = Ring([(sb([128, 4, 512], BF16, "ucT"), TT("ucT%d" % i)) for i in range(1)])
            VG = Ring([(sb([128, 512], F32, "vg"), TT("vg%d" % i)) for i in range(2)])
            VN = Ring([(sb([128, 512], BF16, "vn"), TT("vn%d" % i)) for i in range(2)])
            STMP = Ring([(sb([128, 512], F32, "stmp"), TT("stmp%d" % i)) for i in range(2)])
            bst = Ring([(sb([128, 8], F32, "bst"), TT("bst%d" % i)) for i in range(2)])

            def norm_part(sti):
                tiles = list(range(4 * sti, 4 * sti + 4))
                hT, T_hT = HT.next()
                xns = []
                for j, ti in enumerate(tiles):
                    xt, T_xt = XT.next()
                    sq, T_sq = stat.next()
                    xn, T_xn = XN.next()
                    xns.append((xn, T_xn))
                    P.add("sp", lambda e, xt=xt, ti=ti: e.dma_start(out=xt[:], in_=xres[sl(ti), :]), reads=[T_xres[ti]], writes=[T_xt], dma=True)
                    P.add("act", lambda e, xt=xt, sq=sq: e.activation(out=junk[:], in_=xt[:], func=AF.Square, accum_out=sq[:, 0:1]),
                          reads=[T_xt], writes=[T_junk, T_sq])
                    P.add("act", lambda e, sq=sq: e.activation(out=sq[:, 1:2], in_=sq[:, 0:1], func=AF.Sqrt, scale=1.0 / D, bias=EPS),
                          reads=[T_sq], writes=[T_sq])
                    P.add("dve", lambda e, sq=sq: e.reciprocal(out=sq[:, 2:3], in_=sq[:, 1:2]), reads=[T_sq], writes=[T_sq])
                    P.add("dve", lambda e, xt=xt, xn=xn, sq=sq: e.tensor_scalar(out=xn[:], in0=xt[:], scalar1=sq[:, 2:3], scalar2=None, op0=ALU.mult),
                          reads=[T_xt, T_sq], writes=[T_xn])
                groups = []
                for j, ti in enumerate(tiles):
                    r = cfg.tile_r(ti)
                    if groups and groups[-1][2] == r:
                        groups[-1][1] = j + 1
                    else:
                        groups.append([j, j + 1, r])
                for kc in range(8):
                    pT, T_pT = PSB.next()
                    for j in range(4):
                        xn, T_xn = xns[j]
                        P.add("pe", lambda e, pT=pT, xn=xn, j=j, kc=kc: e.transpose(out=pT[:, sl(j)], in_=xn[:, sl(kc)], identity=ident[:]),
                              reads=[T_xn, T_const], writes=[T_pT])
                    for (j0, j1, r) in groups:
                        P.add("act", lambda e, pT=pT, hT=hT, kc=kc, j0=j0, j1=j1, r=r: e.activation(
                            out=hT[:, kc, j0 * 128:j1 * 128], in_=pT[:, j0 * 128:j1 * 128], func=AF.Identity,
                            scale=gm1[:, kc, r:r + 1], bias=modT[:, kc, r:r + 1]), reads=[T_pT, T_gm1, T_modT], writes=[T_hT])
                return hT, T_hT

            nxt_h = norm_part(0)
            for sti in range(NST):
                hT, T_hT = nxt_h
                nxt_h = None
                tok = slice(sti * 512, (sti + 1) * 512)
                uT, T_uT = UT.next()
                fm = []
                for c in range(8):
                    fm.append(("xa", c, c * 128))
                for c in range(4):
                    fm.append(("zb", c, OFF_B + c * 128))
                for c in range(8):
                    fm.append(("ga", c, OFF_GA + c * 128))
                for c in range(4):
                    fm.append(("u", c, OFF_U + c * 128))
                for c in range(24):
                    fm.append(("g", c, OFF_G + c * 128))
                for fi, (kind, c, col) in enumerate(fm):
                    if fi == 20 and sti + 1 < NST:
                        nxt_h = norm_part(sti + 1)
                    ps, Tp = PSF.next()
                    for kc in range(8):
                        P.add("pe", lambda e, ps=ps, kc=kc, col=col, hT=hT: e.matmul(ps[:], lhsT=win[:, kc, col:col + 128], rhs=hT[:, kc, :],
                                                                                  start=(kc == 0), stop=(kc == 7)),
                              reads=[T_hT] + Twin(col, col + 128), writes=[Tp])
                    bcol = binT[:, col // 128:col // 128 + 1]
                    if kind in ("xa", "zb"):
                        o, To = SF.next()
                        P.add("dve", lambda e, o=o, ps=ps, bcol=bcol: e.tensor_scalar(out=o[:], in0=ps[:], scalar1=bcol, scalar2=None, op0=ALU.add),
                              reads=[Tp, T_par], writes=[To])
                        dst, Td = (xaT_d, T_xaT) if kind == "xa" else (zbT_d, T_zbT)
                        P.add("sp", lambda e, tok=tok, o=o, dst=dst, c=c: e.dma_start(out=dst[sl(c), tok], in_=o[:]), reads=[To], writes=[TT()], dma=True)
                    elif kind == "u":
                        P.add("act", lambda e, ps=ps, bcol=bcol, c=c, uT=uT: e.activation(out=uT[:, c, :], in_=ps[:], func=AF.Gelu, bias=bcol),
                              reads=[Tp, T_par], writes=[T_uT])
                    else:
                        o, To = SH.next()
                        func = AF.Gelu if kind == "ga" else AF.Sigmoid
                        P.add("act", lambda e, o=o, ps=ps, bcol=bcol, func=func: e.activation(out=o[:], in_=ps[:], func=func, bias=bcol),
                              reads=[Tp, T_par], writes=[To])
                        dst, Td = (gaT_d, T_gaT) if kind == "ga" else (gT_d, T_gT)
                        P.add("sp", lambda e, tok=tok, o=o, dst=dst, c=c: e.dma_start(out=dst[sl(c), tok], in_=o[:]), reads=[To], writes=[TT()], dma=True)
                ucT, T_uc = UC.next()
                for j in range(4):
                    ps, Tp = PSF.next()
                    for kc in range(8):
                        P.add("pe", lambda e, ps=ps, kc=kc, j=j, hT=hT: e.matmul(ps[:], lhsT=hT[:, kc, sl(j)], rhs=win[:, kc, OFF_V:OFF_G],
                                                                              start=(kc == 0), stop=(kc == 7)),
                              reads=[T_hT] + Twin(OFF_V, OFF_G), writes=[Tp])
                    vg, T_vg = VG.next()
                    vn, T_vn = VN.next()
                    bs, T_bs = bst.next()
                    P.add("dve", lambda e, vg=vg, ps=ps: e.tensor_tensor(out=vg[:], in0=ps[:], in1=bv_bc[:], op=ALU.add), reads=[Tp, T_par], writes=[T_vg])
                    P.add("act", lambda e, vg=vg: e.activation(out=vg[:], in_=vg[:], func=AF.Gelu), reads=[T_vg], writes=[T_vg])
                    P.add("dve", lambda e, vg=vg, bs=bs: e.bn_stats(out=bs[:, 0:6], in_=vg[:]), reads=[T_vg], writes=[T_bs])
                    P.add("dve", lambda e, bs=bs: e.bn_aggr(out=bs[:, 6:8], in_=bs[:, 0:6]), reads=[T_bs], writes=[T_bs])
                    P.add("act", lambda e, bs=bs: e.activation(out=bs[:, 0:1], in_=bs[:, 7:8], func=AF.Sqrt, scale=1.0, bias=EPS), reads=[T_bs], writes=[T_bs])
                    P.add("dve", lambda e, bs=bs: e.reciprocal(out=bs[:, 1:2], in_=bs[:, 0:1]), reads=[T_bs], writes=[T_bs])
                    P.add("dve", lambda e, vg=vg, bs=bs: e.tensor_scalar(out=vg[:], in0=vg[:], scalar1=bs[:, 6:7], scalar2=bs[:, 1:2],
                                                                        op0=ALU.subtract, op1=ALU.mult), reads=[T_vg, T_bs], writes=[T_vg])
                    P.add("dve", lambda e, vg=vg: e.tensor_tensor(out=vg[:], in0=vg[:], in1=lng[:], op=ALU.mult), reads=[T_vg, T_par], writes=[T_vg])
                    P.add("dve", lambda e, vg=vg, vn=vn: e.tensor_tensor(out=vn[:], in0=vg[:], in1=lnb[:], op=ALU.add), reads=[T_vg, T_par], writes=[T_vn])
                    ps2, Tp2 = PSF.next()
                    for g in range(4):
                        P.add("pe", lambda e, ps2=ps2, vn=vn, g=g: e.matmul(ps2[:, sl(g)], lhsT=vn[:, sl(g)], rhs=sgw[:, g, :], start=True, stop=True),
                              reads=[T_vn, T_par], writes=[Tp2])
                    stp, T_stp = STMP.next()
                    P.add("dve", lambda e, stp=stp, ps2=ps2: e.tensor_tensor(out=stp[:], in0=ps2[:], in1=sgb[:], op=ALU.add), reads=[Tp2, T_par], writes=[T_stp])
                    P.add("pool", lambda e, stp=stp, ucT=ucT, uT=uT, j=j: e.tensor_tensor(
                        out=ucT[:, :, sl(j)], in0=stp[:].rearrange("p (g q) -> p g q", g=4), in1=uT[:, :, sl(j)], op=ALU.mult),
                        reads=[T_stp, T_uT], writes=[T_uc])
                P.add("sp", lambda e, tok=tok, ucT=ucT: e.dma_start(out=ucT_d.rearrange("(c p) t -> p c t", p=128)[:, :, tok], in_=ucT[:]),
                      reads=[T_uc], writes=[TT()], dma=True)
        P.barrier()

    def phase_B(l):
        PADL, GAP, PADR = 2, 4, 2
        WID = PADL + TC + GAP + TL + PADR
        CO = PADL
        LO = PADL + TC + GAP
        with contextlib.ExitStack() as st:
            sb = mk_sb(st)
            wa = sb([128, 16, 128], BF16, "wa")
            wx = sb([128, 16, 128], BF16, "wx")
            cv = sb([128, 8, 5], F32, "cv")
            lr = sb([128, 2, 8, 3], F32, "lr")
            cl = sb([128, 2, 8], F32, "cl")
            T_par, T_cl = TT("parB"), TT("cl")
            P.add("pool", lambda e: e.dma_start(out=wa[:], in_=lru_wa[l].rearrange("d h i j -> i (d h) j")), writes=[T_par], dma=True)
            P.add("pool", lambda e: e.dma_start(out=wx[:], in_=lru_wx[l].rearrange("d h i j -> i (d h) j")), writes=[T_par], dma=True)
            P.add("sp", lambda e: e.dma_start(out=cv[:].rearrange("p a b -> p (a b)"), in_=convT[l]), writes=[T_par], dma=True)
            P.add("sp", lambda e: e.dma_start(out=lr[:].rearrange("p a b c -> p (a b c)"), in_=lruT[l]), writes=[T_par], dma=True)
            P.add("act", lambda e: e.activation(out=cl[:], in_=lr[:, :, :, 2], func=AF.Exp, scale=-1.0), reads=[T_par], writes=[T_cl])
            P.add("act", lambda e: e.activation(out=cl[:], in_=cl[:], func=AF.Ln, scale=1.0, bias=1.0), reads=[T_cl], writes=[T_cl])
            P.add("dve", lambda e: e.tensor_scalar(out=cl[:], in0=cl[:], scalar1=-8.0, scalar2=None, op0=ALU.mult), reads=[T_cl], writes=[T_cl])
            XA = Ring([(sb([128, WID], F32, "xa"), TT("xa%d" % i)) for i in range(2)])
            for xa, T_xa in XA.items:
                P.add("pool", lambda e, xa=xa: e.memset(xa[:], 0.0), writes=[T_xa])
            xc = sb([128, WID], F32, "xc")
            xcb = sb([128, WID], BF16, "xcb")
            H = sb([128, WID], F32, "H")
            T_xc, T_xcb, T_H = TT("xc"), TT("xcb"), TT("H")
            GA = Ring([(sb([128, TS], BF16, "ga"), TT("ga%d" % i)) for i in range(2)])
            UA = Ring([(sb([128, TS], BF16, "ua"), TT("ua%d" % i)) for i in range(2)])
            SEGW = max(SEG, TC)
            AB = Ring([tuple((sb([128, SEGW], F32, "seg"), TT("seg%d_%d" % (i, q))) for q in range(4)) for i in range(2)])
            for s in range(NS):
                base = s * TS
                for c in range(8):
                    xa, T_xa = XA.next()
                    ga, T_ga = GA.next()
                    ua, T_ua = UA.next()
                    P.add("sp", lambda e, base=base, xa=xa, c=c: e.dma_start(out=xa[:, CO:CO + TC], in_=xaT_d[sl(c), base:base + TC]),
                          reads=[T_xaT], writes=[T_xa], dma=True)
                    P.add("sp", lambda e, base=base, xa=xa, c=c: e.dma_start(out=xa[:, LO:LO + TL], in_=xaT_d[sl(c), base + TC:base + TS]),
                          reads=[T_xaT], writes=[T_xa], dma=True)
                    P.add("sp", lambda e, base=base, ga=ga, c=c: e.dma_start(out=ga[:], in_=gaT_d[sl(c), base:base + TS]), reads=[T_gaT], writes=[T_ga], dma=True)
                    n = WID - 3
                    P.add("pool", lambda e, xa=xa, c=c: e.tensor_scalar(out=xc[:, 2:2 + n], in0=xa[:, 0:n], scalar1=cv[:, c, 0:1], scalar2=cv[:, c, 4:5],
                                                                       op0=ALU.mult, op1=ALU.add), reads=[T_xa, T_par], writes=[T_xc])
                    for j in (1, 2, 3):
                        P.add("dve", lambda e, xa=xa, c=c, j=j: e.scalar_tensor_tensor(out=xc[:, 2:2 + n], in0=xa[:, j:j + n], scalar=cv[:, c, j:j + 1],
                                                                                      in1=xc[:, 2:2 + n], op0=ALU.mult, op1=ALU.add),
                              reads=[T_xa, T_par, T_xc], writes=[T_xc])
                    P.add("act", lambda e: e.activation(out=xcb[:, 2:2 + n], in_=xc[:, 2:2 + n], func=AF.Copy), reads=[T_xc], writes=[T_xcb])
                    segs = [(CO, TC)] + [(LO + i * SEG, SEG) for i in range(TL // SEG)]
                    for d in range(2):
                        order = segs if d == 0 else [segs[0]] + segs[1:][::-1]
                        prev = None
                        for (o0, n0) in order:
                            (A, T_A), (B, T_B), (Tq, T_T), (S, T_S) = AB.next()
                            for p0 in range(0, n0, 512):
                                pn = min(512, n0 - p0)
                                ps_r, Tpr = PSF.next()
                                ps_i, Tpi = PSF.next()
                                P.add("pe", lambda e, ps_r=ps_r, d=d, c=c, o0=o0, p0=p0, pn=pn: e.matmul(
                                    ps_r[:, 0:pn], lhsT=wa[:, d * 8 + c, :], rhs=xcb[:, o0 + p0:o0 + p0 + pn], start=True, stop=True),
                                    reads=[T_par, T_xcb], writes=[Tpr])
                                P.add("pe", lambda e, ps_i=ps_i, d=d, c=c, o0=o0, p0=p0, pn=pn: e.matmul(
                                    ps_i[:, 0:pn], lhsT=wx[:, d * 8 + c, :], rhs=xcb[:, o0 + p0:o0 + p0 + pn], start=True, stop=True),
                                    reads=[T_par, T_xcb], writes=[Tpi])
                                P.add("act", lambda e, A=A, ps_r=ps_r, d=d, c=c, p0=p0, pn=pn: e.activation(
                                    out=A[:, p0:p0 + pn], in_=ps_r[:, 0:pn], func=AF.Sigmoid, bias=lr[:, d, c, 0:1]), reads=[Tpr, T_par], writes=[T_A])
                                P.add("act", lambda e, B=B, ps_i=ps_i, d=d, c=c, p0=p0, pn=pn: e.activation(
                                    out=B[:, p0:p0 + pn], in_=ps_i[:, 0:pn], func=AF.Sigmoid, bias=lr[:, d, c, 1:2]), reads=[Tpi, T_par], writes=[T_B])
                            P.add("act", lambda e, A=A, d=d, c=c, n0=n0: e.activation(out=A[:, 0:n0], in_=A[:, 0:n0], func=AF.Exp, scale=cl[:, d, c:c + 1]),
                                  reads=[T_A, T_cl], writes=[T_A])
                            P.add("pool", lambda e, A=A, Tq=Tq, n0=n0: e.tensor_tensor(out=Tq[:, 0:n0], in0=A[:, 0:n0], in1=A[:, 0:n0], op=ALU.mult),
                                  reads=[T_A], writes=[T_T])
                            P.add("act", lambda e, Tq=Tq, n0=n0: e.activation(out=Tq[:, 0:n0], in_=Tq[:, 0:n0], func=AF.Sqrt, scale=-1.0, bias=1.0),
                                  reads=[T_T], writes=[T_T])
                            P.add("pool", lambda e, B=B, o0=o0, n0=n0: e.tensor_tensor(out=B[:, 0:n0], in0=B[:, 0:n0], in1=xc[:, o0:o0 + n0], op=ALU.mult),
                                  reads=[T_B, T_xc], writes=[T_B])
                            P.add("pool", lambda e, B=B, Tq=Tq, n0=n0: e.tensor_tensor(out=B[:, 0:n0], in0=B[:, 0:n0], in1=Tq[:, 0:n0], op=ALU.mult),
                                  reads=[T_B, T_T], writes=[T_B])
                            if d == 0:
                                init = 0.0 if prev is None else H[:, prev - 1:prev]
                                P.add("dve", lambda e, A=A, B=B, o0=o0, n0=n0, init=init: e.tensor_tensor_scan(
                                    out=H[:, o0:o0 + n0], data0=A[:, 0:n0], data1=B[:, 0:n0], initial=init, op0=ALU.mult, op1=ALU.add),
                                    reads=[T_A, T_B, T_H], writes=[T_H])
                                prev = o0 + n0
                            else:
                                init = 0.0 if prev is None else prev

                                def rv(t, a, b):
                                    return t[:, a:b][:, ::-1]
                                P.add("dve", lambda e, A=A, B=B, S=S, n0=n0, init=init: e.tensor_tensor_scan(
                                    out=rv(S, 0, n0), data0=rv(A, 0, n0), data1=rv(B, 0, n0), initial=init, op0=ALU.mult, op1=ALU.add),
                                    reads=[T_A, T_B] + ([] if prev is None else [prevT]), writes=[T_S])
                                P.add("pool", lambda e, S=S, o0=o0, n0=n0: e.tensor_tensor(out=H[:, o0:o0 + n0], in0=H[:, o0:o0 + n0], in1=S[:, 0:n0], op=ALU.add),
                                      reads=[T_S, T_H], writes=[T_H])
                                prev = S[:, 0:1]
                                prevT = T_S
                    P.add("dve", lambda e, ua=ua, ga=ga: e.tensor_tensor(out=ua[:, 0:TC], in0=H[:, CO:CO + TC], in1=ga[:, 0:TC], op=ALU.mult),
                          reads=[T_H, T_ga], writes=[T_ua])
                    P.add("dve", lambda e, ua=ua, ga=ga: e.tensor_tensor(out=ua[:, TC:TS], in0=H[:, LO:LO + TL], in1=ga[:, TC:TS], op=ALU.mult),
                          reads=[T_H, T_ga], writes=[T_ua])
                    P.add("sp", lambda e, base=base, ua=ua, c=c: e.dma_start(out=uaT_d[sl(c), base:base + TS], in_=ua[:]), reads=[T_ua], writes=[TT()], dma=True)
        P.barrier()

    def phase_B2(l):
        M = 8
        RW, CW = R + 2 * M, GRID_W + 2 * M
        with contextlib.ExitStack() as st:
            sb = mk_sb(st)
            pinv = sb([128, 4, 2 * 64 + TC], F32, "pinv")
            T_par = TT("parB2")
            P.add("sp", lambda e: e.dma_start(out=pinv[:].rearrange("p a b -> p (a b)"),
                                              in_=c_pinv.rearrange("a b -> (a b)").rearrange("(o n) -> o n", o=1).to_broadcast([128, 4 * (128 + TC)])),
                  writes=[T_par], dma=True)
            sets = []
            for i in range(2):
                X = sb([128, RW, CW], F32, "pX")
                P1 = sb([128, RW, CW], F32, "pP1")
                P2 = sb([128, RW, CW], F32, "pP2")
                XC = sb([128, TC + 2 * M], F32, "pXC")
                C1 = sb([128, TC + 2 * M], F32, "pC1")
                C2 = sb([128, TC + 2 * M], F32, "pC2")
                O = sb([128, TS], BF16, "pO")
                Ts = [TT("pool%d_%d" % (i, q)) for q in range(7)]
                eng = "dve" if i == 0 else "pool"
                for t, T in ((X, Ts[0]), (P1, Ts[1]), (P2, Ts[2]), (XC, Ts[3]), (C1, Ts[4]), (C2, Ts[5])):
                    P.add(eng, lambda e, t=t: e.memset(t[:], 0.0), writes=[T])
                sets.append((eng, X, P1, P2, XC, C1, C2, O, Ts))
            it = 0
            for s in range(NS):
                base = s * TS
                for g in range(4):
                    eng, X, P1, P2, XC, C1, C2, O, Ts = sets[it % 2]
                    it += 1
                    m = g + 1
                    P.add("sp", lambda e, base=base, XC=XC, g=g: e.dma_start(out=XC[:, M:M + TC], in_=zbT_d[sl(g), base:base + TC]),
                          reads=[T_zbT], writes=[Ts[3]], dma=True)
                    P.add("sp", lambda e, base=base, X=X, g=g: e.dma_start(out=X[:, M:M + R, M:M + GRID_W],
                                                                  in_=zbT_d[sl(g), base + TC:base + TS].rearrange("p (r c) -> p r c", c=GRID_W)),
                          reads=[T_zbT], writes=[Ts[0]], dma=True)
                    cur, Tcur = X, Ts[0]
                    bufs = [(P1, Ts[1]), (P2, Ts[2])]
                    lo, hi = -M, GRID_W + M - 1
                    bi = 0
                    for i in range(1, m + 1):
                        a, b = (0, 1) if i == 1 else (2 ** (i - 2), 2 ** (i - 2))
                        nlo, nhi = lo + b, hi - a
                        dst, Tdst = bufs[bi % 2]
                        bi += 1
                        w = nhi - nlo + 1
                        P.add(eng, lambda e, dst=dst, cur=cur, nlo=nlo, a=a, b=b, w=w: e.tensor_tensor(
                            out=dst[:, :, M + nlo:M + nlo + w], in0=cur[:, :, M + nlo + a:M + nlo + a + w], in1=cur[:, :, M + nlo - b:M + nlo - b + w], op=ALU.add),
                            reads=[Tcur], writes=[Tdst])
                        cur, Tcur, lo, hi = dst, Tdst, nlo, nhi
                    lo, hi = -M, R + M - 1
                    for i in range(1, m + 1):
                        a, b = (0, 1) if i == 1 else (2 ** (i - 2), 2 ** (i - 2))
                        nlo, nhi = lo + b, hi - a
                        dst, Tdst = bufs[bi % 2]
                        bi += 1
                        w = nhi - nlo + 1
                        P.add(eng, lambda e, dst=dst, cur=cur, nlo=nlo, a=a, b=b, w=w: e.tensor_tensor(
                            out=dst[:, M + nlo:M + nlo + w, M:M + GRID_W], in0=cur[:, M + nlo + a:M + nlo + a + w, M:M + GRID_W],
                            in1=cur[:, M + nlo - b:M + nlo - b + w, M:M + GRID_W], op=ALU.add), reads=[Tcur], writes=[Tdst])
                        cur, Tcur, lo, hi = dst, Tdst, nlo, nhi
                    dst, Tdst = bufs[bi % 2]
                    invc = pinv[:, g, 0:64]
                    invr = pinv[:, g, 64:64 + R]
                    P.add(eng, lambda e, dst=dst, cur=cur, invc=invc: e.tensor_tensor(
                        out=dst[:, M:M + R, M:M + GRID_W], in0=cur[:, M:M + R, M:M + GRID_W],
                        in1=invc.rearrange("p (o c) -> p o c", o=1).to_broadcast([128, R, GRID_W]), op=ALU.mult), reads=[Tcur, T_par], writes=[Tdst])
                    P.add(eng, lambda e, dst=dst, invr=invr: e.tensor_tensor(
                        out=dst[:, M:M + R, M:M + GRID_W], in0=dst[:, M:M + R, M:M + GRID_W],
                        in1=invr.rearrange("p (r o) -> p r o", o=1).to_broadcast([128, R, GRID_W]), op=ALU.mult), reads=[Tdst, T_par], writes=[Tdst])
                    P.add(eng, lambda e, dst=dst, X=X, O=O: e.tensor_tensor(
                        out=O[:, TC:TS].rearrange("p (r c) -> p r c", c=GRID_W), in0=dst[:, M:M + R, M:M + GRID_W],
                        in1=X[:, M:M + R, M:M + GRID_W], op=ALU.subtract), reads=[Tdst, Ts[0]], writes=[Ts[6]])
                    cur, Tcur = XC, Ts[3]
                    cb = [(C1, Ts[4]), (C2, Ts[5])]
                    lo, hi = -M, TC + M - 1
                    bi = 0
                    for i in range(1, m + 1):
                        a, b = (0, 1) if i == 1 else (2 ** (i - 2), 2 ** (i - 2))
                        nlo, nhi = lo + b, hi - a
                        dst, Tdst = cb[bi % 2]
                        bi += 1
                        w = nhi - nlo + 1
                        P.add(eng, lambda e, dst=dst, cur=cur, nlo=nlo, a=a, b=b, w=w: e.tensor_tensor(
                            out=dst[:, M + nlo:M + nlo + w], in0=cur[:, M + nlo + a:M + nlo + a + w], in1=cur[:, M + nlo - b:M + nlo - b + w], op=ALU.add),
                            reads=[Tcur], writes=[Tdst])
                        cur, Tcur, lo, hi = dst, Tdst, nlo, nhi
                    dst, Tdst = cb[bi % 2]
                    P.add(eng, lambda e, dst=dst, cur=cur, g=g: e.tensor_tensor(out=dst[:, M:M + TC], in0=cur[:, M:M + TC], in1=pinv[:, g, 128:128 + TC], op=ALU.mult),
                          reads=[Tcur, T_par], writes=[Tdst])
                    P.add(eng, lambda e, dst=dst, XC=XC, O=O: e.tensor_tensor(out=O[:, 0:TC], in0=dst[:, M:M + TC], in1=XC[:, M:M + TC], op=ALU.subtract),
                          reads=[Tdst, Ts[3]], writes=[Ts[6]])
                    P.add("sp", lambda e, base=base, O=O, g=g: e.dma_start(out=pmT_d[sl(g), base:base + TS], in_=O[:]), reads=[Ts[6]], writes=[TT()], dma=True)
        P.barrier()

    RS = {}

    def alloc_routing(stack):
        sbr = mk_sb(stack)
        RS["lg_all"] = sbr([128, NTILE, NE], F32, "lg_all")
        RS["m8_all"] = sbr([128, NTILE, 8], F32, "m8_all")
        RS["pos_all"] = sbr([128, NTILE, NE], F32, "pos_all")
        RS["w4_all"] = sbr([128, NTILE, 4], F32, "w4_all")
        RS["dest_i"] = sbr([128, NTILE * 4], I32, "dest_i")
        RS["cntbase"] = sbr([128, NE], F32, "cntbase")
        RS["idx_w"] = sbr([128, NBLK], I32, "idx_w")
        RS["idx_b"] = sbr([128, NBLK], I32, "idx_b")
        RS["idx_w8"] = sbr([128, NBLK, 8], I32, "idx_w8")
        RS["tokc"] = sbr([128, NTILE, 2], I32, "tokc")
    T_lg, T_m8, T_pos, T_w4, T_dest, T_cnt = [TT(n) for n in "lg m8 pos w4 dest cnt".split()]

    def load_bc(sb, src_row_ap, n, T, reads=(), name="bc"):
        t = sb([128, n], F32, name)
        P.add("sp", lambda e: e.dma_start(out=t[:], in_=src_row_ap.to_broadcast([128, n])), reads=list(reads), writes=[T], dma=True)
        return t

    def phase_C(l):
        lg_all, m8_all, pos_all, w4_all, cntbase = RS["lg_all"], RS["m8_all"], RS["pos_all"], RS["w4_all"], RS["cntbase"]
        with contextlib.ExitStack() as st:
            sb = mk_sb(st)
            T_w = TT("wC")
            oa = sb([128, 8, D], BF16, "oa")
            ob = sb([128, 4, D], BF16, "ob")
            oc_ = sb([128, 4, D], BF16, "oc")
            wo = sb([128, 8, D], BF16, "wo")
            pw = sb([128, 4, 128], BF16, "pw")
            rw = sb([128, 8, NE], BF16, "rw")
            for t, src in ((oa, out_a[l]), (ob, out_b[l]), (oc_, out_c[l]), (wo, w_o[l]), (rw, router_w[l])):
                P.add("pool", lambda e, t=t, src=src: e.dma_start(out=t[:], in_=src.rearrange("(kc p) n -> p kc n", p=128)), writes=[T_w], dma=True)
            P.add("pool", lambda e: e.dma_start(out=pw[:], in_=pool_w[l].rearrange("g i j -> i g j")), writes=[T_w], dma=True)
            pT_ = sb([128, 4, 2], F32, "poolT")
            P.add("sp", lambda e: e.dma_start(out=pT_[:].rearrange("p a b -> p (a b)"), in_=poolT[l]), writes=[T_w], dma=True)
            T_bc = TT("bcC")
            bo_bc = load_bc(sb, b_o[l:l + 1, :], D, T_bc, name="bo")
            n2g_bc = load_bc(sb, n2g[l:l + 1, :], D, T_bc, name="n2g")
            rb_bc = load_bc(sb, router_b[l:l + 1, :], NE, T_bc, name="rb")
            g1s, bog1s, gm2s, sh2s, T_rows = [], [], [], [], []
            for k in range(2):
                g1s.append(sb([128, D], F32, "g1"))
                gm2s.append(sb([128, D], F32, "gm2"))
                sh2s.append(sb([128, D], F32, "sh2"))
                T_rows.append(TT("rows%d" % k))

            def load_rows(k, r):
                T = T_rows[k]
                P.add("sp", lambda e: e.dma_start(out=g1s[k][:], in_=mod_d[r:r + 1, 2 * D:3 * D].to_broadcast([128, D])), reads=[T_mod], writes=[T], dma=True)
                P.add("sp", lambda e: e.dma_start(out=sh2s[k][:], in_=mod_d[r:r + 1, 3 * D:4 * D].to_broadcast([128, D])), reads=[T_mod], writes=[T], dma=True)
                P.add("sp", lambda e: e.dma_start(out=gm2s[k][:], in_=mod_d[r:r + 1, 4 * D:5 * D].to_broadcast([128, D])), reads=[T_mod], writes=[T], dma=True)
                P.add("dve", lambda e: e.scalar_tensor_tensor(out=gm2s[k][:], in0=gm2s[k][:], scalar=1.0, in1=n2g_bc[:], op0=ALU.add, op1=ALU.mult),
                      reads=[T, T_bc], writes=[T])
            load_rows(0, 2)
            cur_s = [-1]
            IN_UA = Ring([(sb([128, 8, 512], BF16, "uaT"), TT("uaT%d" % i)) for i in range(1)])
            IN_PM = Ring([(sb([128, 4, 512], BF16, "pmT"), TT("pmT%d" % i)) for i in range(1)])
            IN_UC = Ring([(sb([128, 4, 512], BF16, "ucTi"), TT("ucTi%d" % i)) for i in range(1)])
            IN_G = Ring([(sb([128, 24, 512], BF16, "gTi"), TT("gTi%d" % i)) for i in range(1)])
            YB = Ring([(sb([128, 4, 512], BF16, "ybin"), TT("ybin%d" % i)) for i in range(1)])
            MG = Ring([(sb([128, 8, 512], BF16, "mg"), TT("mg%d" % i)) for i in range(1)])
            TM = Ring([(sb([128, 512], F32, "tm"), TT("tm%d" % i)) for i in range(6)])
            XT = Ring([(sb([128, D], F32, "xtC"), TT("xtC%d" % i)) for i in range(3)])
            XF = Ring([(sb([128, D], F32, "xf"), TT("xf%d" % i)) for i in range(2)])
            H2 = Ring([(sb([128, D], BF16, "h2"), TT("h2%d" % i)) for i in range(2)])
            H2T = Ring([(sb([128, 8, 128], BF16, "h2T"), TT("h2T%d" % i)) for i in range(2)])
            junk = sb([128, D], BF16, "junkC")
            T_junk = TT("junkC")
            stat = Ring([(sb([128, 8], F32, "statC"), TT("statC%d" % i)) for i in range(4)])
            mk = Ring([(sb([128, NE], BF16, "mk"), TT("mk%d" % i)) for i in range(2)])
            P.add("dve", lambda e: e.memset(cntbase[:], 0.0), writes=[T_cnt])

            for sti in range(NST):
                tok = slice(sti * 512, (sti + 1) * 512)
                uaT, T_ua = IN_UA.next()
                pmT, T_pm = IN_PM.next()
                ucT, T_uc = IN_UC.next()
                gT, T_g = IN_G.next()
                P.add("sp", lambda e, tok=tok, uaT=uaT: e.dma_start(out=uaT[:], in_=uaT_d.rearrange("(c p) t -> p c t", p=128)[:, :, tok]), reads=[T_uaT], writes=[T_ua], dma=True)
                P.add("sp", lambda e, tok=tok, pmT=pmT: e.dma_start(out=pmT[:], in_=pmT_d.rearrange("(c p) t -> p c t", p=128)[:, :, tok]), reads=[T_pmT], writes=[T_pm], dma=True)
                P.add("sp", lambda e, tok=tok, ucT=ucT: e.dma_start(out=ucT[:], in_=ucT_d.rearrange("(c p) t -> p c t", p=128)[:, :, tok]), reads=[T_ucT], writes=[T_uc], dma=True)
                P.add("sp", lambda e, tok=tok, gT=gT: e.dma_start(out=gT[:], in_=gT_d.rearrange("(c p) t -> p c t", p=128)[:, :, tok]), reads=[T_gT], writes=[T_g], dma=True)
                ybin, T_yb = YB.next()
                for g in range(4):
                    ps, Tp = PSF.next()
                    P.add("pe", lambda e, ps=ps, g=g, pmT=pmT: e.matmul(ps[:], lhsT=pw[:, g, :], rhs=pmT[:, g, :], start=True, stop=True), reads=[T_w, T_pm], writes=[Tp])
                    P.add("dve", lambda e, ps=ps, g=g, ybin=ybin: e.tensor_scalar(out=ybin[:, g, :], in0=ps[:], scalar1=pT_[:, g, 0:1], scalar2=pT_[:, g, 1:2],
                                                                                op0=ALU.add, op1=ALU.mult), reads=[Tp, T_w], writes=[T_yb])
                mg, T_mg = MG.next()
                for oc in range(8):
                    pa, Tpa = PSF.next()
                    pb, Tpb = PSF.next()
                    pc, Tpc = PSF.next()
                    for kc in range(8):
                        P.add("pe", lambda e, pa=pa, kc=kc, oc=oc, uaT=uaT: e.matmul(pa[:], lhsT=oa[:, kc, sl(oc)], rhs=uaT[:, kc, :], start=(kc == 0), stop=(kc == 7)),
                              reads=[T_w, T_ua], writes=[Tpa])
                    for kc in range(4):
                        P.add("pe", lambda e, pb=pb, kc=kc, oc=oc, ybin=ybin: e.matmul(pb[:], lhsT=ob[:, kc, sl(oc)], rhs=ybin[:, kc, :], start=(kc == 0), stop=(kc == 3)),
                              reads=[T_w, T_yb], writes=[Tpb])
                    for kc in range(4):
                        P.add("pe", lambda e, pc=pc, kc=kc, oc=oc, ucT=ucT: e.matmul(pc[:], lhsT=oc_[:, kc, sl(oc)], rhs=ucT[:, kc, :], start=(kc == 0), stop=(kc == 3)),
                              reads=[T_w, T_uc], writes=[Tpc])
                    (t1, T1), (t2, T2), (t3, T3) = TM.next(), TM.next(), TM.next()
                    P.add("dve", lambda e, t1=t1, pa=pa, gT=gT, oc=oc: e.tensor_tensor(out=t1[:], in0=pa[:], in1=gT[:, oc, :], op=ALU.mult), reads=[Tpa, T_g], writes=[T1])
                    P.add("dve", lambda e, t2=t2, pb=pb, gT=gT, oc=oc: e.tensor_tensor(out=t2[:], in0=pb[:], in1=gT[:, 8 + oc, :], op=ALU.mult), reads=[Tpb, T_g], writes=[T2])
                    P.add("dve", lambda e, t3=t3, pc=pc, gT=gT, oc=oc: e.tensor_tensor(out=t3[:], in0=pc[:], in1=gT[:, 16 + oc, :], op=ALU.mult), reads=[Tpc, T_g], writes=[T3])
                    P.add("pool", lambda e, t1=t1, t2=t2: e.tensor_tensor(out=t1[:], in0=t1[:], in1=t2[:], op=ALU.add), reads=[T1, T2], writes=[T1])
                    P.add("pool", lambda e, t1=t1, t3=t3, mg=mg, oc=oc: e.tensor_tensor(out=mg[:, oc, :], in0=t1[:], in1=t3[:], op=ALU.add), reads=[T1, T3], writes=[T_mg])
                for j in range(4):
                    ti = 4 * sti + j
                    r = cfg.tile_r(ti)
                    if r == 2:
                        k = 0
                    else:
                        k = 1
                        if cur_s[0] != r:
                            load_rows(1, r)
                            cur_s[0] = r
                    xt, T_xt = XT.next()
                    P.add("sp", lambda e, xt=xt, ti=ti: e.dma_start(out=xt[:], in_=xres[sl(ti), :]), reads=[T_xres[ti]], writes=[T_xt], dma=True)
                    for nb in range(2):
                        ps, Tp = PSF.next()
                        for kc in range(8):
                            P.add("pe", lambda e, ps=ps, kc=kc, j=j, nb=nb, mg=mg: e.matmul(ps[:], lhsT=mg[:, kc, sl(j)], rhs=wo[:, kc, sl(nb, 512)], start=(kc == 0), stop=(kc == 7)),
                                  reads=[T_w, T_mg], writes=[Tp])
                        t1, T1 = TM.next()
                        P.add("dve", lambda e, t1=t1, ps=ps, nb=nb: e.tensor_tensor(out=t1[:], in0=ps[:], in1=bo_bc[:, sl(nb, 512)], op=ALU.add), reads=[Tp, T_bc], writes=[T1])
                        P.add("pool", lambda e, t1=t1, k=k, nb=nb: e.tensor_tensor(out=t1[:], in0=t1[:], in1=g1s[k][:, sl(nb, 512)], op=ALU.mult), reads=[T1, T_rows[k]], writes=[T1])
                        P.add("dve", lambda e, t1=t1, xt=xt, nb=nb: e.tensor_tensor(out=xt[:, sl(nb, 512)], in0=xt[:, sl(nb, 512)], in1=t1[:], op=ALU.add), reads=[T1, T_xt], writes=[T_xt])
                    P.add("sp", lambda e, xt=xt, ti=ti: e.dma_start(out=xres[sl(ti), :], in_=xt[:]), reads=[T_xt], writes=[T_xres[ti]], dma=True)
                    sq, T_sq = stat.next()
                    xf, T_xf = XF.next()
                    h2, T_h2t = H2.next()
                    P.add("act", lambda e, xt=xt, sq=sq: e.activation(out=junk[:], in_=xt[:], func=AF.Square, accum_out=sq[:, 0:1]), reads=[T_xt], writes=[T_junk, T_sq])
                    P.add("act", lambda e, sq=sq: e.activation(out=sq[:, 1:2], in_=sq[:, 0:1], func=AF.Sqrt, scale=1.0 / D, bias=EPS), reads=[T_sq], writes=[T_sq])
                    P.add("dve", lambda e, sq=sq: e.reciprocal(out=sq[:, 2:3], in_=sq[:, 1:2]), reads=[T_sq], writes=[T_sq])
                    P.add("act", lambda e, xt=xt, xf=xf, sq=sq: e.activation(out=xf[:], in_=xt[:], func=AF.Copy, scale=sq[:, 2:3]), reads=[T_xt, T_sq], writes=[T_xf])
                    P.add("pool", lambda e, xf=xf, k=k: e.tensor_tensor(out=xf[:], in0=xf[:], in1=gm2s[k][:], op=ALU.mult), reads=[T_xf, T_rows[k]], writes=[T_xf])
                    P.add("pool", lambda e, xf=xf, h2=h2, k=k: e.tensor_tensor(out=h2[:], in0=xf[:], in1=sh2s[k][:], op=ALU.add), reads=[T_xf, T_rows[k]], writes=[T_h2t])
                    P.add("sp", lambda e, h2=h2, ti=ti: e.dma_start(out=h2_d[sl(ti), :], in_=h2[:]), reads=[T_h2t], writes=[TT()], dma=True)
                    h2T, T_h2T = H2T.next()
                    pT, T_pT = PSB.next()
                    for kc in range(8):
                        P.add("pe", lambda e, pT=pT, h2=h2, kc=kc: e.transpose(out=pT[:, sl(kc)], in_=h2[:, sl(kc)], identity=ident[:]), reads=[T_h2t, T_const], writes=[T_pT])
                    P.add("act", lambda e, pT=pT, h2T=h2T: e.activation(out=h2T[:].rearrange("p a b -> p (a b)"), in_=pT[:], func=AF.Copy), reads=[T_pT], writes=[T_h2T])
                    ps, Tp = PSF.next()
                    for kc in range(8):
                        P.add("pe", lambda e, ps=ps, kc=kc, h2T=h2T: e.matmul(ps[:, 0:NE], lhsT=h2T[:, kc, :], rhs=rw[:, kc, :], start=(kc == 0), stop=(kc == 7)),
                              reads=[T_w, T_h2T], writes=[Tp])
                    lgt = lg_all[:, ti, :]
                    m8 = m8_all[:, ti, :]
                    P.add("dve", lambda e, ps=ps, lgt=lgt: e.tensor_tensor(out=lgt, in0=ps[:, 0:NE], in1=rb_bc[:], op=ALU.add), reads=[Tp, T_bc], writes=[T_lg])
                    P.add("dve", lambda e, lgt=lgt, m8=m8: e.max(out=m8, in_=lgt), reads=[T_lg], writes=[T_m8])
                    P.add("dve", lambda e, sq=sq, m8=m8: e.tensor_scalar(out=sq[:, 3:4], in0=m8[:, 0:1], scalar1=-1.0, scalar2=None, op0=ALU.mult), reads=[T_m8], writes=[T_sq])
                    w4 = w4_all[:, ti, :]
                    P.add("act", lambda e, sq=sq, m8=m8, w4=w4: e.activation(out=w4, in_=m8[:, 0:4], func=AF.Exp, bias=sq[:, 3:4], accum_out=sq[:, 4:5]),
                          reads=[T_m8, T_sq], writes=[T_w4, T_sq])
                    P.add("dve", lambda e, sq=sq: e.reciprocal(out=sq[:, 5:6], in_=sq[:, 4:5]), reads=[T_sq], writes=[T_sq])
                    P.add("dve", lambda e, sq=sq, w4=w4: e.tensor_scalar(out=w4, in0=w4, scalar1=sq[:, 5:6], scalar2=None, op0=ALU.mult), reads=[T_w4, T_sq], writes=[T_w4])
                    mkt, T_mk = mk.next()
                    P.add("dve", lambda e, mkt=mkt, lgt=lgt, m8=m8: e.tensor_scalar(out=mkt[:], in0=lgt, scalar1=m8[:, 3:4], scalar2=None, op0=ALU.is_ge), reads=[T_lg, T_m8], writes=[T_mk])
                    pp, Tpp = PSF.next()
                    P.add("pe", lambda e, pp=pp, mkt=mkt: e.matmul(pp[:, 0:NE], lhsT=ltri[:], rhs=mkt[:], start=True, stop=True), reads=[T_const, T_mk], writes=[Tpp])
                    P.add("pe", lambda e, pp=pp, mkt=mkt: e.matmul(pp[:, NE:2 * NE], lhsT=ones[:], rhs=mkt[:], start=True, stop=True), reads=[T_const, T_mk], writes=[Tpp])
                    P.add("dve", lambda e, pp=pp, ti=ti: e.tensor_tensor(out=pos_all[:, ti, :], in0=pp[:, 0:NE], in1=cntbase[:], op=ALU.add), reads=[Tpp, T_cnt], writes=[T_pos])
                    P.add("dve", lambda e, pp=pp: e.tensor_tensor(out=cntbase[:], in0=pp[:, NE:2 * NE], in1=cntbase[:], op=ALU.add), reads=[Tpp, T_cnt], writes=[T_cnt])
        P.barrier()

    T_be = TT("be")

    def phase_D(l):
        lg_all, m8_all, pos_all, cntbase = RS["lg_all"], RS["m8_all"], RS["pos_all"], RS["cntbase"]
        dest_i, idx_w, idx_b, tokc = RS["dest_i"], RS["idx_w"], RS["idx_b"], RS["tokc"]
        with contextlib.ExitStack() as st:
            sb = mk_sb(st)
            T_d = TT("D")
            padded = sb([128, NE], F32, "padded")
            pend = sb([128, NE], F32, "pend")
            pstart = sb([128, NE], F32, "pstart")
            onesf = sb([128, NE], F32, "onesf")
            blk = sb([128, NBLK], F32, "blk")
            cmp2 = sb([128, NE, NST + 1], F32, "cmp2")
            cmp_ = sb([128, NBLK, NE], F32, "cmp")
            bef = sb([128, NBLK], F32, "bef")
            dp = sb([128, NE], F32, "dp")
            junk = sb([128, NE], F32, "junkD")
            dest_f = sb([128, NTILE * 4], F32, "dest_f")
            meta0 = sb([128, 2 * (NROWS // 128)], I32, "meta0")
            P.add("sp", lambda e: e.dma_start(out=blk[:], in_=c_blk.to_broadcast([128, NBLK])), writes=[T_d], dma=True)
            P.add("sp", lambda e: e.dma_start(out=tokc[:].rearrange("p a b -> p (a b)"), in_=c_tok), writes=[T_d], dma=True)
            P.add("sp", lambda e: e.dma_start(out=meta0[:], in_=c_meta0), writes=[T_d], dma=True)
            P.add("sp", lambda e: e.dma_start(out=meta_d.rearrange("(p j) c -> p (j c)", p=128), in_=meta0[:]), reads=[T_d], writes=[T_meta], dma=True)
            NJ = NST + 1
            P.add("dve", lambda e: e.tensor_tensor(out=cmp2[:], in0=cntbase[:].rearrange("p (n o) -> p n o", o=1).to_broadcast([128, NE, NJ]),
                                                   in1=blk[:, 0:NJ].rearrange("p (o n) -> p o n", o=1).to_broadcast([128, NE, NJ]), op=ALU.is_gt), reads=[T_cnt, T_d], writes=[T_d])
            P.add("dve", lambda e: e.tensor_reduce(out=padded[:], in_=cmp2[:], axis=AX.X, op=ALU.add), reads=[T_d], writes=[T_d])
            P.add("dve", lambda e: e.tensor_scalar(out=padded[:], in0=padded[:], scalar1=float(MOE_BLOCK), scalar2=None, op0=ALU.mult), reads=[T_d], writes=[T_d])
            P.add("dve", lambda e: e.memset(onesf[:], 1.0), writes=[T_d])
            P.add("dve", lambda e: e.tensor_tensor_scan(out=pend[:], data0=onesf[:], data1=padded[:], initial=0.0, op0=ALU.mult, op1=ALU.add), reads=[T_d], writes=[T_d])
            P.add("dve", lambda e: e.tensor_tensor(out=pstart[:], in0=pend[:], in1=padded[:], op=ALU.subtract), reads=[T_d], writes=[T_d])
            P.add("dve", lambda e: e.tensor_tensor(out=cmp_[:], in0=pend[:].rearrange("p (o n) -> p o n", o=1).to_broadcast([128, NBLK, NE]),
                                                   in1=blk[:].rearrange("p (n o) -> p n o", o=1).to_broadcast([128, NBLK, NE]), op=ALU.is_le), reads=[T_d], writes=[T_d])
            P.add("dve", lambda e: e.tensor_reduce(out=bef[:], in_=cmp_[:], axis=AX.X, op=ALU.add), reads=[T_d], writes=[T_d])
            P.add("dve", lambda e: e.tensor_scalar(out=bef[:], in0=bef[:], scalar1=float(NE - 1), scalar2=None, op0=ALU.min), reads=[T_d], writes=[T_d])
            pidx = sb([128, 1], F32, "pidx")
            P.add("sp", lambda e: e.dma_start(out=pidx[:], in_=c_pidx), writes=[T_d], dma=True)
            P.add("dve", lambda e: e.tensor_scalar(out=bef[:], in0=bef[:], scalar1=float(l * NE), scalar2=None, op0=ALU.add), reads=[T_d], writes=[T_d])
            P.add("dve", lambda e: e.tensor_copy(out=idx_b[:], in_=bef[:]), reads=[T_d], writes=[T_be])
            bw = sb([128, NBLK], F32, "bw")
            P.add("dve", lambda e: e.tensor_scalar(out=bw[:], in0=bef[:], scalar1=128.0, scalar2=pidx[:, 0:1], op0=ALU.mult, op1=ALU.add), reads=[T_d, T_be], writes=[T_d])
            P.add("dve", lambda e: e.tensor_copy(out=idx_w[:], in_=bw[:]), reads=[T_d], writes=[T_be])
            plc = sb([128, 8], F32, "plc")
            i8f = sb([128, NBLK, 8], F32, "i8f")
            P.add("sp", lambda e: e.dma_start(out=plc[:], in_=c_pl.to_broadcast([128, 8])), writes=[T_d], dma=True)
            P.add("dve", lambda e: e.tensor_scalar(out=bw[:], in0=bef[:], scalar1=1024.0, scalar2=pidx[:, 0:1], op0=ALU.mult, op1=ALU.add), reads=[T_d, T_be], writes=[T_d])
            P.add("dve", lambda e: e.tensor_tensor(out=i8f[:], in0=bw[:].rearrange("p (n o) -> p n o", o=1).to_broadcast([128, NBLK, 8]),
                                                   in1=plc[:].rearrange("p (o n) -> p o n", o=1).to_broadcast([128, NBLK, 8]), op=ALU.add), reads=[T_d], writes=[T_d])
            P.add("dve", lambda e: e.tensor_copy(out=RS["idx_w8"][:], in_=i8f[:]), reads=[T_d], writes=[T_be])
            for ti in range(NTILE):
                P.add("dve", lambda e, ti=ti: e.tensor_tensor(out=dp[:], in0=pos_all[:, ti, :], in1=pstart[:], op=ALU.add), reads=[T_pos, T_d], writes=[T_d])
                for k in range(4):
                    P.add("dve", lambda e, ti=ti, k=k: e.scalar_tensor_tensor(out=junk[:], in0=lg_all[:, ti, :], scalar=m8_all[:, ti, k:k + 1], in1=dp[:],
                                                                               op0=ALU.is_equal, op1=ALU.mult, accum_out=dest_f[:, ti * 4 + k:ti * 4 + k + 1]),
                          reads=[T_lg, T_m8, T_d], writes=[T_d])
            P.add("dve", lambda e: e.tensor_copy(out=dest_i[:], in_=dest_f[:]), reads=[T_d], writes=[T_dest])
            if cfg.debug:
                dbg = nc.dram_tensor("dbg_d%d" % l, [128, NTILE * 4 + 2 * NBLK], I32, kind="ExternalOutput").ap()
                dbgf = nc.dram_tensor("dbg_f%d" % l, [128, NE * 3], F32, kind="ExternalOutput").ap()
                T_dbg = TT("dbg")
                P.add("sp", lambda e: e.dma_start(out=dbg[:, 0:NTILE * 4], in_=dest_i[:]), reads=[T_dest], writes=[T_dbg], dma=True)
                P.add("sp", lambda e: e.dma_start(out=dbg[:, NTILE * 4:NTILE * 4 + NBLK], in_=idx_w[:]), reads=[T_be], writes=[T_dbg], dma=True)
                P.add("sp", lambda e: e.dma_start(out=dbg[:, NTILE * 4 + NBLK:], in_=idx_b[:]), reads=[T_be], writes=[T_dbg], dma=True)
                P.add("sp", lambda e: e.dma_start(out=dbgf[:, 0:NE], in_=cntbase[:]), reads=[T_cnt], writes=[T_dbg], dma=True)
                P.add("sp", lambda e: e.dma_start(out=dbgf[:, NE:2 * NE], in_=pend[:]), reads=[T_d], writes=[T_dbg], dma=True)
                P.add("sp", lambda e: e.dma_start(out=dbgf[:, 2 * NE:3 * NE], in_=pstart[:]), reads=[T_d], writes=[T_dbg], dma=True)
                if cfg.stop == "Dpre":
                    P.barrier()
                    return
            for ti in range(NTILE):
                for k in range(4):
                    P.add("pool", lambda e, ti=ti, k=k: e.indirect_dma_start(
                        out=meta_d, out_offset=bass.IndirectOffsetOnAxis(ap=dest_i[:, ti * 4 + k:ti * 4 + k + 1], axis=0),
                        in_=tokc[:, ti, :], in_offset=None), reads=[T_dest, T_d, T_meta], writes=[TT("sc")], dma=True)
        P.barrier()

    def phase_E(l):
        idx_w, idx_b, idx_w8 = RS["idx_w"], RS["idx_b"], RS["idx_w8"]
        with contextlib.ExitStack() as st:
            sb = mk_sb(st)
            WR = Ring([(sb([128, 8, D], BF16, "wexp"), [TT("wexp%d_%d" % (i, q)) for q in range(8)]) for i in range(6)])
            BG = Ring([(sb([128, 16], F32, "bgu"), TT("bgu%d" % i)) for i in range(2)])
            BD = Ring([(sb([128, D], F32, "bd"), TT("bd%d" % i)) for i in range(2)])
            MT = Ring([(sb([128, 4, 2], I32, "mt"), TT("mt%d" % i)) for i in range(2)])
            XG = Ring([(sb([128, 4, D], BF16, "xg"), TT("xg%d" % i)) for i in range(2)])
            XGT = Ring([(sb([128, 8, 512], BF16, "xgT"), TT("xgT%d" % i)) for i in range(1)])
            ACT_ = Ring([(sb([128, 8, 512], BF16, "actT"), TT("actT%d" % i)) for i in range(1)])
            TM = Ring([(sb([128, 512], F32, "tmE"), TT("tmE%d" % i)) for i in range(6)])
            YP = Ring([(sb([128, D], F32, "ypt"), TT("ypt%d" % i)) for i in range(2)])
            zt = sb([128, D], BF16, "zrow")
            T_z = TT("z")
            P.add("pool", lambda e: e.memset(zt[:], 0.0), writes=[T_z])
            P.add("sp", lambda e: e.dma_start(out=h2_d[NT:NT + 128, :], in_=zt[:]), reads=[T_z], writes=[T_h2], dma=True)

            def loads(j):
                d = {}
                mt, T_mt = MT.next()
                P.add("sp", lambda e: e.dma_start(out=mt[:], in_=meta_d[j * 512:(j + 1) * 512, :].rearrange("(jj p) c -> p jj c", p=128)), reads=[T_meta], writes=[T_mt], dma=True)
                xg, T_xg = XG.next()
                for jj in range(4):
                    P.add("pool", lambda e, jj=jj: e.indirect_dma_start(out=xg[:, jj, :], out_offset=None, in_=h2_d,
                                                                        in_offset=bass.IndirectOffsetOnAxis(ap=mt[:, jj, 0:1], axis=0)),
                          reads=[T_mt, T_h2], writes=[T_xg], dma=True)
                ws = []
                for wi, wsrc in enumerate((w_gate, w_up, w_down)):
                    wt, T_wt = WR.next()
                    src = wsrc.rearrange("l e k n -> (l e k) n")
                    for pl in range(8):
                        P.add("pool", lambda e, wt=wt, src=src, pl=pl: e.indirect_dma_start(
                            out=wt[:, pl, :], out_offset=None, in_=src, in_offset=bass.IndirectOffsetOnAxis(ap=idx_w8[:, j, pl:pl + 1], axis=0)),
                            reads=[T_be], writes=[T_wt[pl]], dma=True)
                    ws.append((wt, T_wt))
                bg, T_bg = BG.next()
                bd, T_bd = BD.next()
                P.add("pool", lambda e: e.indirect_dma_start(out=bg[:, 0:8], out_offset=None, in_=b_gateT,
                                                             in_offset=bass.IndirectOffsetOnAxis(ap=idx_w[:, j:j + 1], axis=0)), reads=[T_be], writes=[T_bg], dma=True)
                P.add("pool", lambda e: e.indirect_dma_start(out=bg[:, 8:16], out_offset=None, in_=b_upT,
                                                             in_offset=bass.IndirectOffsetOnAxis(ap=idx_w[:, j:j + 1], axis=0)), reads=[T_be], writes=[T_bg], dma=True)
                P.add("pool", lambda e: e.indirect_dma_start(out=bd[:], out_offset=None, in_=b_down,
                                                             in_offset=bass.IndirectOffsetOnAxis(ap=idx_b[:, j:j + 1], axis=0)), reads=[T_be], writes=[T_bd], dma=True)
                return dict(mt=(mt, T_mt), xg=(xg, T_xg), ws=ws, bg=(bg, T_bg), bd=(bd, T_bd))

            def compute(j, dd):
                mt, T_mt = dd["mt"]
                xg, T_xg = dd["xg"]
                (wg, T_wg), (wu, T_wu), (wd, T_wd) = dd["ws"]
                bg, T_bg = dd["bg"]
                bd, T_bd = dd["bd"]
                xgT, T_xgT = XGT.next()
                for kc in range(8):
                    pT, T_pT = PSB.next()
                    for jj in range(4):
                        P.add("pe", lambda e, pT=pT, jj=jj, kc=kc: e.transpose(out=pT[:, sl(jj)], in_=xg[:, jj, sl(kc)], identity=ident[:]), reads=[T_xg, T_const], writes=[T_pT])
                    P.add("act", lambda e, pT=pT, kc=kc: e.activation(out=xgT[:, kc, :], in_=pT[:, 0:512], func=AF.Copy), reads=[T_pT], writes=[T_xgT])
                actT, T_act = ACT_.next()
                for fc in range(8):
                    pg, Tpg = PSF.next()
                    pu, Tpu = PSF.next()
                    for kc in range(8):
                        P.add("pe", lambda e, pg=pg, kc=kc, fc=fc: e.matmul(pg[:], lhsT=wg[:, kc, sl(fc)], rhs=xgT[:, kc, :], start=(kc == 0), stop=(kc == 7)), reads=[T_wg[kc], T_xgT], writes=[Tpg])
                    for kc in range(8):
                        P.add("pe", lambda e, pu=pu, kc=kc, fc=fc: e.matmul(pu[:], lhsT=wu[:, kc, sl(fc)], rhs=xgT[:, kc, :], start=(kc == 0), stop=(kc == 7)), reads=[T_wu[kc], T_xgT], writes=[Tpu])
                    (gt, Tgt), (sg, Tsg), (up, Tup) = TM.next(), TM.next(), TM.next()
                    P.add("dve", lambda e, gt=gt, pg=pg, fc=fc: e.tensor_scalar(out=gt[:], in0=pg[:], scalar1=bg[:, fc:fc + 1], scalar2=7.0, op0=ALU.add, op1=ALU.min), reads=[Tpg, T_bg], writes=[Tgt])
                    P.add("act", lambda e, gt=gt, sg=sg: e.activation(out=sg[:], in_=gt[:], func=AF.Sigmoid, scale=1.702), reads=[Tgt], writes=[Tsg])
                    P.add("dve", lambda e, up=up, pu=pu, fc=fc: e.tensor_scalar(out=up[:], in0=pu[:], scalar1=bg[:, 8 + fc:9 + fc], scalar2=7.0, op0=ALU.add, op1=ALU.min), reads=[Tpu, T_bg], writes=[Tup])
                    P.add("dve", lambda e, up=up: e.tensor_scalar(out=up[:], in0=up[:], scalar1=-7.0, scalar2=1.0, op0=ALU.max, op1=ALU.add), reads=[Tup], writes=[Tup])
                    P.add("dve", lambda e, gt=gt, sg=sg: e.tensor_tensor(out=gt[:], in0=gt[:], in1=sg[:], op=ALU.mult), reads=[Tgt, Tsg], writes=[Tgt])
                    P.add("dve", lambda e, gt=gt, up=up, fc=fc: e.tensor_tensor(out=actT[:, fc, :], in0=gt[:], in1=up[:], op=ALU.mult), reads=[Tgt, Tup], writes=[T_act])
                if cfg.debug and j == 0:
                    dx = nc.dram_tensor("dbg_x%d" % l, [2, 128, 8, 512], BF16, kind="ExternalOutput").ap()
                    T_dx = TT("dbgx")
                    P.add("sp", lambda e: e.dma_start(out=dx[0], in_=xgT[:]), reads=[T_xgT], writes=[T_dx], dma=True)
                    P.add("sp", lambda e: e.dma_start(out=dx[1], in_=actT[:]), reads=[T_act], writes=[T_dx], dma=True)
                for jj in range(4):
                    ypt, T_ypt = YP.next()
                    for nb in range(2):
                        ps, Tp = PSF.next()
                        for fc in range(8):
                            P.add("pe", lambda e, ps=ps, fc=fc, jj=jj, nb=nb: e.matmul(ps[:], lhsT=actT[:, fc, sl(jj)], rhs=wd[:, fc, sl(nb, 512)], start=(fc == 0), stop=(fc == 7)),
                                  reads=[T_wd[fc], T_act], writes=[Tp])
                        P.add("dve", lambda e, ypt=ypt, ps=ps, nb=nb: e.tensor_tensor(out=ypt[:, sl(nb, 512)], in0=ps[:], in1=bd[:, sl(nb, 512)], op=ALU.add), reads=[Tp, T_bd], writes=[T_ypt])
                    P.add("sp", lambda e, ypt=ypt, jj=jj: e.dma_start(out=yp_d[j * 512 + jj * 128:j * 512 + (jj + 1) * 128, :], in_=ypt[:]), reads=[T_ypt], writes=[TT()], dma=True)

            pend = loads(0)
            if cfg.debug:
                dw = nc.dram_tensor("dbg_w%d" % l, [3, 128, 8, D], BF16, kind="ExternalOutput").ap()
                db = nc.dram_tensor("dbg_b%d" % l, [128, 16 + D], F32, kind="ExternalOutput").ap()
                T_dbg = TT("dbgE")
                for wi in range(3):
                    P.add("sp", lambda e, wi=wi, pd=pend: e.dma_start(out=dw[wi], in_=pd["ws"][wi][0][:]), reads=pend["ws"][wi][1], writes=[T_dbg], dma=True)
                P.add("sp", lambda e, pd=pend: e.dma_start(out=db[:, 0:16], in_=pd["bg"][0][:]), reads=[pend["bg"][1]], writes=[T_dbg], dma=True)
                P.add("sp", lambda e, pd=pend: e.dma_start(out=db[:, 16:], in_=pd["bd"][0][:]), reads=[pend["bd"][1]], writes=[T_dbg], dma=True)
            for j in range(NBLK):
                nxt = loads(j + 1) if j + 1 < NBLK else None
                compute(j, pend)
                pend = nxt
        P.barrier()

    def phase_F(l, last):
        dest_i, w4_all = RS["dest_i"], RS["w4_all"]
        with contextlib.ExitStack() as st:
            sb = mk_sb(st)
            T_bc = TT("bcF")
            g2s = [sb([128, D], F32, "g2") for _ in range(2)]
            T_rows = [TT("rowsF%d" % k) for k in range(2)]
            fg = load_bc(sb, final_g[0:1, :], D, T_bc, name="fg") if last else None

            def load_rows(k, r):
                P.add("sp", lambda e: e.dma_start(out=g2s[k][:], in_=mod_d[r:r + 1, 5 * D:6 * D].to_broadcast([128, D])), reads=[T_mod], writes=[T_rows[k]], dma=True)
            load_rows(0, 2)
            cur_s = [-1]
            YK = Ring([tuple((sb([128, D], F32, "yk"), TT("yk%d_%d" % (i, q))) for q in range(4)) for i in range(4)])
            XT = Ring([(sb([128, D], F32, "xtF"), TT("xtF%d" % i)) for i in range(4)])
            junk = sb([128, D], BF16, "junkF")
            T_junk = TT("junkF")
            stat = Ring([(sb([128, 4], F32, "statF"), TT("statF%d" % i)) for i in range(3)])
            tlist = [ti for ti in range(NTILE) if not (last and cfg.tile_r(ti) == 2)]

            def loads(ti):
                yk = YK.next()
                for q in range(4):
                    P.add("pool", lambda e, q=q, yk=yk, ti=ti: e.indirect_dma_start(out=yk[q][0][:], out_offset=None, in_=yp_d,
                                                                                  in_offset=bass.IndirectOffsetOnAxis(ap=dest_i[:, ti * 4 + q:ti * 4 + q + 1], axis=0)),
                          reads=[T_dest, T_yp], writes=[yk[q][1]], dma=True)
                xt, T_xt = XT.next()
                P.add("sp", lambda e, xt=xt, ti=ti: e.dma_start(out=xt[:], in_=xres[sl(ti), :]), reads=[T_xres[ti]], writes=[T_xt], dma=True)
                return yk, xt, T_xt
            AHEAD = 2
            pend = [loads(ti) for ti in tlist[:AHEAD]]
            for n, ti in enumerate(tlist):
                if n + AHEAD < len(tlist):
                    pend.append(loads(tlist[n + AHEAD]))
                yk, xt, T_xt = pend.pop(0)
                r = cfg.tile_r(ti)
                if r == 2:
                    k = 0
                else:
                    k = 1
                    if cur_s[0] != r:
                        load_rows(1, r)
                        cur_s[0] = r
                P.add("act", lambda e, yk=yk, ti=ti: e.activation(out=yk[0][0][:], in_=yk[0][0][:], func=AF.Copy, scale=w4_all[:, ti, 0:1]),
                      reads=[yk[0][1], T_w4], writes=[yk[0][1]])
                for q in (1, 2, 3):
                    P.add("dve", lambda e, yk=yk, ti=ti, q=q: e.scalar_tensor_tensor(out=yk[0][0][:], in0=yk[q][0][:], scalar=w4_all[:, ti, q:q + 1], in1=yk[0][0][:],
                                                                                    op0=ALU.mult, op1=ALU.add), reads=[yk[0][1], yk[q][1], T_w4], writes=[yk[0][1]])
                P.add("dve", lambda e, yk=yk, k=k: e.tensor_tensor(out=yk[0][0][:], in0=yk[0][0][:], in1=g2s[k][:], op=ALU.mult), reads=[yk[0][1], T_rows[k]], writes=[yk[0][1]])
                P.add("dve", lambda e, yk=yk, xt=xt: e.tensor_tensor(out=xt[:], in0=xt[:], in1=yk[0][0][:], op=ALU.add), reads=[yk[0][1], T_xt], writes=[T_xt])
                if not last:
                    P.add("sp", lambda e, xt=xt, ti=ti: e.dma_start(out=xres[sl(ti), :], in_=xt[:]), reads=[T_xt], writes=[T_xres[ti]], dma=True)
                else:
                    sq, T_sq = stat.next()
                    P.add("act", lambda e, xt=xt, sq=sq: e.activation(out=junk[:], in_=xt[:], func=AF.Square, accum_out=sq[:, 0:1]), reads=[T_xt], writes=[T_junk, T_sq])
                    P.add("act", lambda e, sq=sq: e.activation(out=sq[:, 1:2], in_=sq[:, 0:1], func=AF.Sqrt, scale=1.0 / D, bias=EPS), reads=[T_sq], writes=[T_sq])
                    P.add("dve", lambda e, sq=sq: e.reciprocal(out=sq[:, 2:3], in_=sq[:, 1:2]), reads=[T_sq], writes=[T_sq])
                    P.add("act", lambda e, xt=xt, sq=sq: e.activation(out=xt[:], in_=xt[:], func=AF.Copy, scale=sq[:, 2:3]), reads=[T_xt, T_sq], writes=[T_xt])
                    P.add("pool", lambda e, xt=xt: e.tensor_tensor(out=xt[:], in0=xt[:], in1=fg[:], op=ALU.mult), reads=[T_xt, T_bc], writes=[T_xt])
                    s = (ti * 128) // TS
                    orow = s * TL + (ti * 128 - s * TS - TC)
                    P.add("sp", lambda e, xt=xt, orow=orow: e.dma_start(out=out[orow:orow + 128, :], in_=xt[:]), reads=[T_xt], writes=[TT()], dma=True)
        P.barrier()

    P.barrier()
    stop = cfg.stop
    for l in range(L):
        P.phase = "mod"
        phase_mod(l)
        P.phase = "A"
        phase_A(l)
        if stop == "A":
            break
        P.phase = "B"
        phase_B(l)
        if stop == "B":
            break
        P.phase = "B2"
        phase_B2(l)
        if stop == "B2":
            break
        with contextlib.ExitStack() as rst:
            alloc_routing(rst)
            P.phase = "C"
            phase_C(l)
            if stop == "C":
                break
            P.phase = "D"
            phase_D(l)
            if stop in ("D", "Dpre"):
                break
            P.phase = "E"
            phase_E(l)
            if stop == "E":
                break
            P.phase = "F"
            phase_F(l, l == L - 1)
    P.add("sp", lambda e: e.nop(), reads=[T_meta, T_h2, T_mod] + T_xres)
    P.emit()
    top.close()
    return nc, P


def host_consts(cfg):
    bf = ml_dtypes.bfloat16
    ident = np.eye(128, dtype=np.float32).astype(bf)
    ltri = np.triu(np.ones((128, 128), np.float32), 1).astype(bf)
    ones = np.ones((128, 128), np.float32).astype(bf)

    def inv_counts(T, k):
        pos = np.arange(T)
        lo = np.clip(pos - k // 2, 0, T)
        hi = np.clip(pos + (k - k // 2), 0, T)
        return (1.0 / (hi - lo)).astype(np.float32)
    pinv = np.zeros((4, 128 + cfg.TC), np.float32)
    for g, k in enumerate(POOL_K):
        pinv[g, 0:64] = inv_counts(GRID_W, k)
        pinv[g, 64:128] = 0
        ir = inv_counts(cfg.R, k)
        pinv[g, 64:64 + min(64, cfg.R)] = ir[:64]
        pinv[g, 128:] = inv_counts(cfg.TC, k)
    blk = (np.arange(cfg.NBLK, dtype=np.float32) * MOE_BLOCK).reshape(1, -1)
    tok = np.zeros((128, cfg.NTILE, 2), np.int32)
    tok[:, :, 0] = np.arange(cfg.NTILE, dtype=np.int32)[None, :] * 128 + np.arange(128, dtype=np.int32)[:, None]
    tok = tok.reshape(128, -1)
    meta0 = np.zeros((128, cfg.NROWS // 128, 2), np.int32)
    meta0[:, :, 0] = cfg.NT
    return dict(c_pidx=np.arange(128, dtype=np.float32).reshape(128, 1), c_pl=(128.0 * np.arange(8, dtype=np.float32)).reshape(1, 8), c_ident=ident, c_ltri=ltri, c_ones=ones, c_pinv=pinv, c_blk=blk, c_tok=tok,
                c_meta0=meta0.reshape(128, -1))


def host_params(inp, L):
    f = np.float32
    g = lambda k: np.asarray(inp[k], dtype=f)
    d = {}
    d["ada_w"] = g("ada_w")
    d["ada_b"] = g("ada_b")
    d["n1gT"] = np.ascontiguousarray(g("norm1_g").reshape(L, 8, 128).transpose(0, 2, 1))
    d["n2g"] = g("norm2_g")
    d["final_g"] = g("final_g").reshape(1, D)
    d["w_in"] = g("w_in")
    d["b_inT"] = np.ascontiguousarray(g("b_in").reshape(L, 52, 128).transpose(0, 2, 1))
    d["b_in"] = g("b_in")
    cw = g("conv_w").reshape(L, 4, 8, 128)
    cb = g("conv_b").reshape(L, 1, 8, 128)
    d["convT"] = np.ascontiguousarray(np.concatenate([cw, cb], axis=1).transpose(0, 3, 2, 1)).reshape(L, 128, 40)
    lr = np.stack([g("lru_ba"), g("lru_bx"), g("lru_lambda")], axis=-1).reshape(L, 2, 8, 128, 3)
    d["lruT"] = np.ascontiguousarray(lr.transpose(0, 3, 1, 2, 4)).reshape(L, 128, 48)
    d["lru_wa"] = g("lru_wa")
    d["lru_wx"] = g("lru_wx")
    for k in ("out_a", "out_b", "out_c", "w_o", "b_o", "pool_w", "router_w", "router_b", "w_gate", "w_up", "w_down", "b_down"):
        d[k] = g(k)
    pt = np.stack([g("pool_b"), g("pool_scale")], axis=-1).reshape(L, 4, 128, 2)
    d["poolT"] = np.ascontiguousarray(pt.transpose(0, 2, 1, 3)).reshape(L, 128, 8)
    d["sg_ln"] = np.stack([g("sg_ln_g"), g("sg_ln_b")], axis=1)
    d["sg_wT"] = np.ascontiguousarray(g("sg_w").transpose(0, 1, 3, 2))
    d["sg_b"] = g("sg_b").reshape(L, 512)
    d["b_down"] = g("b_down").reshape(L * NE, D)
    d["b_gateT"] = np.ascontiguousarray(g("b_gate").reshape(L, NE, 8, 128).transpose(0, 1, 3, 2)).reshape(L * NE * 128, 8)
    d["b_upT"] = np.ascontiguousarray(g("b_up").reshape(L, NE, 8, 128).transpose(0, 1, 3, 2)).reshape(L * NE * 128, 8)
    return d


def core_inputs(inp, cfg, core):
    f = np.float32
    x = np.asarray(inp["x"], dtype=f)
    ctx = np.asarray(inp["ctx"], dtype=f)
    c = np.asarray(inp["c"], dtype=f)
    cc = np.asarray(inp["c_ctx"], dtype=f)
    rows = []
    for s in range(cfg.NS):
        b = core * cfg.NS + s
        rows.append(ctx[b])
        rows.append(x[b])
    xin = np.ascontiguousarray(np.concatenate(rows, axis=0))
    cv = np.stack([c[core * cfg.NS + s] for s in range(cfg.NS)] + [cc], axis=0)
    cT = np.ascontiguousarray(cv.reshape(3, 8, 128).transpose(2, 1, 0)).reshape(128, 24)
    return dict(xin=xin, cT=cT)


_CACHE = {}


def kernel(**inputs):
    cfg = Cfg()
    n_cores = 8
    if "nc" not in _CACHE:
        _CACHE["nc"] = build(cfg)[0]
    nc = _CACHE["nc"]
    shared = host_params(inputs, cfg.L)
    shared.update(host_consts(cfg))
    in_maps = []
    for core in range(n_cores):
        m = dict(shared)
        m.update(core_inputs(inputs, cfg, core))
        in_maps.append(m)
    res = run_bass_kernel_spmd(nc, in_maps, core_ids=list(range(n_cores)))
    outs = [np.asarray(r["out"]).reshape(cfg.NS, cfg.TL, D) for r in res.results]
    return np.concatenate(outs, axis=0).astype(np.float32)
```

```python
import contextlib
import numpy as np
import ml_dtypes
import concourse.bass as bass
import concourse.mybir as mybir
from concourse.bass_utils import run_bass_kernel_spmd

F32 = mybir.dt.float32
BF16 = mybir.dt.bfloat16
I32 = mybir.dt.int32
AF = mybir.ActivationFunctionType
ALU = mybir.AluOpType
AX = mybir.AxisListType

D = 1024
NE = 32
EPS = 1e-6
POOL_K = (2, 4, 8, 16)
GRID_W = 64
MOE_BLOCK = 512


class TT:
    __slots__ = ("name", "w", "r", "rd")

    def __init__(self, name=""):
        self.name = name
        self.w = None
        self.r = {}
        self.rd = []


class Prog:
    ENGS = ("pe", "act", "dve", "pool", "sp")
    NDMASEM = 24

    profile = False
    same_engine_raw = True

    def __init__(self, nc):
        self.nc = nc
        self.ops = []

    def add(self, eng, fn, reads=(), writes=(), dma=False):
        i = len(self.ops)
        deps = {}

        def dep(j, kind):
            if j is None:
                return
            if deps.get(j) != "raw":
                deps[j] = kind

        for t in reads:
            dep(t.w, "raw")
        for t in writes:
            dep(t.w, "waw")
            for j in t.r.values():
                dep(j, "war")
            for j in t.rd:
                dep(j, "war")
        for t in reads:
            if dma:
                t.rd.append(i)
            else:
                t.r[eng] = i
        for t in writes:
            t.w = i
            t.r = {}
            t.rd = []
        self.ops.append(dict(eng=eng, fn=fn, deps=deps, dma=dma, sig=False, ph=getattr(self, "phase", None)))
        return i

    def barrier(self):
        bt = [TT("bar_" + e) for e in self.ENGS]
        pend = TT("bar_dma")
        pend.rd = [i for i, op in enumerate(self.ops) if op["dma"] and i >= getattr(self, "_bar_from", 0)]
        self.add("sp", lambda e: e.nop(), writes=[pend, bt[4]])
        for k, e in enumerate(self.ENGS[:4]):
            self.add(e, self.bar_fn[e], writes=[bt[k]] + self.bar_tt.get(e, []))
        for k, e in enumerate(self.ENGS):
            self.add(e, (lambda ee: ee.nop()), reads=bt)
        self._bar_from = len(self.ops)

    def emit(self):
        nc = self.nc
        ops = self.ops
        for i, op in enumerate(ops):
            need = []
            for j, kind in op["deps"].items():
                pj = ops[j]
                if pj["dma"]:
                    need.append(j)
                    continue
                if pj["eng"] == op["eng"] and not op["dma"]:
                    if op["eng"] == "pe":
                        continue
                    if kind != "raw" or not self.same_engine_raw:
                        continue
                need.append(j)
                pj["sig"] = True
            op["need"] = need
        st = contextlib.ExitStack()
        esem = {e: st.enter_context(nc.semaphore("S_" + e)) for e in self.ENGS}
        dsem = {e: [st.enter_context(nc.semaphore("D_%s%d" % (e, k))) for k in range(self.NDMASEM)]
                for e in ("sp", "act", "pool")}
        ecount = {e: 0 for e in self.ENGS}
        dcount = {e: [0] * self.NDMASEM for e in dsem}
        dnext = {e: 0 for e in dsem}
        for op in ops:
            e = op["eng"]
            if op["dma"]:
                k = dnext[e] % self.NDMASEM
                dnext[e] += 1
                op["prev"] = (dsem[e][k], dcount[e][k])
                dcount[e][k] += 16
                op["done"] = (dsem[e][k], dcount[e][k])
            elif op["sig"]:
                ecount[e] += 1
                op["done"] = (esem[e], ecount[e])
        self.stats = dict(n_ops=len(ops), sig=dict(ecount), ndma=dict(dnext))
        block = st.enter_context(nc.Block())

        def run(ename):
            def body(eng):
                waited = {}
                for op in ops:
                    if op["eng"] != ename:
                        continue
                    ws = []
                    if op["dma"] and op["prev"][1] > 0:
                        ws.append(op["prev"])
                    for j in op["need"]:
                        ws.append(ops[j]["done"])
                    best = {}
                    for s, c in ws:
                        if c > best.get(s.name, (None, 0))[1]:
                            best[s.name] = (s, c)
                    for s, c in best.values():
                        if waited.get(s.name, 0) >= c:
                            continue
                        eng.wait_ge(s, c)
                        waited[s.name] = c
                    if self.profile and op["ph"]:
                        with nc.named_scope(op["ph"]):
                            ins = op["fn"](eng)
                    else:
                        ins = op["fn"](eng)
                    if op["dma"]:
                        ins.then_inc(op["done"][0], 16)
                    elif op["sig"]:
                        ins.then_inc(op["done"][0], 1)
            return body

        block.tensor(run("pe"))
        block.scalar(run("act"))
        block.vector(run("dve"))
        block.gpsimd(run("pool"))
        block.sync(run("sp"))
        st.close()


class Ring:
    def __init__(self, items):
        self.items = items
        self.i = 0

    def next(self):
        it = self.items[self.i % len(self.items)]
        self.i += 1
        return it


class Cfg:
    def __init__(self, NS=2, TC=256, TL=4096, L=4, debug=False, stop=None):
        self.NS, self.TC, self.TL, self.L, self.debug, self.stop = NS, TC, TL, L, debug, stop
        self.TS = TC + TL
        self.NT = NS * self.TS
        assert self.NT % 512 == 0 and TC % 128 == 0 and TL % 128 == 0
        self.NTILE = self.NT // 128
        self.NST = self.NT // 512
        self.R = TL // GRID_W
        self.SEG = 1024 if TL % 1024 == 0 else TL
        self.NBLK = (self.NT * 4 + NE * (MOE_BLOCK - 1) + MOE_BLOCK - 1) // MOE_BLOCK
        self.NROWS = self.NBLK * MOE_BLOCK

    def tile_r(self, ti):
        s = (ti * 128) // self.TS
        off = ti * 128 - s * self.TS
        return 2 if off < self.TC else s


OFF_GA, OFF_B, OFF_U, OFF_V, OFF_G, IN_COLS = 1024, 2048, 2560, 3072, 3584, 6656


def build(cfg):
    nc = bass.Bass("TRN2", target_bir_lowering=False)
    P = Prog(nc)
    NS, TC, TL, L, TS, NT, NTILE, NST = cfg.NS, cfg.TC, cfg.TL, cfg.L, cfg.TS, cfg.NT, cfg.NTILE, cfg.NST
    NBLK, NROWS, R, SEG = cfg.NBLK, cfg.NROWS, cfg.R, cfg.SEG

    def din(name, shape, dt=F32):
        return nc.dram_tensor(name, list(shape), dt, kind="ExternalInput").ap()

    def dscr(name, shape, dt):
        kind = "ExternalOutput" if cfg.debug else "Internal"
        return nc.dram_tensor(name, list(shape), dt, kind=kind).ap()

    xin = din("xin", [NT, D])
    cT = din("cT", [128, 8 * 3])
    ada_w = din("ada_w", [L, D, 6 * D])
    ada_b = din("ada_b", [L, 6 * D])
    n1gT = din("n1gT", [L, 128, 8])
    n2g = din("n2g", [L, D])
    final_g = din("final_g", [1, D])
    w_in = din("w_in", [L, D, IN_COLS])
    b_inT = din("b_inT", [L, 128, 52])
    b_in = din("b_in", [L, IN_COLS])
    convT = din("convT", [L, 128, 8 * 5])
    lruT = din("lruT", [L, 128, 2 * 8 * 3])
    lru_wa = din("lru_wa", [L, 2, 8, 128, 128])
    lru_wx = din("lru_wx", [L, 2, 8, 128, 128])
    out_a = din("out_a", [L, D, D])
    out_b = din("out_b", [L, 512, D])
    out_c = din("out_c", [L, 512, D])
    w_o = din("w_o", [L, D, D])
    b_o = din("b_o", [L, D])
    pool_w = din("pool_w", [L, 4, 128, 128])
    poolT = din("poolT", [L, 128, 8])
    sg_ln = din("sg_ln", [L, 2, 512])
    sg_wT = din("sg_wT", [L, 4, 128, 128])
    sg_b = din("sg_b", [L, 512])
    router_w = din("router_w", [L, D, NE])
    router_b = din("router_b", [L, NE])
    w_gate = din("w_gate", [L, NE, D, D])
    w_up = din("w_up", [L, NE, D, D])
    w_down = din("w_down", [L, NE, D, D])
    b_gateT = din("b_gateT", [L * NE * 128, 8])
    b_upT = din("b_upT", [L * NE * 128, 8])
    b_down = din("b_down", [L * NE, D])
    c_ident = din("c_ident", [128, 128], BF16)
    c_ltri = din("c_ltri", [128, 128], BF16)
    c_ones = din("c_ones", [128, 128], BF16)
    c_pinv = din("c_pinv", [4, 2 * 64 + TC])
    c_pidx = din("c_pidx", [128, 1])
    c_pl = din("c_pl", [1, 8])
    c_blk = din("c_blk", [1, NBLK])
    c_tok = din("c_tok", [128, NTILE * 2], I32)
    c_meta0 = din("c_meta0", [128, 2 * (NROWS // 128)], I32)
    out = nc.dram_tensor("out", [NS * TL, D], F32, kind="ExternalOutput").ap()

    xres = dscr("xres", [NT, D], F32)
    mod_d = dscr("mod_d", [3, 6 * D], F32)
    xaT_d = dscr("xaT_d", [D, NT], F32)
    gaT_d = dscr("gaT_d", [D, NT], BF16)
    zbT_d = dscr("zbT_d", [512, NT], F32)
    ucT_d = dscr("ucT_d", [512, NT], BF16)
    gT_d = dscr("gT_d", [3 * D, NT], BF16)
    uaT_d = dscr("uaT_d", [D, NT], BF16)
    pmT_d = dscr("pmT_d", [512, NT], BF16)
    h2_d = dscr("h2_d", [NT + 128, D], BF16)
    meta_d = dscr("meta_d", [NROWS, 2], I32)
    yp_d = dscr("yp_d", [NROWS, D], F32)
    T_xres_all, T_mod, T_xaT, T_gaT, T_zbT, T_ucT, T_gT, T_uaT, T_pmT, T_h2, T_meta, T_yp, T_out = [
        TT(n) for n in "xres mod xaT gaT zbT ucT gT uaT pmT h2 meta yp out".split()]

    T_xres = [TT("xres%d" % i) for i in range(NTILE)]
    top = contextlib.ExitStack()

    def mk_sb(stack):
        cnt = [0]

        def sb(shape, dt, name=None):
            cnt[0] += 1
            return stack.enter_context(nc.sbuf_tensor("%s_%d_%d" % (name or "t", id(stack) % 10007, cnt[0]), list(shape), dt))
        return sb

    sbp = mk_sb(top)
    psf = [top.enter_context(nc.psum_tensor("psf%d" % i, [128, 512], F32)) for i in range(6)]
    psb = [top.enter_context(nc.psum_tensor("psb%d" % i, [128, 1024], BF16)) for i in range(2)]
    PSF = Ring([(psf[i], TT("psf%d" % i)) for i in range(6)])
    PSB = Ring([(psb[i], TT("psb%d" % i)) for i in range(2)])

    bscr = sbp([128, 8], F32, "bscr")
    ident = sbp([128, 128], BF16, "ident")
    ltri = sbp([128, 128], BF16, "ltri")
    ones = sbp([128, 128], BF16, "ones")
    T_const = TT("const")
    P.bar_fn = {
        "pe": lambda e: e.matmul(psf[0][0:1, 0:1], lhsT=ident[:, 0:1], rhs=ident[:, 0:1], start=True, stop=True),
        "act": lambda e: e.activation(out=bscr[0:1, 0:1], in_=bscr[0:1, 1:2], func=AF.Copy),
        "dve": lambda e: e.memset(bscr[0:1, 2:3], 0.0),
        "pool": lambda e: e.memset(bscr[0:1, 4:5], 0.0),
    }
    P.bar_tt = {"pe": [PSF.items[0][1]]}
    P.add("dve", lambda e: e.memset(bscr[:], 0.0), writes=[TT("bscr")])
    siluT = sbp([128, 8, 3], F32, "siluT")
    T_silu = TT("silu")
    for tl, src in ((ident, c_ident), (ltri, c_ltri), (ones, c_ones)):
        P.add("sp", lambda e, tl=tl, src=src: e.dma_start(out=tl[:], in_=src), writes=[T_const], dma=True)
    P.add("sp", lambda e: e.dma_start(out=siluT[:].rearrange("p a b -> p (a b)"), in_=cT), writes=[T_silu], dma=True)
    P.add("act", lambda e: e.activation(out=siluT[:], in_=siluT[:], func=AF.Silu), reads=[T_silu], writes=[T_silu])
    for i in range(NST):
        P.add("sp", lambda e, i=i: e.dma_start(out=xres[i * 512:(i + 1) * 512, :], in_=xin[i * 512:(i + 1) * 512, :]),
              writes=T_xres[4 * i:4 * i + 4], dma=True)

    def sl(i, n=128):
        return slice(i * n, (i + 1) * n)

    def phase_mod(l):
        with contextlib.ExitStack() as st:
            sb = mk_sb(st)
            wr = Ring([(sb([128, 8, 512], F32, "adaw"), TT("adaw%d" % i)) for i in range(2)])
            modrow = sb([3, 6 * D], F32, "modrow")
            adab = sb([3, 6 * D], F32, "adab")
            T_modrow, T_adab = TT("modrow"), TT("adab")
            P.add("sp", lambda e: e.dma_start(out=adab[:], in_=ada_b[l:l + 1, :].to_broadcast([3, 6 * D])),
                  writes=[T_adab], dma=True)
            for cb in range(12):
                wt, Tw = wr.next()
                P.add("sp", lambda e, wt=wt, cb=cb: e.dma_start(
                    out=wt[:], in_=ada_w[l].rearrange("(kc p) n -> p kc n", p=128)[:, :, sl(cb, 512)]), writes=[Tw], dma=True)
                ps, Tp = PSF.next()
                for kc in range(8):
                    P.add("pe", lambda e, ps=ps, wt=wt, kc=kc: e.matmul(ps[0:3, :], lhsT=siluT[:, kc, :], rhs=wt[:, kc, :],
                                                                      start=(kc == 0), stop=(kc == 7)),
                          reads=[T_silu, Tw], writes=[Tp])
                P.add("dve", lambda e, ps=ps, cb=cb: e.tensor_tensor(out=modrow[:, sl(cb, 512)], in0=ps[0:3, :], in1=adab[:, sl(cb, 512)], op=ALU.add),
                      reads=[Tp, T_adab], writes=[T_modrow])
            P.add("sp", lambda e: e.dma_start(out=mod_d, in_=modrow[:]), reads=[T_modrow], writes=[T_mod], dma=True)
        P.barrier()

    def phase_A(l):
        with contextlib.ExitStack() as st:
            sb = mk_sb(st)
            win = sb([128, 8, IN_COLS], BF16, "win")
            T_win = [TT("win%d" % i) for i in range(13)]
            for cb in range(13):
                P.add("pool", lambda e, cb=cb: e.dma_start(out=win[:, :, sl(cb, 512)],
                                                            in_=w_in[l].rearrange("(kc p) n -> p kc n", p=128)[:, :, sl(cb, 512)]),
                      writes=[T_win[cb]], dma=True)

            def Twin(c0, c1):
                return T_win[c0 // 512:(c1 - 1) // 512 + 1]
            binT = sb([128, 52], F32, "binT")
            bv_bc = sb([128, 512], F32, "bv")
            lng = sb([128, 512], F32, "lng")
            lnb = sb([128, 512], F32, "lnb")
            sgb = sb([128, 512], F32, "sgb")
            sgw = sb([128, 4, 128], BF16, "sgw")
            n1g = sb([128, 8], F32, "n1g")
            modT = sb([128, 16, 3], F32, "modT")
            gm1 = sb([128, 8, 3], F32, "gm1")
            T_par, T_modT, T_gm1 = TT("parA"), TT("modT"), TT("gm1")
            P.add("sp", lambda e: e.dma_start(out=binT[:], in_=b_inT[l]), writes=[T_par], dma=True)
            P.add("sp", lambda e: e.dma_start(out=bv_bc[:], in_=b_in[l:l + 1, OFF_V:OFF_G].to_broadcast([128, 512])), writes=[T_par], dma=True)
            P.add("sp", lambda e: e.dma_start(out=lng[:], in_=sg_ln[l, 0:1, :].to_broadcast([128, 512])), writes=[T_par], dma=True)
            P.add("sp", lambda e: e.dma_start(out=lnb[:], in_=sg_ln[l, 1:2, :].to_broadcast([128, 512])), writes=[T_par], dma=True)
            P.add("sp", lambda e: e.dma_start(out=sgb[:], in_=sg_b[l:l + 1, :].to_broadcast([128, 512])), writes=[T_par], dma=True)
            P.add("pool", lambda e: e.dma_start(out=sgw[:], in_=sg_wT[l].rearrange("g q p -> q g p")), writes=[T_par], dma=True)
            P.add("sp", lambda e: e.dma_start(out=n1g[:], in_=n1gT[l]), writes=[T_par], dma=True)
            for r in range(3):
                P.add("sp", lambda e, r=r: e.dma_start(out=modT[:, :, r:r + 1], in_=mod_d[r, 0:2048].rearrange("(c p o) -> p c o", p=128, o=1),
                                                       allow_slow_non_contiguous=True),
                      reads=[T_mod], writes=[T_modT], dma=True)
            for r in range(3):
                P.add("dve", lambda e, r=r: e.scalar_tensor_tensor(out=gm1[:, :, r], in0=modT[:, 8:16, r], scalar=1.0, in1=n1g[:],
                                                                  op0=ALU.add, op1=ALU.mult), reads=[T_modT, T_par], writes=[T_gm1])
            XT = Ring([(sb([128, D], F32, "xt"), TT("xt%d" % i)) for i in range(4)])
            XN = Ring([(sb([128, D], BF16, "xn"), TT("xn%d" % i)) for i in range(4)])
            HT = Ring([(sb([128, 8, 512], BF16, "hT"), TT("hT%d" % i)) for i in range(2)])
            junk = sb([128, D], BF16, "junk")
            T_junk = TT("junk")
            stat = Ring([(sb([128, 4], F32, "stat"), TT("stat%d" % i)) for i in range(4)])
            SF = Ring([(sb([128, 512], F32, "sf"), TT("sf%d" % i)) for i in range(4)])
            SH = Ring([(sb([128, 512], BF16, "sh"), TT("sh%d" % i)) for i in range(6)])
            UT = Ring([(sb([128, 4, 512], BF16, "uT"), TT("uT%d" % i)) for i in range(1)])
            UC = Ring([(sb([128, 4, 512], BF16, "ucT"), TT("ucT%d" % i)) for i in range(1)])
            VG = Ring([(sb([128, 512], F32, "vg"), TT("vg%d" % i)) for i in range(2)])
            VN = Ring([(sb([128, 512], BF16, "vn"), TT("vn%d" % i)) for i in range(2)])
            STMP = Ring([(sb([128, 512], F32, "stmp"), TT("stmp%d" % i)) for i in range(2)])
            bst = Ring([(sb([128, 8], F32, "bst"), TT("bst%d" % i)) for i in range(2)])

            def norm_part(sti):
                tiles = list(range(4 * sti, 4 * sti + 4))
                hT, T_hT = HT.next()
                xns = []
                for j, ti in enumerate(tiles):
                    xt, T_xt = XT.next()
                    sq, T_sq = stat.next()
                    xn, T_xn = XN.next()
                    xns.append((xn, T_xn))
                    P.add("sp", lambda e, xt=xt, ti=ti: e.dma_start(out=xt[:], in_=xres[sl(ti), :]), reads=[T_xres[ti]], writes=[T_xt], dma=True)
                    P.add("act", lambda e, xt=xt, sq=sq: e.activation(out=junk[:], in_=xt[:], func=AF.Square, accum_out=sq[:, 0:1]),
                          reads=[T_xt], writes=[T_junk, T_sq])
                    P.add("act", lambda e, sq=sq: e.activation(out=sq[:, 1:2], in_=sq[:, 0:1], func=AF.Sqrt, scale=1.0 / D, bias=EPS),
                          reads=[T_sq], writes=[T_sq])
                    P.add("dve", lambda e, sq=sq: e.reciprocal(out=sq[:, 2:3], in_=sq[:, 1:2]), reads=[T_sq], writes=[T_sq])
                    P.add("dve", lambda e, xt=xt, xn=xn, sq=sq: e.tensor_scalar(out=xn[:], in0=xt[:], scalar1=sq[:, 2:3], scalar2=None, op0=ALU.mult),
                          reads=[T_xt, T_sq], writes=[T_xn])
                groups = []
                for j, ti in enumerate(tiles):
                    r = cfg.tile_r(ti)
                    if groups and groups[-1][2] == r:
                        groups[-1][1] = j + 1
                    else:
                        groups.append([j, j + 1, r])
                for kc in range(8):
                    pT, T_pT = PSB.next()
                    for j in range(4):
                        xn, T_xn = xns[j]
                        P.add("pe", lambda e, pT=pT, xn=xn, j=j, kc=kc: e.transpose(out=pT[:, sl(j)], in_=xn[:, sl(kc)], identity=ident[:]),
                              reads=[T_xn, T_const], writes=[T_pT])
                    for (j0, j1, r) in groups:
                        P.add("act", lambda e, pT=pT, hT=hT, kc=kc, j0=j0, j1=j1, r=r: e.activation(
                            out=hT[:, kc, j0 * 128:j1 * 128], in_=pT[:, j0 * 128:j1 * 128], func=AF.Identity,
                            scale=gm1[:, kc, r:r + 1], bias=modT[:, kc, r:r + 1]), reads=[T_pT, T_gm1, T_modT], writes=[T_hT])
                return hT, T_hT

            nxt_h = norm_part(0)
            for sti in range(NST):
                hT, T_hT = nxt_h
                nxt_h = None
                tok = slice(sti * 512, (sti + 1) * 512)
                uT, T_uT = UT.next()
                fm = []
                for c in range(8):
                    fm.append(("xa", c, c * 128))
                for c in range(4):
                    fm.append(("zb", c, OFF_B + c * 128))
                for c in range(8):
                    fm.append(("ga", c, OFF_GA + c * 128))
                for c in range(4):
                    fm.append(("u", c, OFF_U + c * 128))
                for c in range(24):
                    fm.append(("g", c, OFF_G + c * 128))
                for fi, (kind, c, col) in enumerate(fm):
                    if fi == 20 and sti + 1 < NST:
                        nxt_h = norm_part(sti + 1)
                    ps, Tp = PSF.next()
                    for kc in range(8):
                        P.add("pe", lambda e, ps=ps, kc=kc, col=col, hT=hT: e.matmul(ps[:], lhsT=win[:, kc, col:col + 128], rhs=hT[:, kc, :],
                                                                                  start=(kc == 0), stop=(kc == 7)),
                              reads=[T_hT] + Twin(col, col + 128), writes=[Tp])
                    bcol = binT[:, col // 128:col // 128 + 1]
                    if kind in ("xa", "zb"):
                        o, To = SF.next()
                        P.add("dve", lambda e, o=o, ps=ps, bcol=bcol: e.tensor_scalar(out=o[:], in0=ps[:], scalar1=bcol, scalar2=None, op0=ALU.add),
                              reads=[Tp, T_par], writes=[To])
                        dst, Td = (xaT_d, T_xaT) if kind == "xa" else (zbT_d, T_zbT)
                        P.add("sp", lambda e, tok=tok, o=o, dst=dst, c=c: e.dma_start(out=dst[sl(c), tok], in_=o[:]), reads=[To], writes=[TT()], dma=True)
                    elif kind == "u":
                        P.add("act", lambda e, ps=ps, bcol=bcol, c=c, uT=uT: e.activation(out=uT[:, c, :], in_=ps[:], func=AF.Gelu, bias=bcol),
                              reads=[Tp, T_par], writes=[T_uT])
                    else:
                        o, To = SH.next()
                        func = AF.Gelu if kind == "ga" else AF.Sigmoid
                        P.add("act", lambda e, o=o, ps=ps, bcol=bcol, func=func: e.activation(out=o[:], in_=ps[:], func=func, bias=bcol),
                              reads=[Tp, T_par], writes=[To])
                        dst, Td = (gaT_d, T_gaT) if kind == "ga" else (gT_d, T_gT)
                        P.add("sp", lambda e, tok=tok, o=o, dst=dst, c=c: e.dma_start(out=dst[sl(c), tok], in_=o[:]), reads=[To], writes=[TT()], dma=True)
                ucT, T_uc = UC.next()
                for j in range(4):
                    ps, Tp = PSF.next()
                    for kc in range(8):
                        P.add("pe", lambda e, ps=ps, kc=kc, j=j, hT=hT: e.matmul(ps[:], lhsT=hT[:, kc, sl(j)], rhs=win[:, kc, OFF_V:OFF_G],
                                                                              start=(kc == 0), stop=(kc == 7)),
                              reads=[T_hT] + Twin(OFF_V, OFF_G), writes=[Tp])
                    vg, T_vg = VG.next()
                    vn, T_vn = VN.next()
                    bs, T_bs = bst.next()
                    P.add("dve", lambda e, vg=vg, ps=ps: e.tensor_tensor(out=vg[:], in0=ps[:], in1=bv_bc[:], op=ALU.add), reads=[Tp, T_par], writes=[T_vg])
                    P.add("act", lambda e, vg=vg: e.activation(out=vg[:], in_=vg[:], func=AF.Gelu), reads=[T_vg], writes=[T_vg])
                    P.add("dve", lambda e, vg=vg, bs=bs: e.bn_stats(out=bs[:, 0:6], in_=vg[:]), reads=[T_vg], writes=[T_bs])
                    P.add("dve", lambda e, bs=bs: e.bn_aggr(out=bs[:, 6:8], in_=bs[:, 0:6]), reads=[T_bs], writes=[T_bs])
                    P.add("act", lambda e, bs=bs: e.activation(out=bs[:, 0:1], in_=bs[:, 7:8], func=AF.Sqrt, scale=1.0, bias=EPS), reads=[T_bs], writes=[T_bs])
                    P.add("dve", lambda e, bs=bs: e.reciprocal(out=bs[:, 1:2], in_=bs[:, 0:1]), reads=[T_bs], writes=[T_bs])
                    P.add("dve", lambda e, vg=vg, bs=bs: e.tensor_scalar(out=vg[:], in0=vg[:], scalar1=bs[:, 6:7], scalar2=bs[:, 1:2],
                                                                        op0=ALU.subtract, op1=ALU.mult), reads=[T_vg, T_bs], writes=[T_vg])
                    P.add("dve", lambda e, vg=vg: e.tensor_tensor(out=vg[:], in0=vg[:], in1=lng[:], op=ALU.mult), reads=[T_vg, T_par], writes=[T_vg])
                    P.add("dve", lambda e, vg=vg, vn=vn: e.tensor_tensor(out=vn[:], in0=vg[:], in1=lnb[:], op=ALU.add), reads=[T_vg, T_par], writes=[T_vn])
                    ps2, Tp2 = PSF.next()
                    for g in range(4):
                        P.add("pe", lambda e, ps2=ps2, vn=vn, g=g: e.matmul(ps2[:, sl(g)], lhsT=vn[:, sl(g)], rhs=sgw[:, g, :], start=True, stop=True),
                              reads=[T_vn, T_par], writes=[Tp2])
                    stp, T_stp = STMP.next()
                    P.add("dve", lambda e, stp=stp, ps2=ps2: e.tensor_tensor(out=stp[:], in0=ps2[:], in1=sgb[:], op=ALU.add), reads=[Tp2, T_par], writes=[T_stp])
                    P.add("pool", lambda e, stp=stp, ucT=ucT, uT=uT, j=j: e.tensor_tensor(
                        out=ucT[:, :, sl(j)], in0=stp[:].rearrange("p (g q) -> p g q", g=4), in1=uT[:, :, sl(j)], op=ALU.mult),
                        reads=[T_stp, T_uT], writes=[T_uc])
                P.add("sp", lambda e, tok=tok, ucT=ucT: e.dma_start(out=ucT_d.rearrange("(c p) t -> p c t", p=128)[:, :, tok], in_=ucT[:]),
                      reads=[T_uc], writes=[TT()], dma=True)
        P.barrier()

    def phase_B(l):
        PADL, GAP, PADR = 2, 4, 2
        WID = PADL + TC + GAP + TL + PADR
        CO = PADL
        LO = PADL + TC + GAP
        with contextlib.ExitStack() as st:
            sb = mk_sb(st)
            wa = sb([128, 16, 128], BF16, "wa")
            wx = sb([128, 16, 128], BF16, "wx")
            cv = sb([128, 8, 5], F32, "cv")
            lr = sb([128, 2, 8, 3], F32, "lr")
            cl = sb([128, 2, 8], F32, "cl")
            T_par, T_cl = TT("parB"), TT("cl")
            P.add("pool", lambda e: e.dma_start(out=wa[:], in_=lru_wa[l].rearrange("d h i j -> i (d h) j")), writes=[T_par], dma=True)
            P.add("pool", lambda e: e.dma_start(out=wx[:], in_=lru_wx[l].rearrange("d h i j -> i (d h) j")), writes=[T_par], dma=True)
            P.add("sp", lambda e: e.dma_start(out=cv[:].rearrange("p a b -> p (a b)"), in_=convT[l]), writes=[T_par], dma=True)
            P.add("sp", lambda e: e.dma_start(out=lr[:].rearrange("p a b c -> p (a b c)"), in_=lruT[l]), writes=[T_par], dma=True)
            P.add("act", lambda e: e.activation(out=cl[:], in_=lr[:, :, :, 2], func=AF.Exp, scale=-1.0), reads=[T_par], writes=[T_cl])
            P.add("act", lambda e: e.activation(out=cl[:], in_=cl[:], func=AF.Ln, scale=1.0, bias=1.0), reads=[T_cl], writes=[T_cl])
            P.add("dve", lambda e: e.tensor_scalar(out=cl[:], in0=cl[:], scalar1=-8.0, scalar2=None, op0=ALU.mult), reads=[T_cl], writes=[T_cl])
            XA = Ring([(sb([128, WID], F32, "xa"), TT("xa%d" % i)) for i in range(2)])
            for xa, T_xa in XA.items:
                P.add("pool", lambda e, xa=xa: e.memset(xa[:], 0.0), writes=[T_xa])
            XCH = Ring([(sb([128, WID], F32, "xc"), sb([128, WID], BF16, "xcb"), sb([128, WID], F32, "H"), TT("xc%d" % i), TT("xcb%d" % i), TT("H%d" % i)) for i in range(2)])
            GA = Ring([(sb([128, TS], BF16, "ga"), TT("ga%d" % i)) for i in range(2)])
            UA = Ring([(sb([128, TS], BF16, "ua"), TT("ua%d" % i)) for i in range(1)])
            SEGW = max(SEG, TC)
            AB = Ring([tuple((sb([128, SEGW], F32, "seg"), TT("seg%d_%d" % (i, q))) for q in range(4)) for i in range(3)])
            for s in range(NS):
                base = s * TS
                for c in range(8):
                    xa, T_xa = XA.next()
                    ga, T_ga = GA.next()
                    ua, T_ua = UA.next()
                    xc, xcb, H, T_xc, T_xcb, T_H = XCH.next()
                    P.add("sp", lambda e, base=base, xa=xa, c=c: e.dma_start(out=xa[:, CO:CO + TC], in_=xaT_d[sl(c), base:base + TC]),
                          reads=[T_xaT], writes=[T_xa], dma=True)
                    P.add("sp", lambda e, base=base, xa=xa, c=c: e.dma_start(out=xa[:, LO:LO + TL], in_=xaT_d[sl(c), base + TC:base + TS]),
                          reads=[T_xaT], writes=[T_xa], dma=True)
                    P.add("sp", lambda e, base=base, ga=ga, c=c: e.dma_start(out=ga[:], in_=gaT_d[sl(c), base:base + TS]), reads=[T_gaT], writes=[T_ga], dma=True)
                    n = WID - 3
                    P.add("pool", lambda e, xc=xc, xa=xa, c=c: e.tensor_scalar(out=xc[:, 2:2 + n], in0=xa[:, 0:n], scalar1=cv[:, c, 0:1], scalar2=cv[:, c, 4:5],
                                                                       op0=ALU.mult, op1=ALU.add), reads=[T_xa, T_par], writes=[T_xc])
                    for j in (1, 2, 3):
                        P.add("dve", lambda e, xc=xc, xa=xa, c=c, j=j: e.scalar_tensor_tensor(out=xc[:, 2:2 + n], in0=xa[:, j:j + n], scalar=cv[:, c, j:j + 1],
                                                                                      in1=xc[:, 2:2 + n], op0=ALU.mult, op1=ALU.add),
                              reads=[T_xa, T_par, T_xc], writes=[T_xc])
                    P.add("act", lambda e, xcb=xcb, xc=xc: e.activation(out=xcb[:, 2:2 + n], in_=xc[:, 2:2 + n], func=AF.Copy), reads=[T_xc], writes=[T_xcb])
                    segs = [(CO, TC)] + [(LO + i * SEG, SEG) for i in range(TL // SEG)]
                    for d in range(2):
                        order = segs if d == 0 else [segs[0]] + segs[1:][::-1]
                        prev = None
                        for (o0, n0) in order:
                            (A, T_A), (B, T_B), (Tq, T_T), (S, T_S) = AB.next()
                            for p0 in range(0, n0, 512):
                                pn = min(512, n0 - p0)
                                ps_r, Tpr = PSF.next()
                                ps_i, Tpi = PSF.next()
                                P.add("pe", lambda e, xcb=xcb, ps_r=ps_r, d=d, c=c, o0=o0, p0=p0, pn=pn: e.matmul(
                                    ps_r[:, 0:pn], lhsT=wa[:, d * 8 + c, :], rhs=xcb[:, o0 + p0:o0 + p0 + pn], start=True, stop=True),
                                    reads=[T_par, T_xcb], writes=[Tpr])
                                P.add("pe", lambda e, xcb=xcb, ps_i=ps_i, d=d, c=c, o0=o0, p0=p0, pn=pn: e.matmul(
                                    ps_i[:, 0:pn], lhsT=wx[:, d * 8 + c, :], rhs=xcb[:, o0 + p0:o0 + p0 + pn], start=True, stop=True),
                                    reads=[T_par, T_xcb], writes=[Tpi])
                                P.add("act", lambda e, A=A, ps_r=ps_r, d=d, c=c, p0=p0, pn=pn: e.activation(
                                    out=A[:, p0:p0 + pn], in_=ps_r[:, 0:pn], func=AF.Sigmoid, bias=lr[:, d, c, 0:1]), reads=[Tpr, T_par], writes=[T_A])
                                P.add("act", lambda e, B=B, ps_i=ps_i, d=d, c=c, p0=p0, pn=pn: e.activation(
                                    out=B[:, p0:p0 + pn], in_=ps_i[:, 0:pn], func=AF.Sigmoid, bias=lr[:, d, c, 1:2]), reads=[Tpi, T_par], writes=[T_B])
                            P.add("act", lambda e, A=A, d=d, c=c, n0=n0: e.activation(out=A[:, 0:n0], in_=A[:, 0:n0], func=AF.Exp, scale=cl[:, d, c:c + 1]),
                                  reads=[T_A, T_cl], writes=[T_A])
                            P.add("dve", lambda e, A=A, Tq=Tq, n0=n0: e.tensor_tensor(out=Tq[:, 0:n0], in0=A[:, 0:n0], in1=A[:, 0:n0], op=ALU.mult),
                                  reads=[T_A], writes=[T_T])
                            P.add("act", lambda e, Tq=Tq, n0=n0: e.activation(out=Tq[:, 0:n0], in_=Tq[:, 0:n0], func=AF.Sqrt, scale=-1.0, bias=1.0),
                                  reads=[T_T], writes=[T_T])
                            P.add("pool", lambda e, xc=xc, B=B, o0=o0, n0=n0: e.tensor_tensor(out=B[:, 0:n0], in0=B[:, 0:n0], in1=xc[:, o0:o0 + n0], op=ALU.mult),
                                  reads=[T_B, T_xc], writes=[T_B])
                            P.add("pool", lambda e, B=B, Tq=Tq, n0=n0: e.tensor_tensor(out=B[:, 0:n0], in0=B[:, 0:n0], in1=Tq[:, 0:n0], op=ALU.mult),
                                  reads=[T_B, T_T], writes=[T_B])
                            if d == 0:
                                init = 0.0 if prev is None else H[:, prev - 1:prev]
                                P.add("dve", lambda e, H=H, A=A, B=B, o0=o0, n0=n0, init=init: e.tensor_tensor_scan(
                                    out=H[:, o0:o0 + n0], data0=A[:, 0:n0], data1=B[:, 0:n0], initial=init, op0=ALU.mult, op1=ALU.add),
                                    reads=[T_A, T_B, T_H], writes=[T_H])
                                prev = o0 + n0
                            else:
                                init = 0.0 if prev is None else prev

                                def rv(t, a, b):
                                    return t[:, a:b][:, ::-1]
                                P.add("dve", lambda e, A=A, B=B, S=S, n0=n0, init=init: e.tensor_tensor_scan(
                                    out=rv(S, 0, n0), data0=rv(A, 0, n0), data1=rv(B, 0, n0), initial=init, op0=ALU.mult, op1=ALU.add),
                                    reads=[T_A, T_B] + ([] if prev is None else [prevT]), writes=[T_S])
                                P.add("pool", lambda e, H=H, S=S, o0=o0, n0=n0: e.tensor_tensor(out=H[:, o0:o0 + n0], in0=H[:, o0:o0 + n0], in1=S[:, 0:n0], op=ALU.add),
                                      reads=[T_S, T_H], writes=[T_H])
                                prev = S[:, 0:1]
                                prevT = T_S
                    P.add("dve", lambda e, H=H, ua=ua, ga=ga: e.tensor_tensor(out=ua[:, 0:TC], in0=H[:, CO:CO + TC], in1=ga[:, 0:TC], op=ALU.mult),
                          reads=[T_H, T_ga], writes=[T_ua])
                    P.add("dve", lambda e, H=H, ua=ua, ga=ga: e.tensor_tensor(out=ua[:, TC:TS], in0=H[:, LO:LO + TL], in1=ga[:, TC:TS], op=ALU.mult),
                          reads=[T_H, T_ga], writes=[T_ua])
                    P.add("sp", lambda e, base=base, ua=ua, c=c: e.dma_start(out=uaT_d[sl(c), base:base + TS], in_=ua[:]), reads=[T_ua], writes=[TT()], dma=True)
        P.barrier()

    def phase_B2(l):
        M = 8
        RW, CW = R + 2 * M, GRID_W + 2 * M
        with contextlib.ExitStack() as st:
            sb = mk_sb(st)
            pinv = sb([128, 4, 2 * 64 + TC], F32, "pinv")
            T_par = TT("parB2")
            P.add("sp", lambda e: e.dma_start(out=pinv[:].rearrange("p a b -> p (a b)"),
                                              in_=c_pinv.rearrange("a b -> (a b)").rearrange("(o n) -> o n", o=1).to_broadcast([128, 4 * (128 + TC)])),
                  writes=[T_par], dma=True)
            sets = []
            for i in range(2):
                X = sb([128, RW, CW], F32, "pX")
                P1 = sb([128, RW, CW], F32, "pP1")
                P2 = sb([128, RW, CW], F32, "pP2")
                XC = sb([128, TC + 2 * M], F32, "pXC")
                C1 = sb([128, TC + 2 * M], F32, "pC1")
                C2 = sb([128, TC + 2 * M], F32, "pC2")
                O = sb([128, TS], BF16, "pO")
                Ts = [TT("pool%d_%d" % (i, q)) for q in range(7)]
                eng = "dve" if i == 0 else "pool"
                for t, T in ((X, Ts[0]), (P1, Ts[1]), (P2, Ts[2]), (XC, Ts[3]), (C1, Ts[4]), (C2, Ts[5])):
                    P.add(eng, lambda e, t=t: e.memset(t[:], 0.0), writes=[T])
                sets.append((eng, X, P1, P2, XC, C1, C2, O, Ts))
            it = 0
            for s in range(NS):
                base = s * TS
                for g in range(4):
                    _, X, P1, P2, XC, C1, C2, O, Ts = sets[it % 2]
                    eng = "pool" if it % 3 == 2 else "dve"
                    it += 1
                    m = g + 1
                    P.add("sp", lambda e, base=base, XC=XC, g=g: e.dma_start(out=XC[:, M:M + TC], in_=zbT_d[sl(g), base:base + TC]),
                          reads=[T_zbT], writes=[Ts[3]], dma=True)
                    P.add("sp", lambda e, base=base, X=X, g=g: e.dma_start(out=X[:, M:M + R, M:M + GRID_W],
                                                                  in_=zbT_d[sl(g), base + TC:base + TS].rearrange("p (r c) -> p r c", c=GRID_W)),
                          reads=[T_zbT], writes=[Ts[0]], dma=True)
                    cur, Tcur = X, Ts[0]
                    bufs = [(P1, Ts[1]), (P2, Ts[2])]
                    lo, hi = -M, GRID_W + M - 1
                    bi = 0
                    for i in range(1, m + 1):
                        a, b = (0, 1) if i == 1 else (2 ** (i - 2), 2 ** (i - 2))
                        nlo, nhi = lo + b, hi - a
                        dst, Tdst = bufs[bi % 2]
                        bi += 1
                        w = nhi - nlo + 1
                        P.add(eng, lambda e, dst=dst, cur=cur, nlo=nlo, a=a, b=b, w=w: e.tensor_tensor(
                            out=dst[:, :, M + nlo:M + nlo + w], in0=cur[:, :, M + nlo + a:M + nlo + a + w], in1=cur[:, :, M + nlo - b:M + nlo - b + w], op=ALU.add),
                            reads=[Tcur], writes=[Tdst])
                        cur, Tcur, lo, hi = dst, Tdst, nlo, nhi
                    lo, hi = -M, R + M - 1
                    for i in range(1, m + 1):
                        a, b = (0, 1) if i == 1 else (2 ** (i - 2), 2 ** (i - 2))
                        nlo, nhi = lo + b, hi - a
                        dst, Tdst = bufs[bi % 2]
                        bi += 1
                        w = nhi - nlo + 1
                        P.add(eng, lambda e, dst=dst, cur=cur, nlo=nlo, a=a, b=b, w=w: e.tensor_tensor(
                            out=dst[:, M + nlo:M + nlo + w, M:M + GRID_W], in0=cur[:, M + nlo + a:M + nlo + a + w, M:M + GRID_W],
                            in1=cur[:, M + nlo - b:M + nlo - b + w, M:M + GRID_W], op=ALU.add), reads=[Tcur], writes=[Tdst])
                        cur, Tcur, lo, hi = dst, Tdst, nlo, nhi
                    dst, Tdst = bufs[bi % 2]
                    invc = pinv[:, g, 0:64]
                    invr = pinv[:, g, 64:64 + R]
                    P.add(eng, lambda e, dst=dst, cur=cur, invc=invc: e.tensor_tensor(
                        out=dst[:, M:M + R, M:M + GRID_W], in0=cur[:, M:M + R, M:M + GRID_W],
                        in1=invc.rearrange("p (o c) -> p o c", o=1).to_broadcast([128, R, GRID_W]), op=ALU.mult), reads=[Tcur, T_par], writes=[Tdst])
                    P.add(eng, lambda e, dst=dst, invr=invr: e.tensor_tensor(
                        out=dst[:, M:M + R, M:M + GRID_W], in0=dst[:, M:M + R, M:M + GRID_W],
                        in1=invr.rearrange("p (r o) -> p r o", o=1).to_broadcast([128, R, GRID_W]), op=ALU.mult), reads=[Tdst, T_par], writes=[Tdst])
                    P.add(eng, lambda e, dst=dst, X=X, O=O: e.tensor_tensor(
                        out=O[:, TC:TS].rearrange("p (r c) -> p r c", c=GRID_W), in0=dst[:, M:M + R, M:M + GRID_W],
                        in1=X[:, M:M + R, M:M + GRID_W], op=ALU.subtract), reads=[Tdst, Ts[0]], writes=[Ts[6]])
                    cur, Tcur = XC, Ts[3]
                    cb = [(C1, Ts[4]), (C2, Ts[5])]
                    lo, hi = -M, TC + M - 1
                    bi = 0
                    for i in range(1, m + 1):
                        a, b = (0, 1) if i == 1 else (2 ** (i - 2), 2 ** (i - 2))
                        nlo, nhi = lo + b, hi - a
                        dst, Tdst = cb[bi % 2]
                        bi += 1
                        w = nhi - nlo + 1
                        P.add(eng, lambda e, dst=dst, cur=cur, nlo=nlo, a=a, b=b, w=w: e.tensor_tensor(
                            out=dst[:, M + nlo:M + nlo + w], in0=cur[:, M + nlo + a:M + nlo + a + w], in1=cur[:, M + nlo - b:M + nlo - b + w], op=ALU.add),
                            reads=[Tcur], writes=[Tdst])
                        cur, Tcur, lo, hi = dst, Tdst, nlo, nhi
                    dst, Tdst = cb[bi % 2]
                    P.add(eng, lambda e, dst=dst, cur=cur, g=g: e.tensor_tensor(out=dst[:, M:M + TC], in0=cur[:, M:M + TC], in1=pinv[:, g, 128:128 + TC], op=ALU.mult),
                          reads=[Tcur, T_par], writes=[Tdst])
                    P.add(eng, lambda e, dst=dst, XC=XC, O=O: e.tensor_tensor(out=O[:, 0:TC], in0=dst[:, M:M + TC], in1=XC[:, M:M + TC], op=ALU.subtract),
                          reads=[Tdst, Ts[3]], writes=[Ts[6]])
                    P.add("sp", lambda e, base=base, O=O, g=g: e.dma_start(out=pmT_d[sl(g), base:base + TS], in_=O[:]), reads=[Ts[6]], writes=[TT()], dma=True)
        P.barrier()

    RS = {}

    def alloc_routing(stack):
        sbr = mk_sb(stack)
        RS["lg_all"] = sbr([128, NTILE, NE], F32, "lg_all")
        RS["m8_all"] = sbr([128, NTILE, 8], F32, "m8_all")
        RS["pos_all"] = sbr([128, NTILE, NE], F32, "pos_all")
        RS["w4_all"] = sbr([128, NTILE, 4], F32, "w4_all")
        RS["dest_i"] = sbr([128, NTILE * 4], I32, "dest_i")
        RS["cntbase"] = sbr([128, NE], F32, "cntbase")
        RS["idx_w"] = sbr([128, NBLK], I32, "idx_w")
        RS["idx_b"] = sbr([128, NBLK], I32, "idx_b")
        RS["idx_w8"] = sbr([128, NBLK, 8], I32, "idx_w8")
        RS["tokc"] = sbr([128, NTILE, 2], I32, "tokc")
    T_lg, T_m8, T_pos, T_w4, T_dest, T_cnt = [TT(n) for n in "lg m8 pos w4 dest cnt".split()]

    def load_bc(sb, src_row_ap, n, T, reads=(), name="bc"):
        t = sb([128, n], F32, name)
        P.add("sp", lambda e: e.dma_start(out=t[:], in_=src_row_ap.to_broadcast([128, n])), reads=list(reads), writes=[T], dma=True)
        return t

    def phase_C(l):
        lg_all, m8_all, pos_all, w4_all, cntbase = RS["lg_all"], RS["m8_all"], RS["pos_all"], RS["w4_all"], RS["cntbase"]
        with contextlib.ExitStack() as st:
            sb = mk_sb(st)
            T_w = TT("wC")
            oa = sb([128, 8, D], BF16, "oa")
            ob = sb([128, 4, D], BF16, "ob")
            oc_ = sb([128, 4, D], BF16, "oc")
            wo = sb([128, 8, D], BF16, "wo")
            pw = sb([128, 4, 128], BF16, "pw")
            rw = sb([128, 8, NE], BF16, "rw")
            for t, src in ((oa, out_a[l]), (ob, out_b[l]), (oc_, out_c[l]), (wo, w_o[l]), (rw, router_w[l])):
                P.add("pool", lambda e, t=t, src=src: e.dma_start(out=t[:], in_=src.rearrange("(kc p) n -> p kc n", p=128)), writes=[T_w], dma=True)
            P.add("pool", lambda e: e.dma_start(out=pw[:], in_=pool_w[l].rearrange("g i j -> i g j")), writes=[T_w], dma=True)
            pT_ = sb([128, 4, 2], F32, "poolT")
            P.add("sp", lambda e: e.dma_start(out=pT_[:].rearrange("p a b -> p (a b)"), in_=poolT[l]), writes=[T_w], dma=True)
            T_bc = TT("bcC")
            bo_bc = load_bc(sb, b_o[l:l + 1, :], D, T_bc, name="bo")
            n2g_bc = load_bc(sb, n2g[l:l + 1, :], D, T_bc, name="n2g")
            rb_bc = load_bc(sb, router_b[l:l + 1, :], NE, T_bc, name="rb")
            g1s, bog1s, gm2s, sh2s, T_rows = [], [], [], [], []
            for k in range(2):
                g1s.append(sb([128, D], F32, "g1"))
                gm2s.append(sb([128, D], F32, "gm2"))
                sh2s.append(sb([128, D], F32, "sh2"))
                T_rows.append(TT("rows%d" % k))

            def load_rows(k, r):
                T = T_rows[k]
                P.add("sp", lambda e: e.dma_start(out=g1s[k][:], in_=mod_d[r:r + 1, 2 * D:3 * D].to_broadcast([128, D])), reads=[T_mod], writes=[T], dma=True)
                P.add("sp", lambda e: e.dma_start(out=sh2s[k][:], in_=mod_d[r:r + 1, 3 * D:4 * D].to_broadcast([128, D])), reads=[T_mod], writes=[T], dma=True)
                P.add("sp", lambda e: e.dma_start(out=gm2s[k][:], in_=mod_d[r:r + 1, 4 * D:5 * D].to_broadcast([128, D])), reads=[T_mod], writes=[T], dma=True)
                P.add("dve", lambda e: e.scalar_tensor_tensor(out=gm2s[k][:], in0=gm2s[k][:], scalar=1.0, in1=n2g_bc[:], op0=ALU.add, op1=ALU.mult),
                      reads=[T, T_bc], writes=[T])
            load_rows(0, 2)
            cur_s = [-1]
            IN_UA = Ring([(sb([128, 8, 512], BF16, "uaT"), TT("uaT%d" % i)) for i in range(1)])
            IN_PM = Ring([(sb([128, 4, 512], BF16, "pmT"), TT("pmT%d" % i)) for i in range(1)])
            IN_UC = Ring([(sb([128, 4, 512], BF16, "ucTi"), TT("ucTi%d" % i)) for i in range(1)])
            IN_G = Ring([(sb([128, 24, 512], BF16, "gTi"), TT("gTi%d" % i)) for i in range(1)])
            YB = Ring([(sb([128, 4, 512], BF16, "ybin"), TT("ybin%d" % i)) for i in range(1)])
            MG = Ring([(sb([128, 8, 512], BF16, "mg"), TT("mg%d" % i)) for i in range(1)])
            TM = Ring([(sb([128, 512], F32, "tm"), TT("tm%d" % i)) for i in range(6)])
            XT = Ring([(sb([128, D], F32, "xtC"), TT("xtC%d" % i)) for i in range(3)])
            XF = Ring([(sb([128, D], F32, "xf"), TT("xf%d" % i)) for i in range(2)])
            H2 = Ring([(sb([128, D], BF16, "h2"), TT("h2%d" % i)) for i in range(2)])
            H2T = Ring([(sb([128, 8, 128], BF16, "h2T"), TT("h2T%d" % i)) for i in range(2)])
            junk = sb([128, D], BF16, "junkC")
            T_junk = TT("junkC")
            stat = Ring([(sb([128, 8], F32, "statC"), TT("statC%d" % i)) for i in range(4)])
            mk = Ring([(sb([128, NE], BF16, "mk"), TT("mk%d" % i)) for i in range(2)])
            P.add("dve", lambda e: e.memset(cntbase[:], 0.0), writes=[T_cnt])

            def loads_C(sti):
                tok = slice(sti * 512, (sti + 1) * 512)
                uaT, T_ua = IN_UA.next()
                pmT, T_pm = IN_PM.next()
                ucT, T_uc = IN_UC.next()
                gT, T_g = IN_G.next()
                P.add("sp", lambda e, tok=tok, uaT=uaT: e.dma_start(out=uaT[:], in_=uaT_d.rearrange("(c p) t -> p c t", p=128)[:, :, tok]), reads=[T_uaT], writes=[T_ua], dma=True)
                P.add("sp", lambda e, tok=tok, pmT=pmT: e.dma_start(out=pmT[:], in_=pmT_d.rearrange("(c p) t -> p c t", p=128)[:, :, tok]), reads=[T_pmT], writes=[T_pm], dma=True)
                P.add("sp", lambda e, tok=tok, ucT=ucT: e.dma_start(out=ucT[:], in_=ucT_d.rearrange("(c p) t -> p c t", p=128)[:, :, tok]), reads=[T_ucT], writes=[T_uc], dma=True)
                P.add("sp", lambda e, tok=tok, gT=gT: e.dma_start(out=gT[:], in_=gT_d.rearrange("(c p) t -> p c t", p=128)[:, :, tok]), reads=[T_gT], writes=[T_g], dma=True)
                return uaT, T_ua, pmT, T_pm, ucT, T_uc, gT, T_g
            nxt_in = loads_C(0)
            for sti in range(NST):
                tok = slice(sti * 512, (sti + 1) * 512)
                uaT, T_ua, pmT, T_pm, ucT, T_uc, gT, T_g = nxt_in
                ybin, T_yb = YB.next()
                for g in range(4):
                    ps, Tp = PSF.next()
                    P.add("pe", lambda e, ps=ps, g=g, pmT=pmT: e.matmul(ps[:], lhsT=pw[:, g, :], rhs=pmT[:, g, :], start=True, stop=True), reads=[T_w, T_pm], writes=[Tp])
                    P.add("dve", lambda e, ps=ps, g=g, ybin=ybin: e.tensor_scalar(out=ybin[:, g, :], in0=ps[:], scalar1=pT_[:, g, 0:1], scalar2=pT_[:, g, 1:2],
                                                                                op0=ALU.add, op1=ALU.mult), reads=[Tp, T_w], writes=[T_yb])
                mg, T_mg = MG.next()
                for oc in range(8):
                    pa, Tpa = PSF.next()
                    pb, Tpb = PSF.next()
                    pc, Tpc = PSF.next()
                    for kc in range(8):
                        P.add("pe", lambda e, pa=pa, kc=kc, oc=oc, uaT=uaT: e.matmul(pa[:], lhsT=oa[:, kc, sl(oc)], rhs=uaT[:, kc, :], start=(kc == 0), stop=(kc == 7)),
                              reads=[T_w, T_ua], writes=[Tpa])
                    for kc in range(4):
                        P.add("pe", lambda e, pb=pb, kc=kc, oc=oc, ybin=ybin: e.matmul(pb[:], lhsT=ob[:, kc, sl(oc)], rhs=ybin[:, kc, :], start=(kc == 0), stop=(kc == 3)),
                              reads=[T_w, T_yb], writes=[Tpb])
                    for kc in range(4):
                        P.add("pe", lambda e, pc=pc, kc=kc, oc=oc, ucT=ucT: e.matmul(pc[:], lhsT=oc_[:, kc, sl(oc)], rhs=ucT[:, kc, :], start=(kc == 0), stop=(kc == 3)),
                              reads=[T_w, T_uc], writes=[Tpc])
                    (t1, T1), (t2, T2), (t3, T3) = TM.next(), TM.next(), TM.next()
                    P.add("dve", lambda e, t1=t1, pa=pa, gT=gT, oc=oc: e.tensor_tensor(out=t1[:], in0=pa[:], in1=gT[:, oc, :], op=ALU.mult), reads=[Tpa, T_g], writes=[T1])
                    P.add("dve", lambda e, t2=t2, pb=pb, gT=gT, oc=oc: e.tensor_tensor(out=t2[:], in0=pb[:], in1=gT[:, 8 + oc, :], op=ALU.mult), reads=[Tpb, T_g], writes=[T2])
                    P.add("dve", lambda e, t3=t3, pc=pc, gT=gT, oc=oc: e.tensor_tensor(out=t3[:], in0=pc[:], in1=gT[:, 16 + oc, :], op=ALU.mult), reads=[Tpc, T_g], writes=[T3])
                    P.add("pool", lambda e, t1=t1, t2=t2: e.tensor_tensor(out=t1[:], in0=t1[:], in1=t2[:], op=ALU.add), reads=[T1, T2], writes=[T1])
                    P.add("pool", lambda e, t1=t1, t3=t3, mg=mg, oc=oc: e.tensor_tensor(out=mg[:, oc, :], in0=t1[:], in1=t3[:], op=ALU.add), reads=[T1, T3], writes=[T_mg])
                if sti + 1 < NST:
                    nxt_in = loads_C(sti + 1)
                for j in range(4):
                    ti = 4 * sti + j
                    r = cfg.tile_r(ti)
                    if r == 2:
                        k = 0
                    else:
                        k = 1
                        if cur_s[0] != r:
                            load_rows(1, r)
                            cur_s[0] = r
                    xt, T_xt = XT.next()
                    P.add("sp", lambda e, xt=xt, ti=ti: e.dma_start(out=xt[:], in_=xres[sl(ti), :]), reads=[T_xres[ti]], writes=[T_xt], dma=True)
                    for nb in range(2):
                        ps, Tp = PSF.next()
                        for kc in range(8):
                            P.add("pe", lambda e, ps=ps, kc=kc, j=j, nb=nb, mg=mg: e.matmul(ps[:], lhsT=mg[:, kc, sl(j)], rhs=wo[:, kc, sl(nb, 512)], start=(kc == 0), stop=(kc == 7)),
                                  reads=[T_w, T_mg], writes=[Tp])
                        t1, T1 = TM.next()
                        P.add("dve", lambda e, t1=t1, ps=ps, nb=nb: e.tensor_tensor(out=t1[:], in0=ps[:], in1=bo_bc[:, sl(nb, 512)], op=ALU.add), reads=[Tp, T_bc], writes=[T1])
                        P.add("pool", lambda e, t1=t1, k=k, nb=nb: e.tensor_tensor(out=t1[:], in0=t1[:], in1=g1s[k][:, sl(nb, 512)], op=ALU.mult), reads=[T1, T_rows[k]], writes=[T1])
                        P.add("dve", lambda e, t1=t1, xt=xt, nb=nb: e.tensor_tensor(out=xt[:, sl(nb, 512)], in0=xt[:, sl(nb, 512)], in1=t1[:], op=ALU.add), reads=[T1, T_xt], writes=[T_xt])
                    P.add("sp", lambda e, xt=xt, ti=ti: e.dma_start(out=xres[sl(ti), :], in_=xt[:]), reads=[T_xt], writes=[T_xres[ti]], dma=True)
                    sq, T_sq = stat.next()
                    xf, T_xf = XF.next()
                    h2, T_h2t = H2.next()
                    P.add("act", lambda e, xt=xt, sq=sq: e.activation(out=junk[:], in_=xt[:], func=AF.Square, accum_out=sq[:, 0:1]), reads=[T_xt], writes=[T_junk, T_sq])
                    P.add("act", lambda e, sq=sq: e.activation(out=sq[:, 1:2], in_=sq[:, 0:1], func=AF.Sqrt, scale=1.0 / D, bias=EPS), reads=[T_sq], writes=[T_sq])
                    P.add("dve", lambda e, sq=sq: e.reciprocal(out=sq[:, 2:3], in_=sq[:, 1:2]), reads=[T_sq], writes=[T_sq])
                    P.add("act", lambda e, xt=xt, xf=xf, sq=sq: e.activation(out=xf[:], in_=xt[:], func=AF.Copy, scale=sq[:, 2:3]), reads=[T_xt, T_sq], writes=[T_xf])
                    P.add("pool", lambda e, xf=xf, k=k: e.tensor_tensor(out=xf[:], in0=xf[:], in1=gm2s[k][:], op=ALU.mult), reads=[T_xf, T_rows[k]], writes=[T_xf])
                    P.add("pool", lambda e, xf=xf, h2=h2, k=k: e.tensor_tensor(out=h2[:], in0=xf[:], in1=sh2s[k][:], op=ALU.add), reads=[T_xf, T_rows[k]], writes=[T_h2t])
                    P.add("sp", lambda e, h2=h2, ti=ti: e.dma_start(out=h2_d[sl(ti), :], in_=h2[:]), reads=[T_h2t], writes=[TT()], dma=True)
                    h2T, T_h2T = H2T.next()
                    pT, T_pT = PSB.next()
                    for kc in range(8):
                        P.add("pe", lambda e, pT=pT, h2=h2, kc=kc: e.transpose(out=pT[:, sl(kc)], in_=h2[:, sl(kc)], identity=ident[:]), reads=[T_h2t, T_const], writes=[T_pT])
                    P.add("act", lambda e, pT=pT, h2T=h2T: e.activation(out=h2T[:].rearrange("p a b -> p (a b)"), in_=pT[:], func=AF.Copy), reads=[T_pT], writes=[T_h2T])
                    ps, Tp = PSF.next()
                    for kc in range(8):
                        P.add("pe", lambda e, ps=ps, kc=kc, h2T=h2T: e.matmul(ps[:, 0:NE], lhsT=h2T[:, kc, :], rhs=rw[:, kc, :], start=(kc == 0), stop=(kc == 7)),
                              reads=[T_w, T_h2T], writes=[Tp])
                    lgt = lg_all[:, ti, :]
                    m8 = m8_all[:, ti, :]
                    P.add("dve", lambda e, ps=ps, lgt=lgt: e.tensor_tensor(out=lgt, in0=ps[:, 0:NE], in1=rb_bc[:], op=ALU.add), reads=[Tp, T_bc], writes=[T_lg])
                    P.add("dve", lambda e, lgt=lgt, m8=m8: e.max(out=m8, in_=lgt), reads=[T_lg], writes=[T_m8])
                    P.add("dve", lambda e, sq=sq, m8=m8: e.tensor_scalar(out=sq[:, 3:4], in0=m8[:, 0:1], scalar1=-1.0, scalar2=None, op0=ALU.mult), reads=[T_m8], writes=[T_sq])
                    w4 = w4_all[:, ti, :]
                    P.add("act", lambda e, sq=sq, m8=m8, w4=w4: e.activation(out=w4, in_=m8[:, 0:4], func=AF.Exp, bias=sq[:, 3:4], accum_out=sq[:, 4:5]),
                          reads=[T_m8, T_sq], writes=[T_w4, T_sq])
                    P.add("dve", lambda e, sq=sq: e.reciprocal(out=sq[:, 5:6], in_=sq[:, 4:5]), reads=[T_sq], writes=[T_sq])
                    P.add("dve", lambda e, sq=sq, w4=w4: e.tensor_scalar(out=w4, in0=w4, scalar1=sq[:, 5:6], scalar2=None, op0=ALU.mult), reads=[T_w4, T_sq], writes=[T_w4])
                    mkt, T_mk = mk.next()
                    P.add("dve", lambda e, mkt=mkt, lgt=lgt, m8=m8: e.tensor_scalar(out=mkt[:], in0=lgt, scalar1=m8[:, 3:4], scalar2=None, op0=ALU.is_ge), reads=[T_lg, T_m8], writes=[T_mk])
                    pp, Tpp = PSF.next()
                    P.add("pe", lambda e, pp=pp, mkt=mkt: e.matmul(pp[:, 0:NE], lhsT=ltri[:], rhs=mkt[:], start=True, stop=True), reads=[T_const, T_mk], writes=[Tpp])
                    P.add("pe", lambda e, pp=pp, mkt=mkt: e.matmul(pp[:, NE:2 * NE], lhsT=ones[:], rhs=mkt[:], start=True, stop=True), reads=[T_const, T_mk], writes=[Tpp])
                    P.add("dve", lambda e, pp=pp, ti=ti: e.tensor_tensor(out=pos_all[:, ti, :], in0=pp[:, 0:NE], in1=cntbase[:], op=ALU.add), reads=[Tpp, T_cnt], writes=[T_pos])
                    P.add("dve", lambda e, pp=pp: e.tensor_tensor(out=cntbase[:], in0=pp[:, NE:2 * NE], in1=cntbase[:], op=ALU.add), reads=[Tpp, T_cnt], writes=[T_cnt])
        P.barrier()

    T_be = TT("be")

    def phase_D(l):
        lg_all, m8_all, pos_all, cntbase = RS["lg_all"], RS["m8_all"], RS["pos_all"], RS["cntbase"]
        dest_i, idx_w, idx_b, tokc = RS["dest_i"], RS["idx_w"], RS["idx_b"], RS["tokc"]
        with contextlib.ExitStack() as st:
            sb = mk_sb(st)
            T_d = TT("D")
            padded = sb([128, NE], F32, "padded")
            pend = sb([128, NE], F32, "pend")
            pstart = sb([128, NE], F32, "pstart")
            onesf = sb([128, NE], F32, "onesf")
            blk = sb([128, NBLK], F32, "blk")
            cmp2 = sb([128, NE, NST + 1], F32, "cmp2")
            cmp_ = sb([128, NBLK, NE], F32, "cmp")
            bef = sb([128, NBLK], F32, "bef")
            dp = sb([128, NE], F32, "dp")
            junk = sb([128, NE], F32, "junkD")
            dest_f = sb([128, NTILE * 4], F32, "dest_f")
            meta0 = sb([128, 2 * (NROWS // 128)], I32, "meta0")
            P.add("sp", lambda e: e.dma_start(out=blk[:], in_=c_blk.to_broadcast([128, NBLK])), writes=[T_d], dma=True)
            P.add("sp", lambda e: e.dma_start(out=tokc[:].rearrange("p a b -> p (a b)"), in_=c_tok), writes=[T_d], dma=True)
            P.add("sp", lambda e: e.dma_start(out=meta0[:], in_=c_meta0), writes=[T_d], dma=True)
            P.add("sp", lambda e: e.dma_start(out=meta_d.rearrange("(p j) c -> p (j c)", p=128), in_=meta0[:]), reads=[T_d], writes=[T_meta], dma=True)
            NJ = NST + 1
            P.add("dve", lambda e: e.tensor_tensor(out=cmp2[:], in0=cntbase[:].rearrange("p (n o) -> p n o", o=1).to_broadcast([128, NE, NJ]),
                                                   in1=blk[:, 0:NJ].rearrange("p (o n) -> p o n", o=1).to_broadcast([128, NE, NJ]), op=ALU.is_gt), reads=[T_cnt, T_d], writes=[T_d])
            P.add("dve", lambda e: e.tensor_reduce(out=padded[:], in_=cmp2[:], axis=AX.X, op=ALU.add), reads=[T_d], writes=[T_d])
            P.add("dve", lambda e: e.tensor_scalar(out=padded[:], in0=padded[:], scalar1=float(MOE_BLOCK), scalar2=None, op0=ALU.mult), reads=[T_d], writes=[T_d])
            P.add("dve", lambda e: e.memset(onesf[:], 1.0), writes=[T_d])
            P.add("dve", lambda e: e.tensor_tensor_scan(out=pend[:], data0=onesf[:], data1=padded[:], initial=0.0, op0=ALU.mult, op1=ALU.add), reads=[T_d], writes=[T_d])
            P.add("dve", lambda e: e.tensor_tensor(out=pstart[:], in0=pend[:], in1=padded[:], op=ALU.subtract), reads=[T_d], writes=[T_d])
            P.add("dve", lambda e: e.tensor_tensor(out=cmp_[:], in0=pend[:].rearrange("p (o n) -> p o n", o=1).to_broadcast([128, NBLK, NE]),
                                                   in1=blk[:].rearrange("p (n o) -> p n o", o=1).to_broadcast([128, NBLK, NE]), op=ALU.is_le), reads=[T_d], writes=[T_d])
            P.add("dve", lambda e: e.tensor_reduce(out=bef[:], in_=cmp_[:], axis=AX.X, op=ALU.add), reads=[T_d], writes=[T_d])
            P.add("dve", lambda e: e.tensor_scalar(out=bef[:], in0=bef[:], scalar1=float(NE - 1), scalar2=None, op0=ALU.min), reads=[T_d], writes=[T_d])
            pidx = sb([128, 1], F32, "pidx")
            P.add("sp", lambda e: e.dma_start(out=pidx[:], in_=c_pidx), writes=[T_d], dma=True)
            P.add("dve", lambda e: e.tensor_scalar(out=bef[:], in0=bef[:], scalar1=float(l * NE), scalar2=None, op0=ALU.add), reads=[T_d], writes=[T_d])
            P.add("dve", lambda e: e.tensor_copy(out=idx_b[:], in_=bef[:]), reads=[T_d], writes=[T_be])
            bw = sb([128, NBLK], F32, "bw")
            P.add("dve", lambda e: e.tensor_scalar(out=bw[:], in0=bef[:], scalar1=128.0, scalar2=pidx[:, 0:1], op0=ALU.mult, op1=ALU.add), reads=[T_d, T_be], writes=[T_d])
            P.add("dve", lambda e: e.tensor_copy(out=idx_w[:], in_=bw[:]), reads=[T_d], writes=[T_be])
            plc = sb([128, 8], F32, "plc")
            i8f = sb([128, NBLK, 8], F32, "i8f")
            P.add("sp", lambda e: e.dma_start(out=plc[:], in_=c_pl.to_broadcast([128, 8])), writes=[T_d], dma=True)
            P.add("dve", lambda e: e.tensor_scalar(out=bw[:], in0=bef[:], scalar1=1024.0, scalar2=pidx[:, 0:1], op0=ALU.mult, op1=ALU.add), reads=[T_d, T_be], writes=[T_d])
            P.add("dve", lambda e: e.tensor_tensor(out=i8f[:], in0=bw[:].rearrange("p (n o) -> p n o", o=1).to_broadcast([128, NBLK, 8]),
                                                   in1=plc[:].rearrange("p (o n) -> p o n", o=1).to_broadcast([128, NBLK, 8]), op=ALU.add), reads=[T_d], writes=[T_d])
            P.add("dve", lambda e: e.tensor_copy(out=RS["idx_w8"][:], in_=i8f[:]), reads=[T_d], writes=[T_be])
            for ti in range(NTILE):
                P.add("dve", lambda e, ti=ti: e.tensor_tensor(out=dp[:], in0=pos_all[:, ti, :], in1=pstart[:], op=ALU.add), reads=[T_pos, T_d], writes=[T_d])
                for k in range(4):
                    P.add("dve", lambda e, ti=ti, k=k: e.scalar_tensor_tensor(out=junk[:], in0=lg_all[:, ti, :], scalar=m8_all[:, ti, k:k + 1], in1=dp[:],
                                                                               op0=ALU.is_equal, op1=ALU.mult, accum_out=dest_f[:, ti * 4 + k:ti * 4 + k + 1]),
                          reads=[T_lg, T_m8, T_d], writes=[T_d])
            P.add("dve", lambda e: e.tensor_copy(out=dest_i[:], in_=dest_f[:]), reads=[T_d], writes=[T_dest])
            if cfg.debug:
                dbg = nc.dram_tensor("dbg_d%d" % l, [128, NTILE * 4 + 2 * NBLK], I32, kind="ExternalOutput").ap()
                dbgf = nc.dram_tensor("dbg_f%d" % l, [128, NE * 3], F32, kind="ExternalOutput").ap()
                T_dbg = TT("dbg")
                P.add("sp", lambda e: e.dma_start(out=dbg[:, 0:NTILE * 4], in_=dest_i[:]), reads=[T_dest], writes=[T_dbg], dma=True)
                P.add("sp", lambda e: e.dma_start(out=dbg[:, NTILE * 4:NTILE * 4 + NBLK], in_=idx_w[:]), reads=[T_be], writes=[T_dbg], dma=True)
                P.add("sp", lambda e: e.dma_start(out=dbg[:, NTILE * 4 + NBLK:], in_=idx_b[:]), reads=[T_be], writes=[T_dbg], dma=True)
                P.add("sp", lambda e: e.dma_start(out=dbgf[:, 0:NE], in_=cntbase[:]), reads=[T_cnt], writes=[T_dbg], dma=True)
                P.add("sp", lambda e: e.dma_start(out=dbgf[:, NE:2 * NE], in_=pend[:]), reads=[T_d], writes=[T_dbg], dma=True)
                P.add("sp", lambda e: e.dma_start(out=dbgf[:, 2 * NE:3 * NE], in_=pstart[:]), reads=[T_d], writes=[T_dbg], dma=True)
                if cfg.stop == "Dpre":
                    P.barrier()
                    return
            for ti in range(NTILE):
                for k in range(4):
                    P.add("pool", lambda e, ti=ti, k=k: e.indirect_dma_start(
                        out=meta_d, out_offset=bass.IndirectOffsetOnAxis(ap=dest_i[:, ti * 4 + k:ti * 4 + k + 1], axis=0),
                        in_=tokc[:, ti, :], in_offset=None), reads=[T_dest, T_d, T_meta], writes=[TT("sc")], dma=True)
        P.barrier()

    def phase_E(l):
        idx_w, idx_b, idx_w8 = RS["idx_w"], RS["idx_b"], RS["idx_w8"]
        with contextlib.ExitStack() as st:
            sb = mk_sb(st)
            WR = Ring([(sb([128, 8, D], BF16, "wexp"), [TT("wexp%d_%d" % (i, q)) for q in range(8)]) for i in range(6)])
            BG = Ring([(sb([128, 16], F32, "bgu"), TT("bgu%d" % i)) for i in range(2)])
            BD = Ring([(sb([128, D], F32, "bd"), TT("bd%d" % i)) for i in range(2)])
            MT = Ring([(sb([128, 4, 2], I32, "mt"), TT("mt%d" % i)) for i in range(2)])
            XG = Ring([(sb([128, 4, D], BF16, "xg"), TT("xg%d" % i)) for i in range(2)])
            XGT = Ring([(sb([128, 8, 512], BF16, "xgT"), TT("xgT%d" % i)) for i in range(2)])
            ACT_ = Ring([(sb([128, 8, 512], BF16, "actT"), TT("actT%d" % i)) for i in range(2)])
            TM = Ring([(sb([128, 512], F32, "tmE"), TT("tmE%d" % i)) for i in range(6)])
            YP = Ring([(sb([128, D], F32, "ypt"), TT("ypt%d" % i)) for i in range(2)])
            zt = sb([128, D], BF16, "zrow")
            T_z = TT("z")
            P.add("pool", lambda e: e.memset(zt[:], 0.0), writes=[T_z])
            P.add("sp", lambda e: e.dma_start(out=h2_d[NT:NT + 128, :], in_=zt[:]), reads=[T_z], writes=[T_h2], dma=True)

            def loads_x(j):
                mt, T_mt = MT.next()
                P.add("sp", lambda e: e.dma_start(out=mt[:], in_=meta_d[j * 512:(j + 1) * 512, :].rearrange("(jj p) c -> p jj c", p=128)), reads=[T_meta], writes=[T_mt], dma=True)
                xg, T_xg = XG.next()
                for jj in range(4):
                    P.add("pool", lambda e, jj=jj: e.indirect_dma_start(out=xg[:, jj, :], out_offset=None, in_=h2_d,
                                                                        in_offset=bass.IndirectOffsetOnAxis(ap=mt[:, jj, 0:1], axis=0)),
                          reads=[T_mt, T_h2], writes=[T_xg], dma=True)
                return dict(mt=(mt, T_mt), xg=(xg, T_xg))

            def loads_w(j, d):
                ws = []
                for wi, wsrc in enumerate((w_gate, w_up, w_down)):
                    wt, T_wt = WR.next()
                    src = wsrc.rearrange("l e k n -> (l e k) n")
                    for pl in range(8):
                        P.add("pool", lambda e, wt=wt, src=src, pl=pl: e.indirect_dma_start(
                            out=wt[:, pl, :], out_offset=None, in_=src, in_offset=bass.IndirectOffsetOnAxis(ap=idx_w8[:, j, pl:pl + 1], axis=0)),
                            reads=[T_be], writes=[T_wt[pl]], dma=True)
                    ws.append((wt, T_wt))
                bg, T_bg = BG.next()
                bd, T_bd = BD.next()
                P.add("pool", lambda e: e.indirect_dma_start(out=bg[:, 0:8], out_offset=None, in_=b_gateT,
                                                             in_offset=bass.IndirectOffsetOnAxis(ap=idx_w[:, j:j + 1], axis=0)), reads=[T_be], writes=[T_bg], dma=True)
                P.add("pool", lambda e: e.indirect_dma_start(out=bg[:, 8:16], out_offset=None, in_=b_upT,
                                                             in_offset=bass.IndirectOffsetOnAxis(ap=idx_w[:, j:j + 1], axis=0)), reads=[T_be], writes=[T_bg], dma=True)
                P.add("pool", lambda e: e.indirect_dma_start(out=bd[:], out_offset=None, in_=b_down,
                                                             in_offset=bass.IndirectOffsetOnAxis(ap=idx_b[:, j:j + 1], axis=0)), reads=[T_be], writes=[T_bd], dma=True)
                d.update(ws=ws, bg=(bg, T_bg), bd=(bd, T_bd))

            def stage_T(j, dd):
                xg, T_xg = dd["xg"]
                xgT, T_xgT = XGT.next()
                dd["xgT"] = (xgT, T_xgT)
                for kc in range(8):
                    pT, T_pT = PSB.next()
                    for jj in range(4):
                        P.add("pe", lambda e, pT=pT, jj=jj, kc=kc: e.transpose(out=pT[:, sl(jj)], in_=xg[:, jj, sl(kc)], identity=ident[:]), reads=[T_xg, T_const], writes=[T_pT])
                    P.add("act", lambda e, pT=pT, kc=kc: e.activation(out=xgT[:, kc, :], in_=pT[:, 0:512], func=AF.Copy), reads=[T_pT], writes=[T_xgT])

            def stage_GU(j, dd, fcs):
                (wg, T_wg), (wu, T_wu), (wd, T_wd) = dd["ws"]
                bg, T_bg = dd["bg"]
                xgT, T_xgT = dd["xgT"]
                if "actT" not in dd:
                    dd["actT"] = ACT_.next()
                actT, T_act = dd["actT"]
                for fc in fcs:
                    pg, Tpg = PSF.next()
                    pu, Tpu = PSF.next()
                    for kc in range(8):
                        P.add("pe", lambda e, pg=pg, kc=kc, fc=fc: e.matmul(pg[:], lhsT=wg[:, kc, sl(fc)], rhs=xgT[:, kc, :], start=(kc == 0), stop=(kc == 7)), reads=[T_wg[kc], T_xgT], writes=[Tpg])
                    for kc in range(8):
                        P.add("pe", lambda e, pu=pu, kc=kc, fc=fc: e.matmul(pu[:], lhsT=wu[:, kc, sl(fc)], rhs=xgT[:, kc, :], start=(kc == 0), stop=(kc == 7)), reads=[T_wu[kc], T_xgT], writes=[Tpu])
                    (gt, Tgt), (sg, Tsg), (up, Tup) = TM.next(), TM.next(), TM.next()
                    P.add("dve", lambda e, gt=gt, pg=pg, fc=fc: e.tensor_scalar(out=gt[:], in0=pg[:], scalar1=bg[:, fc:fc + 1], scalar2=7.0, op0=ALU.add, op1=ALU.min), reads=[Tpg, T_bg], writes=[Tgt])
                    P.add("act", lambda e, gt=gt, sg=sg: e.activation(out=sg[:], in_=gt[:], func=AF.Sigmoid, scale=1.702), reads=[Tgt], writes=[Tsg])
                    P.add("dve", lambda e, up=up, pu=pu, fc=fc: e.tensor_scalar(out=up[:], in0=pu[:], scalar1=bg[:, 8 + fc:9 + fc], scalar2=7.0, op0=ALU.add, op1=ALU.min), reads=[Tpu, T_bg], writes=[Tup])
                    P.add("dve", lambda e, up=up: e.tensor_scalar(out=up[:], in0=up[:], scalar1=-7.0, scalar2=1.0, op0=ALU.max, op1=ALU.add), reads=[Tup], writes=[Tup])
                    P.add("dve", lambda e, gt=gt, sg=sg: e.tensor_tensor(out=gt[:], in0=gt[:], in1=sg[:], op=ALU.mult), reads=[Tgt, Tsg], writes=[Tgt])
                    P.add("dve", lambda e, gt=gt, up=up, fc=fc: e.tensor_tensor(out=actT[:, fc, :], in0=gt[:], in1=up[:], op=ALU.mult), reads=[Tgt, Tup], writes=[T_act])

            def stage_DN(j, dd):
                (wg, T_wg), (wu, T_wu), (wd, T_wd) = dd["ws"]
                bd, T_bd = dd["bd"]
                actT, T_act = dd["actT"]
                for jj in range(4):
                    ypt, T_ypt = YP.next()
                    for nb in range(2):
                        ps, Tp = PSF.next()
                        for fc in range(8):
                            P.add("pe", lambda e, ps=ps, fc=fc, jj=jj, nb=nb: e.matmul(ps[:], lhsT=actT[:, fc, sl(jj)], rhs=wd[:, fc, sl(nb, 512)], start=(fc == 0), stop=(fc == 7)),
                                  reads=[T_wd[fc], T_act], writes=[Tp])
                        P.add("dve", lambda e, ypt=ypt, ps=ps, nb=nb: e.tensor_tensor(out=ypt[:, sl(nb, 512)], in0=ps[:], in1=bd[:, sl(nb, 512)], op=ALU.add), reads=[Tp, T_bd], writes=[T_ypt])
                    P.add("sp", lambda e, ypt=ypt, jj=jj: e.dma_start(out=yp_d[j * 512 + jj * 128:j * 512 + (jj + 1) * 128, :], in_=ypt[:]), reads=[T_ypt], writes=[TT()], dma=True)

            blk = {}

            def LX(j):
                if j < NBLK:
                    blk[j] = loads_x(j)

            def LW(j):
                if j < NBLK:
                    loads_w(j, blk[j])
            FA, FB = range(0, 4), range(4, 8)
            LX(0); LW(0); LX(1); LW(1)
            stage_T(0, blk[0])
            LX(2)
            stage_GU(0, blk[0], FA)
            if NBLK > 1:
                stage_T(1, blk[1])
            stage_GU(0, blk[0], FB)
            for j in range(NBLK):
                if j + 1 < NBLK:
                    stage_GU(j + 1, blk[j + 1], FA)
                if j + 2 < NBLK:
                    stage_T(j + 2, blk[j + 2])
                LX(j + 3)
                stage_DN(j, blk[j])
                LW(j + 2)
                if j + 1 < NBLK:
                    stage_GU(j + 1, blk[j + 1], FB)
                del blk[j]
        P.barrier()

    def phase_F(l, last):
        dest_i, w4_all = RS["dest_i"], RS["w4_all"]
        with contextlib.ExitStack() as st:
            sb = mk_sb(st)
            T_bc = TT("bcF")
            g2s = [sb([128, D], F32, "g2") for _ in range(2)]
            T_rows = [TT("rowsF%d" % k) for k in range(2)]
            fg = load_bc(sb, final_g[0:1, :], D, T_bc, name="fg") if last else None

            def load_rows(k, r):
                P.add("sp", lambda e: e.dma_start(out=g2s[k][:], in_=mod_d[r:r + 1, 5 * D:6 * D].to_broadcast([128, D])), reads=[T_mod], writes=[T_rows[k]], dma=True)
            load_rows(0, 2)
            cur_s = [-1]
            YK = Ring([tuple((sb([128, D], F32, "yk"), TT("yk%d_%d" % (i, q))) for q in range(4)) for i in range(4)])
            XT = Ring([(sb([128, D], F32, "xtF"), TT("xtF%d" % i)) for i in range(4)])
            junk = sb([128, D], BF16, "junkF")
            T_junk = TT("junkF")
            stat = Ring([(sb([128, 4], F32, "statF"), TT("statF%d" % i)) for i in range(3)])
            tlist = [ti for ti in range(NTILE) if not (last and cfg.tile_r(ti) == 2)]

            def loads(ti):
                yk = YK.next()
                for q in range(4):
                    P.add("pool", lambda e, q=q, yk=yk, ti=ti: e.indirect_dma_start(out=yk[q][0][:], out_offset=None, in_=yp_d,
                                                                                  in_offset=bass.IndirectOffsetOnAxis(ap=dest_i[:, ti * 4 + q:ti * 4 + q + 1], axis=0)),
                          reads=[T_dest, T_yp], writes=[yk[q][1]], dma=True)
                xt, T_xt = XT.next()
                P.add("sp", lambda e, xt=xt, ti=ti: e.dma_start(out=xt[:], in_=xres[sl(ti), :]), reads=[T_xres[ti]], writes=[T_xt], dma=True)
                return yk, xt, T_xt
            AHEAD = 2
            pend = [loads(ti) for ti in tlist[:AHEAD]]
            for n, ti in enumerate(tlist):
                if n + AHEAD < len(tlist):
                    pend.append(loads(tlist[n + AHEAD]))
                yk, xt, T_xt = pend.pop(0)
                r = cfg.tile_r(ti)
                if r == 2:
                    k = 0
                else:
                    k = 1
                    if cur_s[0] != r:
                        load_rows(1, r)
                        cur_s[0] = r
                P.add("act", lambda e, yk=yk, ti=ti: e.activation(out=yk[0][0][:], in_=yk[0][0][:], func=AF.Copy, scale=w4_all[:, ti, 0:1]),
                      reads=[yk[0][1], T_w4], writes=[yk[0][1]])
                for q in (1, 2, 3):
                    P.add("dve", lambda e, yk=yk, ti=ti, q=q: e.scalar_tensor_tensor(out=yk[0][0][:], in0=yk[q][0][:], scalar=w4_all[:, ti, q:q + 1], in1=yk[0][0][:],
                                                                                    op0=ALU.mult, op1=ALU.add), reads=[yk[0][1], yk[q][1], T_w4], writes=[yk[0][1]])
                P.add("dve", lambda e, yk=yk, k=k: e.tensor_tensor(out=yk[0][0][:], in0=yk[0][0][:], in1=g2s[k][:], op=ALU.mult), reads=[yk[0][1], T_rows[k]], writes=[yk[0][1]])
                P.add("dve", lambda e, yk=yk, xt=xt: e.tensor_tensor(out=xt[:], in0=xt[:], in1=yk[0][0][:], op=ALU.add), reads=[yk[0][1], T_xt], writes=[T_xt])
                if not last:
                    P.add("sp", lambda e, xt=xt, ti=ti: e.dma_start(out=xres[sl(ti), :], in_=xt[:]), reads=[T_xt], writes=[T_xres[ti]], dma=True)
                else:
                    sq, T_sq = stat.next()
                    P.add("act", lambda e, xt=xt, sq=sq: e.activation(out=junk[:], in_=xt[:], func=AF.Square, accum_out=sq[:, 0:1]), reads=[T_xt], writes=[T_junk, T_sq])
                    P.add("act", lambda e, sq=sq: e.activation(out=sq[:, 1:2], in_=sq[:, 0:1], func=AF.Sqrt, scale=1.0 / D, bias=EPS), reads=[T_sq], writes=[T_sq])
                    P.add("dve", lambda e, sq=sq: e.reciprocal(out=sq[:, 2:3], in_=sq[:, 1:2]), reads=[T_sq], writes=[T_sq])
                    P.add("act", lambda e, xt=xt, sq=sq: e.activation(out=xt[:], in_=xt[:], func=AF.Copy, scale=sq[:, 2:3]), reads=[T_xt, T_sq], writes=[T_xt])
                    P.add("pool", lambda e, xt=xt: e.tensor_tensor(out=xt[:], in0=xt[:], in1=fg[:], op=ALU.mult), reads=[T_xt, T_bc], writes=[T_xt])
                    s = (ti * 128) // TS
                    orow = s * TL + (ti * 128 - s * TS - TC)
                    P.add("sp", lambda e, xt=xt, orow=orow: e.dma_start(out=out[orow:orow + 128, :], in_=xt[:]), reads=[T_xt], writes=[TT()], dma=True)
        P.barrier()

    P.barrier()
    stop = cfg.stop
    for l in range(L):
        P.phase = "mod"
        phase_mod(l)
        P.phase = "A"
        phase_A(l)
        if stop == "A":
            break
        P.phase = "B"
        phase_B(l)
        if stop == "B":
            break
        P.phase = "B2"
        phase_B2(l)
        if stop == "B2":
            break
        with contextlib.ExitStack() as rst:
            alloc_routing(rst)
            P.phase = "C"
            phase_C(l)
            if stop == "C":
                break
            P.phase = "D"
            phase_D(l)
            if stop in ("D", "Dpre"):
                break
            P.phase = "E"
            phase_E(l)
            if stop == "E":
                break
            P.phase = "F"
            phase_F(l, l == L - 1)
    P.add("sp", lambda e: e.nop(), reads=[T_meta, T_h2, T_mod] + T_xres)
    P.emit()
    top.close()
    return nc, P


def host_consts(cfg):
    bf = ml_dtypes.bfloat16
    ident = np.eye(128, dtype=np.float32).astype(bf)
    ltri = np.triu(np.ones((128, 128), np.float32), 1).astype(bf)
    ones = np.ones((128, 128), np.float32).astype(bf)

    def inv_counts(T, k):
        pos = np.arange(T)
        lo = np.clip(pos - k // 2, 0, T)
        hi = np.clip(pos + (k - k // 2), 0, T)
        return (1.0 / (hi - lo)).astype(np.float32)
    pinv = np.zeros((4, 128 + cfg.TC), np.float32)
    for g, k in enumerate(POOL_K):
        pinv[g, 0:64] = inv_counts(GRID_W, k)
        pinv[g, 64:128] = 0
        ir = inv_counts(cfg.R, k)
        pinv[g, 64:64 + min(64, cfg.R)] = ir[:64]
        pinv[g, 128:] = inv_counts(cfg.TC, k)
    blk = (np.arange(cfg.NBLK, dtype=np.float32) * MOE_BLOCK).reshape(1, -1)
    tok = np.zeros((128, cfg.NTILE, 2), np.int32)
    tok[:, :, 0] = np.arange(cfg.NTILE, dtype=np.int32)[None, :] * 128 + np.arange(128, dtype=np.int32)[:, None]
    tok = tok.reshape(128, -1)
    meta0 = np.zeros((128, cfg.NROWS // 128, 2), np.int32)
    meta0[:, :, 0] = cfg.NT
    return dict(c_pidx=np.arange(128, dtype=np.float32).reshape(128, 1), c_pl=(128.0 * np.arange(8, dtype=np.float32)).reshape(1, 8), c_ident=ident, c_ltri=ltri, c_ones=ones, c_pinv=pinv, c_blk=blk, c_tok=tok,
                c_meta0=meta0.reshape(128, -1))


def host_params(inp, L):
    f = np.float32
    g = lambda k: np.asarray(inp[k], dtype=f)
    d = {}
    d["ada_w"] = g("ada_w")
    d["ada_b"] = g("ada_b")
    d["n1gT"] = np.ascontiguousarray(g("norm1_g").reshape(L, 8, 128).transpose(0, 2, 1))
    d["n2g"] = g("norm2_g")
    d["final_g"] = g("final_g").reshape(1, D)
    d["w_in"] = g("w_in")
    d["b_inT"] = np.ascontiguousarray(g("b_in").reshape(L, 52, 128).transpose(0, 2, 1))
    d["b_in"] = g("b_in")
    cw = g("conv_w").reshape(L, 4, 8, 128)
    cb = g("conv_b").reshape(L, 1, 8, 128)
    d["convT"] = np.ascontiguousarray(np.concatenate([cw, cb], axis=1).transpose(0, 3, 2, 1)).reshape(L, 128, 40)
    lr = np.stack([g("lru_ba"), g("lru_bx"), g("lru_lambda")], axis=-1).reshape(L, 2, 8, 128, 3)
    d["lruT"] = np.ascontiguousarray(lr.transpose(0, 3, 1, 2, 4)).reshape(L, 128, 48)
    d["lru_wa"] = g("lru_wa")
    d["lru_wx"] = g("lru_wx")
    for k in ("out_a", "out_b", "out_c", "w_o", "b_o", "pool_w", "router_w", "router_b", "w_gate", "w_up", "w_down", "b_down"):
        d[k] = g(k)
    pt = np.stack([g("pool_b"), g("pool_scale")], axis=-1).reshape(L, 4, 128, 2)
    d["poolT"] = np.ascontiguousarray(pt.transpose(0, 2, 1, 3)).reshape(L, 128, 8)
    d["sg_ln"] = np.stack([g("sg_ln_g"), g("sg_ln_b")], axis=1)
    d["sg_wT"] = np.ascontiguousarray(g("sg_w").transpose(0, 1, 3, 2))
    d["sg_b"] = g("sg_b").reshape(L, 512)
    d["b_down"] = g("b_down").reshape(L * NE, D)
    d["b_gateT"] = np.ascontiguousarray(g("b_gate").reshape(L, NE, 8, 128).transpose(0, 1, 3, 2)).reshape(L * NE * 128, 8)
    d["b_upT"] = np.ascontiguousarray(g("b_up").reshape(L, NE, 8, 128).transpose(0, 1, 3, 2)).reshape(L * NE * 128, 8)
    return d


def core_inputs(inp, cfg, core):
    f = np.float32
    x = np.asarray(inp["x"], dtype=f)
    ctx = np.asarray(inp["ctx"], dtype=f)
    c = np.asarray(inp["c"], dtype=f)
    cc = np.asarray(inp["c_ctx"], dtype=f)
    rows = []
    for s in range(cfg.NS):
        b = core * cfg.NS + s
        rows.append(ctx[b])
        rows.append(x[b])
    xin = np.ascontiguousarray(np.concatenate(rows, axis=0))
    cv = np.stack([c[core * cfg.NS + s] for s in range(cfg.NS)] + [cc], axis=0)
    cT = np.ascontiguousarray(cv.reshape(3, 8, 128).transpose(2, 1, 0)).reshape(128, 24)
    return dict(xin=xin, cT=cT)


_CACHE = {}


def kernel(**inputs):
    cfg = Cfg()
    n_cores = 8
    if "nc" not in _CACHE:
        _CACHE["nc"] = build(cfg)[0]
    nc = _CACHE["nc"]
    shared = host_params(inputs, cfg.L)
    shared.update(host_consts(cfg))
    in_maps = []
    for core in range(n_cores):
        m = dict(shared)
        m.update(core_inputs(inputs, cfg, core))
        in_maps.append(m)
    res = run_bass_kernel_spmd(nc, in_maps, core_ids=list(range(n_cores)))
    outs = [np.asarray(r["out"]).reshape(cfg.NS, cfg.TL, D) for r in res.results]
    return np.concatenate(outs, axis=0).astype(np.float32)
```

```python
import contextlib
import numpy as np
import ml_dtypes
import concourse.bass as bass
import concourse.mybir as mybir
from concourse.bass_utils import run_bass_kernel_spmd

F32 = mybir.dt.float32
BF16 = mybir.dt.bfloat16
I32 = mybir.dt.int32
AF = mybir.ActivationFunctionType
ALU = mybir.AluOpType
AX = mybir.AxisListType

D = 1024
NE = 32
EPS = 1e-6
POOL_K = (2, 4, 8, 16)
GRID_W = 64
MOE_BLOCK = 512


class TT:
    __slots__ = ("name", "w", "r", "rd")

    def __init__(self, name=""):
        self.name = name
        self.w = None
        self.r = {}
        self.rd = []


class Prog:
    ENGS = ("pe", "act", "dve", "pool", "sp")
    NDMASEM = 24

    profile = False

    def __init__(self, nc):
        self.nc = nc
        self.ops = []

    def add(self, eng, fn, reads=(), writes=(), dma=False):
        i = len(self.ops)
        deps = {}

        def dep(j, kind):
            if j is None:
                return
            if deps.get(j) != "raw":
                deps[j] = kind

        for t in reads:
            dep(t.w, "raw")
        for t in writes:
            dep(t.w, "waw")
            for j in t.r.values():
                dep(j, "war")
            for j in t.rd:
                dep(j, "war")
        for t in reads:
            if dma:
                t.rd.append(i)
            else:
                t.r[eng] = i
        for t in writes:
            t.w = i
            t.r = {}
            t.rd = []
        self.ops.append(dict(eng=eng, fn=fn, deps=deps, dma=dma, sig=False, ph=getattr(self, "phase", None)))
        return i

    def barrier(self):
        bt = [TT("bar_" + e) for e in self.ENGS]
        pend = TT("bar_dma")
        pend.rd = [i for i, op in enumerate(self.ops) if op["dma"] and i >= getattr(self, "_bar_from", 0)]
        self.add("sp", lambda e: e.nop(), writes=[pend, bt[4]])
        for k, e in enumerate(self.ENGS[:4]):
            self.add(e, self.bar_fn[e], writes=[bt[k]] + self.bar_tt.get(e, []))
        for k, e in enumerate(self.ENGS):
            self.add(e, (lambda ee: ee.nop()), reads=bt)
        self._bar_from = len(self.ops)

    def emit(self):
        nc = self.nc
        ops = self.ops
        for i, op in enumerate(ops):
            need = []
            for j, kind in op["deps"].items():
                pj = ops[j]
                if pj["dma"]:
                    need.append(j)
                    continue
                if pj["eng"] == op["eng"] and not op["dma"]:
                    if op["eng"] == "pe":
                        continue
                    if kind != "raw":
                        continue
                need.append(j)
                pj["sig"] = True
            op["need"] = need
        st = contextlib.ExitStack()
        esem = {e: st.enter_context(nc.semaphore("S_" + e)) for e in self.ENGS}
        dsem = {e: [st.enter_context(nc.semaphore("D_%s%d" % (e, k))) for k in range(self.NDMASEM)]
                for e in ("sp", "act", "pool")}
        ecount = {e: 0 for e in self.ENGS}
        dcount = {e: [0] * self.NDMASEM for e in dsem}
        dnext = {e: 0 for e in dsem}
        for op in ops:
            e = op["eng"]
            if op["dma"]:
                k = dnext[e] % self.NDMASEM
                dnext[e] += 1
                op["prev"] = (dsem[e][k], dcount[e][k])
                dcount[e][k] += 16
                op["done"] = (dsem[e][k], dcount[e][k])
            elif op["sig"]:
                ecount[e] += 1
                op["done"] = (esem[e], ecount[e])
        self.stats = dict(n_ops=len(ops), sig=dict(ecount), ndma=dict(dnext))
        block = st.enter_context(nc.Block())

        def run(ename):
            def body(eng):
                waited = {}
                for op in ops:
                    if op["eng"] != ename:
                        continue
                    ws = []
                    if op["dma"] and op["prev"][1] > 0:
                        ws.append(op["prev"])
                    for j in op["need"]:
                        ws.append(ops[j]["done"])
                    best = {}
                    for s, c in ws:
                        if c > best.get(s.name, (None, 0))[1]:
                            best[s.name] = (s, c)
                    for s, c in best.values():
                        if waited.get(s.name, 0) >= c:
                            continue
                        eng.wait_ge(s, c)
                        waited[s.name] = c
                    if self.profile and op["ph"]:
                        with nc.named_scope(op["ph"]):
                            ins = op["fn"](eng)
                    else:
                        ins = op["fn"](eng)
                    if op["dma"]:
                        ins.then_inc(op["done"][0], 16)
                    elif op["sig"]:
                        ins.then_inc(op["done"][0], 1)
            return body

        block.tensor(run("pe"))
        block.scalar(run("act"))
        block.vector(run("dve"))
        block.gpsimd(run("pool"))
        block.sync(run("sp"))
        st.close()


class Ring:
    def __init__(self, items):
        self.items = items
        self.i = 0

    def next(self):
        it = self.items[self.i % len(self.items)]
        self.i += 1
        return it


class Cfg:
    def __init__(self, NS=2, TC=256, TL=4096, L=4, debug=False, stop=None):
        self.NS, self.TC, self.TL, self.L, self.debug, self.stop = NS, TC, TL, L, debug, stop
        self.TS = TC + TL
        self.NT = NS * self.TS
        assert self.NT % 512 == 0 and TC % 128 == 0 and TL % 128 == 0
        self.NTILE = self.NT // 128
        self.NST = self.NT // 512
        self.R = TL // GRID_W
        self.SEG = 1024 if TL % 1024 == 0 else TL
        self.NBLK = (self.NT * 4 + NE * (MOE_BLOCK - 1) + MOE_BLOCK - 1) // MOE_BLOCK
        self.NROWS = self.NBLK * MOE_BLOCK

    def tile_r(self, ti):
        s = (ti * 128) // self.TS
        off = ti * 128 - s * self.TS
        return 2 if off < self.TC else s


OFF_GA, OFF_B, OFF_U, OFF_V, OFF_G, IN_COLS = 1024, 2048, 2560, 3072, 3584, 6656


def build(cfg):
    nc = bass.Bass("TRN2", target_bir_lowering=False)
    P = Prog(nc)
    NS, TC, TL, L, TS, NT, NTILE, NST = cfg.NS, cfg.TC, cfg.TL, cfg.L, cfg.TS, cfg.NT, cfg.NTILE, cfg.NST
    NBLK, NROWS, R, SEG = cfg.NBLK, cfg.NROWS, cfg.R, cfg.SEG

    def din(name, shape, dt=F32):
        return nc.dram_tensor(name, list(shape), dt, kind="ExternalInput").ap()

    def dscr(name, shape, dt):
        kind = "ExternalOutput" if cfg.debug else "Internal"
        return nc.dram_tensor(name, list(shape), dt, kind=kind).ap()

    xin = din("xin", [NT, D])
    cT = din("cT", [128, 8 * 3])
    ada_w = din("ada_w", [L, D, 6 * D])
    ada_b = din("ada_b", [L, 6 * D])
    n1gT = din("n1gT", [L, 128, 8])
    n2g = din("n2g", [L, D])
    final_g = din("final_g", [1, D])
    w_in = din("w_in", [L, D, IN_COLS])
    b_inT = din("b_inT", [L, 128, 52])
    b_in = din("b_in", [L, IN_COLS])
    convT = din("convT", [L, 128, 8 * 5])
    lruT = din("lruT", [L, 128, 2 * 8 * 3])
    lru_wa = din("lru_wa", [L, 2, 8, 128, 128])
    lru_wx = din("lru_wx", [L, 2, 8, 128, 128])
    out_a = din("out_a", [L, D, D])
    out_b = din("out_b", [L, 512, D])
    out_c = din("out_c", [L, 512, D])
    w_o = din("w_o", [L, D, D])
    b_o = din("b_o", [L, D])
    pool_w = din("pool_w", [L, 4, 128, 128])
    poolT = din("poolT", [L, 128, 8])
    sg_ln = din("sg_ln", [L, 2, 512])
    sg_wT = din("sg_wT", [L, 4, 128, 128])
    sg_b = din("sg_b", [L, 512])
    router_w = din("router_w", [L, D, NE])
    router_b = din("router_b", [L, NE])
    w_gate = din("w_gate", [L, NE, D, D])
    w_up = din("w_up", [L, NE, D, D])
    w_down = din("w_down", [L, NE, D, D])
    b_gateT = din("b_gateT", [L * NE * 128, 8])
    b_upT = din("b_upT", [L * NE * 128, 8])
    b_down = din("b_down", [L * NE, D])
    c_ident = din("c_ident", [128, 128], BF16)
    c_ltri = din("c_ltri", [128, 128], BF16)
    c_ones = din("c_ones", [128, 128], BF16)
    c_pinv = din("c_pinv", [4, 2 * 64 + TC])
    c_pidx = din("c_pidx", [128, 1])
    c_pl = din("c_pl", [1, 8])
    c_blk = din("c_blk", [1, NBLK])
    c_tok = din("c_tok", [128, NTILE * 2], I32)
    c_meta0 = din("c_meta0", [128, 2 * (NROWS // 128)], I32)
    out = nc.dram_tensor("out", [NS * TL, D], F32, kind="ExternalOutput").ap()

    xres = dscr("xres", [NT, D], F32)
    mod_d = dscr("mod_d", [3, 6 * D], F32)
    xaT_d = dscr("xaT_d", [D, NT], F32)
    gaT_d = dscr("gaT_d", [D, NT], BF16)
    zbT_d = dscr("zbT_d", [512, NT], F32)
    ucT_d = dscr("ucT_d", [512, NT], BF16)
    gT_d = dscr("gT_d", [3 * D, NT], BF16)
    uaT_d = dscr("uaT_d", [D, NT], BF16)
    pmT_d = dscr("pmT_d", [512, NT], BF16)
    h2_d = dscr("h2_d", [NT + 128, D], BF16)
    meta_d = dscr("meta_d", [NROWS, 2], I32)
    yp_d = dscr("yp_d", [NROWS, D], F32)
    T_xres_all, T_mod, T_xaT, T_gaT, T_zbT, T_ucT, T_gT, T_uaT, T_pmT, T_h2, T_meta, T_yp, T_out = [
        TT(n) for n in "xres mod xaT gaT zbT ucT gT uaT pmT h2 meta yp out".split()]

    T_xres = [TT("xres%d" % i) for i in range(NTILE)]
    top = contextlib.ExitStack()

    def mk_sb(stack):
        cnt = [0]

        def sb(shape, dt, name=None):
            cnt[0] += 1
            return stack.enter_context(nc.sbuf_tensor("%s_%d_%d" % (name or "t", id(stack) % 10007, cnt[0]), list(shape), dt))
        return sb

    sbp = mk_sb(top)
    psf = [top.enter_context(nc.psum_tensor("psf%d" % i, [128, 512], F32)) for i in range(6)]
    psb = [top.enter_context(nc.psum_tensor("psb%d" % i, [128, 1024], BF16)) for i in range(2)]
    PSF = Ring([(psf[i], TT("psf%d" % i)) for i in range(6)])
    PSB = Ring([(psb[i], TT("psb%d" % i)) for i in range(2)])

    bscr = sbp([128, 8], F32, "bscr")
    ident = sbp([128, 128], BF16, "ident")
    ltri = sbp([128, 128], BF16, "ltri")
    ones = sbp([128, 128], BF16, "ones")
    T_const = TT("const")
    P.bar_fn = {
        "pe": lambda e: e.matmul(psf[0][0:1, 0:1], lhsT=ident[:, 0:1], rhs=ident[:, 0:1], start=True, stop=True),
        "act": lambda e: e.activation(out=bscr[0:1, 0:1], in_=bscr[0:1, 1:2], func=AF.Copy),
        "dve": lambda e: e.memset(bscr[0:1, 2:3], 0.0),
        "pool": lambda e: e.memset(bscr[0:1, 4:5], 0.0),
    }
    P.bar_tt = {"pe": [PSF.items[0][1]]}
    P.add("dve", lambda e: e.memset(bscr[:], 0.0), writes=[TT("bscr")])
    siluT = sbp([128, 8, 3], F32, "siluT")
    T_silu = TT("silu")
    for tl, src in ((ident, c_ident), (ltri, c_ltri), (ones, c_ones)):
        P.add("sp", lambda e, tl=tl, src=src: e.dma_start(out=tl[:], in_=src), writes=[T_const], dma=True)
    P.add("sp", lambda e: e.dma_start(out=siluT[:].rearrange("p a b -> p (a b)"), in_=cT), writes=[T_silu], dma=True)
    P.add("act", lambda e: e.activation(out=siluT[:], in_=siluT[:], func=AF.Silu), reads=[T_silu], writes=[T_silu])
    for i in range(NST):
        P.add("sp", lambda e, i=i: e.dma_start(out=xres[i * 512:(i + 1) * 512, :], in_=xin[i * 512:(i + 1) * 512, :]),
              writes=T_xres[4 * i:4 * i + 4], dma=True)

    def sl(i, n=128):
        return slice(i * n, (i + 1) * n)

    def phase_mod(l):
        with contextlib.ExitStack() as st:
            sb = mk_sb(st)
            wr = Ring([(sb([128, 8, 512], F32, "adaw"), TT("adaw%d" % i)) for i in range(2)])
            modrow = sb([3, 6 * D], F32, "modrow")
            adab = sb([3, 6 * D], F32, "adab")
            T_modrow, T_adab = TT("modrow"), TT("adab")
            P.add("sp", lambda e: e.dma_start(out=adab[:], in_=ada_b[l:l + 1, :].to_broadcast([3, 6 * D])),
                  writes=[T_adab], dma=True)
            for cb in range(12):
                wt, Tw = wr.next()
                P.add("sp", lambda e, wt=wt, cb=cb: e.dma_start(
                    out=wt[:], in_=ada_w[l].rearrange("(kc p) n -> p kc n", p=128)[:, :, sl(cb, 512)]), writes=[Tw], dma=True)
                ps, Tp = PSF.next()
                for kc in range(8):
                    P.add("pe", lambda e, ps=ps, wt=wt, kc=kc: e.matmul(ps[0:3, :], lhsT=siluT[:, kc, :], rhs=wt[:, kc, :],
                                                                      start=(kc == 0), stop=(kc == 7)),
                          reads=[T_silu, Tw], writes=[Tp])
                P.add("dve", lambda e, ps=ps, cb=cb: e.tensor_tensor(out=modrow[:, sl(cb, 512)], in0=ps[0:3, :], in1=adab[:, sl(cb, 512)], op=ALU.add),
                      reads=[Tp, T_adab], writes=[T_modrow])
            P.add("sp", lambda e: e.dma_start(out=mod_d, in_=modrow[:]), reads=[T_modrow], writes=[T_mod], dma=True)
        P.barrier()

    def phase_A(l):
        with contextlib.ExitStack() as st:
            sb = mk_sb(st)
            win = sb([128, 8, IN_COLS], BF16, "win")
            T_win = [TT("win%d" % i) for i in range(13)]
            for cb in range(13):
                P.add("pool", lambda e, cb=cb: e.dma_start(out=win[:, :, sl(cb, 512)],
                                                            in_=w_in[l].rearrange("(kc p) n -> p kc n", p=128)[:, :, sl(cb, 512)]),
                      writes=[T_win[cb]], dma=True)

            def Twin(c0, c1):
                return T_win[c0 // 512:(c1 - 1) // 512 + 1]
            binT = sb([128, 52], F32, "binT")
            bv_bc = sb([128, 512], F32, "bv")
            lng = sb([128, 512], F32, "lng")
            lnb = sb([128, 512], F32, "lnb")
            sgb = sb([128, 512], F32, "sgb")
            sgw = sb([128, 4, 128], BF16, "sgw")
            n1g = sb([128, 8], F32, "n1g")
            modT = sb([128, 16, 3], F32, "modT")
            gm1 = sb([128, 8, 3], F32, "gm1")
            T_par, T_modT, T_gm1 = TT("parA"), TT("modT"), TT("gm1")
            P.add("sp", lambda e: e.dma_start(out=binT[:], in_=b_inT[l]), writes=[T_par], dma=True)
            P.add("sp", lambda e: e.dma_start(out=bv_bc[:], in_=b_in[l:l + 1, OFF_V:OFF_G].to_broadcast([128, 512])), writes=[T_par], dma=True)
            P.add("sp", lambda e: e.dma_start(out=lng[:], in_=sg_ln[l, 0:1, :].to_broadcast([128, 512])), writes=[T_par], dma=True)
            P.add("sp", lambda e: e.dma_start(out=lnb[:], in_=sg_ln[l, 1:2, :].to_broadcast([128, 512])), writes=[T_par], dma=True)
            P.add("sp", lambda e: e.dma_start(out=sgb[:], in_=sg_b[l:l + 1, :].to_broadcast([128, 512])), writes=[T_par], dma=True)
            P.add("pool", lambda e: e.dma_start(out=sgw[:], in_=sg_wT[l].rearrange("g q p -> q g p")), writes=[T_par], dma=True)
            P.add("sp", lambda e: e.dma_start(out=n1g[:], in_=n1gT[l]), writes=[T_par], dma=True)
            for r in range(3):
                P.add("sp", lambda e, r=r: e.dma_start(out=modT[:, :, r:r + 1], in_=mod_d[r, 0:2048].rearrange("(c p o) -> p c o", p=128, o=1),
                                                       allow_slow_non_contiguous=True),
                      reads=[T_mod], writes=[T_modT], dma=True)
            for r in range(3):
                P.add("dve", lambda e, r=r: e.scalar_tensor_tensor(out=gm1[:, :, r], in0=modT[:, 8:16, r], scalar=1.0, in1=n1g[:],
                                                                  op0=ALU.add, op1=ALU.mult), reads=[T_modT, T_par], writes=[T_gm1])
            XT = Ring([(sb([128, D], F32, "xt"), TT("xt%d" % i)) for i in range(4)])
            XN = Ring([(sb([128, D], BF16, "xn"), TT("xn%d" % i)) for i in range(4)])
            HT = Ring([(sb([128, 8, 512], BF16, "hT"), TT("hT%d" % i)) for i in range(2)])
            junk = sb([128, D], BF16, "junk")
            T_junk = TT("junk")
            stat = Ring([(sb([128, 4], F32, "stat"), TT("stat%d" % i)) for i in range(4)])
            SF = Ring([(sb([128, 512], F32, "sf"), TT("sf%d" % i)) for i in range(4)])
            SH = Ring([(sb([128, 512], BF16, "sh"), TT("sh%d" % i)) for i in range(6)])
            UT = Ring([(sb([128, 4, 512], BF16, "uT"), TT("uT%d" % i)) for i in range(1)])
            UC = Ring([(sb([128, 4, 512], BF16, "ucT"), TT("ucT%d" % i)) for i in range(1)])
            VG = Ring([(sb([128, 512], F32, "vg"), TT("vg%d" % i)) for i in range(2)])
            VN = Ring([(sb([128, 512], BF16, "vn"), TT("vn%d" % i)) for i in range(2)])
            STMP = Ring([(sb([128, 512], F32, "stmp"), TT("stmp%d" % i)) for i in range(2)])
            bst = Ring([(sb([128, 8], F32, "bst"), TT("bst%d" % i)) for i in range(2)])

            def norm_part(sti):
                tiles = list(range(4 * sti, 4 * sti + 4))
                hT, T_hT = HT.next()
                xns = []
                for j, ti in enumerate(tiles):
                    xt, T_xt = XT.next()
                    sq, T_sq = stat.next()
                    xn, T_xn = XN.next()
                    xns.append((xn, T_xn))
                    P.add("sp", lambda e, xt=xt, ti=ti: e.dma_start(out=xt[:], in_=xres[sl(ti), :]), reads=[T_xres[ti]], writes=[T_xt], dma=True)
                    P.add("act", lambda e, xt=xt, sq=sq: e.activation(out=junk[:], in_=xt[:], func=AF.Square, accum_out=sq[:, 0:1]),
                          reads=[T_xt], writes=[T_junk, T_sq])
                    P.add("act", lambda e, sq=sq: e.activation(out=sq[:, 1:2], in_=sq[:, 0:1], func=AF.Sqrt, scale=1.0 / D, bias=EPS),
                          reads=[T_sq], writes=[T_sq])
                    P.add("dve", lambda e, sq=sq: e.reciprocal(out=sq[:, 2:3], in_=sq[:, 1:2]), reads=[T_sq], writes=[T_sq])
                    P.add("dve", lambda e, xt=xt, xn=xn, sq=sq: e.tensor_scalar(out=xn[:], in0=xt[:], scalar1=sq[:, 2:3], scalar2=None, op0=ALU.mult),
                          reads=[T_xt, T_sq], writes=[T_xn])
                groups = []
                for j, ti in enumerate(tiles):
                    r = cfg.tile_r(ti)
                    if groups and groups[-1][2] == r:
                        groups[-1][1] = j + 1
                    else:
                        groups.append([j, j + 1, r])
                for kc in range(8):
                    pT, T_pT = PSB.next()
                    for j in range(4):
                        xn, T_xn = xns[j]
                        P.add("pe", lambda e, pT=pT, xn=xn, j=j, kc=kc: e.transpose(out=pT[:, sl(j)], in_=xn[:, sl(kc)], identity=ident[:]),
                              reads=[T_xn, T_const], writes=[T_pT])
                    for (j0, j1, r) in groups:
                        P.add("act", lambda e, pT=pT, hT=hT, kc=kc, j0=j0, j1=j1, r=r: e.activation(
                            out=hT[:, kc, j0 * 128:j1 * 128], in_=pT[:, j0 * 128:j1 * 128], func=AF.Identity,
                            scale=gm1[:, kc, r:r + 1], bias=modT[:, kc, r:r + 1]), reads=[T_pT, T_gm1, T_modT], writes=[T_hT])
                return hT, T_hT

            nxt_h = norm_part(0)
            for sti in range(NST):
                hT, T_hT = nxt_h
                nxt_h = None
                tok = slice(sti * 512, (sti + 1) * 512)
                uT, T_uT = UT.next()
                fm = []
                for c in range(8):
                    fm.append(("xa", c, c * 128))
                for c in range(4):
                    fm.append(("zb", c, OFF_B + c * 128))
                for c in range(8):
                    fm.append(("ga", c, OFF_GA + c * 128))
                for c in range(4):
                    fm.append(("u", c, OFF_U + c * 128))
                for c in range(24):
                    fm.append(("g", c, OFF_G + c * 128))
                for fi, (kind, c, col) in enumerate(fm):
                    if fi == 20 and sti + 1 < NST:
                        nxt_h = norm_part(sti + 1)
                    ps, Tp = PSF.next()
                    for kc in range(8):
                        P.add("pe", lambda e, ps=ps, kc=kc, col=col, hT=hT: e.matmul(ps[:], lhsT=win[:, kc, col:col + 128], rhs=hT[:, kc, :],
                                                                                  start=(kc == 0), stop=(kc == 7)),
                              reads=[T_hT] + Twin(col, col + 128), writes=[Tp])
                    bcol = binT[:, col // 128:col // 128 + 1]
                    if kind in ("xa", "zb"):
                        o, To = SF.next()
                        P.add("dve", lambda e, o=o, ps=ps, bcol=bcol: e.tensor_scalar(out=o[:], in0=ps[:], scalar1=bcol, scalar2=None, op0=ALU.add),
                              reads=[Tp, T_par], writes=[To])
                        dst, Td = (xaT_d, T_xaT) if kind == "xa" else (zbT_d, T_zbT)
                        P.add("sp", lambda e, tok=tok, o=o, dst=dst, c=c: e.dma_start(out=dst[sl(c), tok], in_=o[:]), reads=[To], writes=[TT()], dma=True)
                    elif kind == "u":
                        P.add("act", lambda e, ps=ps, bcol=bcol, c=c, uT=uT: e.activation(out=uT[:, c, :], in_=ps[:], func=AF.Gelu, bias=bcol),
                              reads=[Tp, T_par], writes=[T_uT])
                    else:
                        o, To = SH.next()
                        func = AF.Gelu if kind == "ga" else AF.Sigmoid
                        P.add("act", lambda e, o=o, ps=ps, bcol=bcol, func=func: e.activation(out=o[:], in_=ps[:], func=func, bias=bcol),
                              reads=[Tp, T_par], writes=[To])
                        dst, Td = (gaT_d, T_gaT) if kind == "ga" else (gT_d, T_gT)
                        P.add("sp", lambda e, tok=tok, o=o, dst=dst, c=c: e.dma_start(out=dst[sl(c), tok], in_=o[:]), reads=[To], writes=[TT()], dma=True)
                ucT, T_uc = UC.next()
                for j in range(4):
                    ps, Tp = PSF.next()
                    for kc in range(8):
                        P.add("pe", lambda e, ps=ps, kc=kc, j=j, hT=hT: e.matmul(ps[:], lhsT=hT[:, kc, sl(j)], rhs=win[:, kc, OFF_V:OFF_G],
                                                                              start=(kc == 0), stop=(kc == 7)),
                              reads=[T_hT] + Twin(OFF_V, OFF_G), writes=[Tp])
                    vg, T_vg = VG.next()
                    vn, T_vn = VN.next()
                    bs, T_bs = bst.next()
                    P.add("dve", lambda e, vg=vg, ps=ps: e.tensor_tensor(out=vg[:], in0=ps[:], in1=bv_bc[:], op=ALU.add), reads=[Tp, T_par], writes=[T_vg])
                    P.add("act", lambda e, vg=vg: e.activation(out=vg[:], in_=vg[:], func=AF.Gelu), reads=[T_vg], writes=[T_vg])
                    P.add("dve", lambda e, vg=vg, bs=bs: e.bn_stats(out=bs[:, 0:6], in_=vg[:]), reads=[T_vg], writes=[T_bs])
                    P.add("dve", lambda e, bs=bs: e.bn_aggr(out=bs[:, 6:8], in_=bs[:, 0:6]), reads=[T_bs], writes=[T_bs])
                    P.add("act", lambda e, bs=bs: e.activation(out=bs[:, 0:1], in_=bs[:, 7:8], func=AF.Sqrt, scale=1.0, bias=EPS), reads=[T_bs], writes=[T_bs])
                    P.add("dve", lambda e, bs=bs: e.reciprocal(out=bs[:, 1:2], in_=bs[:, 0:1]), reads=[T_bs], writes=[T_bs])
                    P.add("dve", lambda e, vg=vg, bs=bs: e.tensor_scalar(out=vg[:], in0=vg[:], scalar1=bs[:, 6:7], scalar2=bs[:, 1:2],
                                                                        op0=ALU.subtract, op1=ALU.mult), reads=[T_vg, T_bs], writes=[T_vg])
                    P.add("dve", lambda e, vg=vg: e.tensor_tensor(out=vg[:], in0=vg[:], in1=lng[:], op=ALU.mult), reads=[T_vg, T_par], writes=[T_vg])
                    P.add("dve", lambda e, vg=vg, vn=vn: e.tensor_tensor(out=vn[:], in0=vg[:], in1=lnb[:], op=ALU.add), reads=[T_vg, T_par], writes=[T_vn])
                    ps2, Tp2 = PSF.next()
                    for g in range(4):
                        P.add("pe", lambda e, ps2=ps2, vn=vn, g=g: e.matmul(ps2[:, sl(g)], lhsT=vn[:, sl(g)], rhs=sgw[:, g, :], start=True, stop=True),
                              reads=[T_vn, T_par], writes=[Tp2])
                    stp, T_stp = STMP.next()
                    P.add("dve", lambda e, stp=stp, ps2=ps2: e.tensor_tensor(out=stp[:], in0=ps2[:], in1=sgb[:], op=ALU.add), reads=[Tp2, T_par], writes=[T_stp])
                    P.add("pool", lambda e, stp=stp, ucT=ucT, uT=uT, j=j: e.tensor_tensor(
                        out=ucT[:, :, sl(j)], in0=stp[:].rearrange("p (g q) -> p g q", g=4), in1=uT[:, :, sl(j)], op=ALU.mult),
                        reads=[T_stp, T_uT], writes=[T_uc])
                P.add("sp", lambda e, tok=tok, ucT=ucT: e.dma_start(out=ucT_d.rearrange("(c p) t -> p c t", p=128)[:, :, tok], in_=ucT[:]),
                      reads=[T_uc], writes=[TT()], dma=True)
        P.barrier()

    def phase_B(l):
        PADL, GAP, PADR = 2, 4, 2
        WID = PADL + TC + GAP + TL + PADR
        CO = PADL
        LO = PADL + TC + GAP
        with contextlib.ExitStack() as st:
            sb = mk_sb(st)
            wa = sb([128, 16, 128], BF16, "wa")
            wx = sb([128, 16, 128], BF16, "wx")
            cv = sb([128, 8, 5], F32, "cv")
            lr = sb([128, 2, 8, 3], F32, "lr")
            cl = sb([128, 2, 8], F32, "cl")
            T_par, T_cl = TT("parB"), TT("cl")
            P.add("pool", lambda e: e.dma_start(out=wa[:], in_=lru_wa[l].rearrange("d h i j -> i (d h) j")), writes=[T_par], dma=True)
            P.add("pool", lambda e: e.dma_start(out=wx[:], in_=lru_wx[l].rearrange("d h i j -> i (d h) j")), writes=[T_par], dma=True)
            P.add("sp", lambda e: e.dma_start(out=cv[:].rearrange("p a b -> p (a b)"), in_=convT[l]), writes=[T_par], dma=True)
            P.add("sp", lambda e: e.dma_start(out=lr[:].rearrange("p a b c -> p (a b c)"), in_=lruT[l]), writes=[T_par], dma=True)
            P.add("act", lambda e: e.activation(out=cl[:], in_=lr[:, :, :, 2], func=AF.Exp, scale=-1.0), reads=[T_par], writes=[T_cl])
            P.add("act", lambda e: e.activation(out=cl[:], in_=cl[:], func=AF.Ln, scale=1.0, bias=1.0), reads=[T_cl], writes=[T_cl])
            P.add("dve", lambda e: e.tensor_scalar(out=cl[:], in0=cl[:], scalar1=-8.0, scalar2=None, op0=ALU.mult), reads=[T_cl], writes=[T_cl])
            XA = Ring([(sb([128, WID], F32, "xa"), TT("xa%d" % i)) for i in range(2)])
            for xa, T_xa in XA.items:
                P.add("pool", lambda e, xa=xa: e.memset(xa[:], 0.0), writes=[T_xa])
            xc = sb([128, WID], F32, "xc")
            xcb = sb([128, WID], BF16, "xcb")
            H = sb([128, WID], F32, "H")
            T_xc, T_xcb, T_H = TT("xc"), TT("xcb"), TT("H")
            GA = Ring([(sb([128, TS], BF16, "ga"), TT("ga%d" % i)) for i in range(2)])
            UA = Ring([(sb([128, TS], BF16, "ua"), TT("ua%d" % i)) for i in range(2)])
            SEGW = max(SEG, TC)
            AB = Ring([tuple((sb([128, SEGW], F32, "seg"), TT("seg%d_%d" % (i, q))) for q in range(4)) for i in range(2)])
            for s in range(NS):
                base = s * TS
                for c in range(8):
                    xa, T_xa = XA.next()
                    ga, T_ga = GA.next()
                    ua, T_ua = UA.next()
                    P.add("sp", lambda e, base=base, xa=xa, c=c: e.dma_start(out=xa[:, CO:CO + TC], in_=xaT_d[sl(c), base:base + TC]),
                          reads=[T_xaT], writes=[T_xa], dma=True)
                    P.add("sp", lambda e, base=base, xa=xa, c=c: e.dma_start(out=xa[:, LO:LO + TL], in_=xaT_d[sl(c), base + TC:base + TS]),
                          reads=[T_xaT], writes=[T_xa], dma=True)
                    P.add("sp", lambda e, base=base, ga=ga, c=c: e.dma_start(out=ga[:], in_=gaT_d[sl(c), base:base + TS]), reads=[T_gaT], writes=[T_ga], dma=True)
                    n = WID - 3
                    P.add("pool", lambda e, xa=xa, c=c: e.tensor_scalar(out=xc[:, 2:2 + n], in0=xa[:, 0:n], scalar1=cv[:, c, 0:1], scalar2=cv[:, c, 4:5],
                                                                       op0=ALU.mult, op1=ALU.add), reads=[T_xa, T_par], writes=[T_xc])
                    for j in (1, 2, 3):
                        P.add("dve", lambda e, xa=xa, c=c, j=j: e.scalar_tensor_tensor(out=xc[:, 2:2 + n], in0=xa[:, j:j + n], scalar=cv[:, c, j:j + 1],
                                                                                      in1=xc[:, 2:2 + n], op0=ALU.mult, op1=ALU.add),
                              reads=[T_xa, T_par, T_xc], writes=[T_xc])
                    P.add("act", lambda e: e.activation(out=xcb[:, 2:2 + n], in_=xc[:, 2:2 + n], func=AF.Copy), reads=[T_xc], writes=[T_xcb])
                    segs = [(CO, TC)] + [(LO + i * SEG, SEG) for i in range(TL // SEG)]
                    for d in range(2):
                        order = segs if d == 0 else [segs[0]] + segs[1:][::-1]
                        prev = None
                        for (o0, n0) in order:
                            (A, T_A), (B, T_B), (Tq, T_T), (S, T_S) = AB.next()
                            for p0 in range(0, n0, 512):
                                pn = min(512, n0 - p0)
                                ps_r, Tpr = PSF.next()
                                ps_i, Tpi = PSF.next()
                                P.add("pe", lambda e, ps_r=ps_r, d=d, c=c, o0=o0, p0=p0, pn=pn: e.matmul(
                                    ps_r[:, 0:pn], lhsT=wa[:, d * 8 + c, :], rhs=xcb[:, o0 + p0:o0 + p0 + pn], start=True, stop=True),
                                    reads=[T_par, T_xcb], writes=[Tpr])
                                P.add("pe", lambda e, ps_i=ps_i, d=d, c=c, o0=o0, p0=p0, pn=pn: e.matmul(
                                    ps_i[:, 0:pn], lhsT=wx[:, d * 8 + c, :], rhs=xcb[:, o0 + p0:o0 + p0 + pn], start=True, stop=True),
                                    reads=[T_par, T_xcb], writes=[Tpi])
                                P.add("act", lambda e, A=A, ps_r=ps_r, d=d, c=c, p0=p0, pn=pn: e.activation(
                                    out=A[:, p0:p0 + pn], in_=ps_r[:, 0:pn], func=AF.Sigmoid, bias=lr[:, d, c, 0:1]), reads=[Tpr, T_par], writes=[T_A])
                                P.add("act", lambda e, B=B, ps_i=ps_i, d=d, c=c, p0=p0, pn=pn: e.activation(
                                    out=B[:, p0:p0 + pn], in_=ps_i[:, 0:pn], func=AF.Sigmoid, bias=lr[:, d, c, 1:2]), reads=[Tpi, T_par], writes=[T_B])
                            P.add("act", lambda e, A=A, d=d, c=c, n0=n0: e.activation(out=A[:, 0:n0], in_=A[:, 0:n0], func=AF.Exp, scale=cl[:, d, c:c + 1]),
                                  reads=[T_A, T_cl], writes=[T_A])
                            P.add("pool", lambda e, A=A, Tq=Tq, n0=n0: e.tensor_tensor(out=Tq[:, 0:n0], in0=A[:, 0:n0], in1=A[:, 0:n0], op=ALU.mult),
                                  reads=[T_A], writes=[T_T])
                            P.add("act", lambda e, Tq=Tq, n0=n0: e.activation(out=Tq[:, 0:n0], in_=Tq[:, 0:n0], func=AF.Sqrt, scale=-1.0, bias=1.0),
                                  reads=[T_T], writes=[T_T])
                            P.add("pool", lambda e, B=B, o0=o0, n0=n0: e.tensor_tensor(out=B[:, 0:n0], in0=B[:, 0:n0], in1=xc[:, o0:o0 + n0], op=ALU.mult),
                                  reads=[T_B, T_xc], writes=[T_B])
                            P.add("pool", lambda e, B=B, Tq=Tq, n0=n0: e.tensor_tensor(out=B[:, 0:n0], in0=B[:, 0:n0], in1=Tq[:, 0:n0], op=ALU.mult),
                                  reads=[T_B, T_T], writes=[T_B])
                            if d == 0:
                                init = 0.0 if prev is None else H[:, prev - 1:prev]
                                P.add("dve", lambda e, A=A, B=B, o0=o0, n0=n0, init=init: e.tensor_tensor_scan(
                                    out=H[:, o0:o0 + n0], data0=A[:, 0:n0], data1=B[:, 0:n0], initial=init, op0=ALU.mult, op1=ALU.add),
                                    reads=[T_A, T_B, T_H], writes=[T_H])
                                prev = o0 + n0
                            else:
                                init = 0.0 if prev is None else prev

                                def rv(t, a, b):
                                    return t[:, a:b][:, ::-1]
                                P.add("dve", lambda e, A=A, B=B, S=S, n0=n0, init=init: e.tensor_tensor_scan(
                                    out=rv(S, 0, n0), data0=rv(A, 0, n0), data1=rv(B, 0, n0), initial=init, op0=ALU.mult, op1=ALU.add),
                                    reads=[T_A, T_B] + ([] if prev is None else [prevT]), writes=[T_S])
                                P.add("pool", lambda e, S=S, o0=o0, n0=n0: e.tensor_tensor(out=H[:, o0:o0 + n0], in0=H[:, o0:o0 + n0], in1=S[:, 0:n0], op=ALU.add),
                                      reads=[T_S, T_H], writes=[T_H])
                                prev = S[:, 0:1]
                                prevT = T_S
                    P.add("dve", lambda e, ua=ua, ga=ga: e.tensor_tensor(out=ua[:, 0:TC], in0=H[:, CO:CO + TC], in1=ga[:, 0:TC], op=ALU.mult),
                          reads=[T_H, T_ga], writes=[T_ua])
                    P.add("dve", lambda e, ua=ua, ga=ga: e.tensor_tensor(out=ua[:, TC:TS], in0=H[:, LO:LO + TL], in1=ga[:, TC:TS], op=ALU.mult),
                          reads=[T_H, T_ga], writes=[T_ua])
                    P.add("sp", lambda e, base=base, ua=ua, c=c: e.dma_start(out=uaT_d[sl(c), base:base + TS], in_=ua[:]), reads=[T_ua], writes=[TT()], dma=True)
        P.barrier()

    def phase_B2(l):
        M = 8
        RW, CW = R + 2 * M, GRID_W + 2 * M
        with contextlib.ExitStack() as st:
            sb = mk_sb(st)
            pinv = sb([128, 4, 2 * 64 + TC], F32, "pinv")
            T_par = TT("parB2")
            P.add("sp", lambda e: e.dma_start(out=pinv[:].rearrange("p a b -> p (a b)"),
                                              in_=c_pinv.rearrange("a b -> (a b)").rearrange("(o n) -> o n", o=1).to_broadcast([128, 4 * (128 + TC)])),
                  writes=[T_par], dma=True)
            sets = []
            for i in range(2):
                X = sb([128, RW, CW], F32, "pX")
                P1 = sb([128, RW, CW], F32, "pP1")
                P2 = sb([128, RW, CW], F32, "pP2")
                XC = sb([128, TC + 2 * M], F32, "pXC")
                C1 = sb([128, TC + 2 * M], F32, "pC1")
                C2 = sb([128, TC + 2 * M], F32, "pC2")
                O = sb([128, TS], BF16, "pO")
                Ts = [TT("pool%d_%d" % (i, q)) for q in range(7)]
                eng = "dve" if i == 0 else "pool"
                for t, T in ((X, Ts[0]), (P1, Ts[1]), (P2, Ts[2]), (XC, Ts[3]), (C1, Ts[4]), (C2, Ts[5])):
                    P.add(eng, lambda e, t=t: e.memset(t[:], 0.0), writes=[T])
                sets.append((eng, X, P1, P2, XC, C1, C2, O, Ts))
            it = 0
            for s in range(NS):
                base = s * TS
                for g in range(4):
                    _, X, P1, P2, XC, C1, C2, O, Ts = sets[it % 2]
                    eng = "pool" if it % 3 == 2 else "dve"
                    it += 1
                    m = g + 1
                    P.add("sp", lambda e, base=base, XC=XC, g=g: e.dma_start(out=XC[:, M:M + TC], in_=zbT_d[sl(g), base:base + TC]),
                          reads=[T_zbT], writes=[Ts[3]], dma=True)
                    P.add("sp", lambda e, base=base, X=X, g=g: e.dma_start(out=X[:, M:M + R, M:M + GRID_W],
                                                                  in_=zbT_d[sl(g), base + TC:base + TS].rearrange("p (r c) -> p r c", c=GRID_W)),
                          reads=[T_zbT], writes=[Ts[0]], dma=True)
                    cur, Tcur = X, Ts[0]
                    bufs = [(P1, Ts[1]), (P2, Ts[2])]
                    lo, hi = -M, GRID_W + M - 1
                    bi = 0
                    for i in range(1, m + 1):
                        a, b = (0, 1) if i == 1 else (2 ** (i - 2), 2 ** (i - 2))
                        nlo, nhi = lo + b, hi - a
                        dst, Tdst = bufs[bi % 2]
                        bi += 1
                        w = nhi - nlo + 1
                        P.add(eng, lambda e, dst=dst, cur=cur, nlo=nlo, a=a, b=b, w=w: e.tensor_tensor(
                            out=dst[:, :, M + nlo:M + nlo + w], in0=cur[:, :, M + nlo + a:M + nlo + a + w], in1=cur[:, :, M + nlo - b:M + nlo - b + w], op=ALU.add),
                            reads=[Tcur], writes=[Tdst])
                        cur, Tcur, lo, hi = dst, Tdst, nlo, nhi
                    lo, hi = -M, R + M - 1
                    for i in range(1, m + 1):
                        a, b = (0, 1) if i == 1 else (2 ** (i - 2), 2 ** (i - 2))
                        nlo, nhi = lo + b, hi - a
                        dst, Tdst = bufs[bi % 2]
                        bi += 1
                        w = nhi - nlo + 1
                        P.add(eng, lambda e, dst=dst, cur=cur, nlo=nlo, a=a, b=b, w=w: e.tensor_tensor(
                            out=dst[:, M + nlo:M + nlo + w, M:M + GRID_W], in0=cur[:, M + nlo + a:M + nlo + a + w, M:M + GRID_W],
                            in1=cur[:, M + nlo - b:M + nlo - b + w, M:M + GRID_W], op=ALU.add), reads=[Tcur], writes=[Tdst])
                        cur, Tcur, lo, hi = dst, Tdst, nlo, nhi
                    dst, Tdst = bufs[bi % 2]
                    invc = pinv[:, g, 0:64]
                    invr = pinv[:, g, 64:64 + R]
                    P.add(eng, lambda e, dst=dst, cur=cur, invc=invc: e.tensor_tensor(
                        out=dst[:, M:M + R, M:M + GRID_W], in0=cur[:, M:M + R, M:M + GRID_W],
                        in1=invc.rearrange("p (o c) -> p o c", o=1).to_broadcast([128, R, GRID_W]), op=ALU.mult), reads=[Tcur, T_par], writes=[Tdst])
                    P.add(eng, lambda e, dst=dst, invr=invr: e.tensor_tensor(
                        out=dst[:, M:M + R, M:M + GRID_W], in0=dst[:, M:M + R, M:M + GRID_W],
                        in1=invr.rearrange("p (r o) -> p r o", o=1).to_broadcast([128, R, GRID_W]), op=ALU.mult), reads=[Tdst, T_par], writes=[Tdst])
                    P.add(eng, lambda e, dst=dst, X=X, O=O: e.tensor_tensor(
                        out=O[:, TC:TS].rearrange("p (r c) -> p r c", c=GRID_W), in0=dst[:, M:M + R, M:M + GRID_W],
                        in1=X[:, M:M + R, M:M + GRID_W], op=ALU.subtract), reads=[Tdst, Ts[0]], writes=[Ts[6]])
                    cur, Tcur = XC, Ts[3]
                    cb = [(C1, Ts[4]), (C2, Ts[5])]
                    lo, hi = -M, TC + M - 1
                    bi = 0
                    for i in range(1, m + 1):
                        a, b = (0, 1) if i == 1 else (2 ** (i - 2), 2 ** (i - 2))
                        nlo, nhi = lo + b, hi - a
                        dst, Tdst = cb[bi % 2]
                        bi += 1
                        w = nhi - nlo + 1
                        P.add(eng, lambda e, dst=dst, cur=cur, nlo=nlo, a=a, b=b, w=w: e.tensor_tensor(
                            out=dst[:, M + nlo:M + nlo + w], in0=cur[:, M + nlo + a:M + nlo + a + w], in1=cur[:, M + nlo - b:M + nlo - b + w], op=ALU.add),
                            reads=[Tcur], writes=[Tdst])
                        cur, Tcur, lo, hi = dst, Tdst, nlo, nhi
                    dst, Tdst = cb[bi % 2]
                    P.add(eng, lambda e, dst=dst, cur=cur, g=g: e.tensor_tensor(out=dst[:, M:M + TC], in0=cur[:, M:M + TC], in1=pinv[:, g, 128:128 + TC], op=ALU.mult),
                          reads=[Tcur, T_par], writes=[Tdst])
                    P.add(eng, lambda e, dst=dst, XC=XC, O=O: e.tensor_tensor(out=O[:, 0:TC], in0=dst[:, M:M + TC], in1=XC[:, M:M + TC], op=ALU.subtract),
                          reads=[Tdst, Ts[3]], writes=[Ts[6]])
                    P.add("sp", lambda e, base=base, O=O, g=g: e.dma_start(out=pmT_d[sl(g), base:base + TS], in_=O[:]), reads=[Ts[6]], writes=[TT()], dma=True)
        P.barrier()

    RS = {}

    def alloc_routing(stack):
        sbr = mk_sb(stack)
        RS["lg_all"] = sbr([128, NTILE, NE], F32, "lg_all")
        RS["m8_all"] = sbr([128, NTILE, 8], F32, "m8_all")
        RS["pos_all"] = sbr([128, NTILE, NE], F32, "pos_all")
        RS["w4_all"] = sbr([128, NTILE, 4], F32, "w4_all")
        RS["dest_i"] = sbr([128, NTILE * 4], I32, "dest_i")
        RS["cntbase"] = sbr([128, NE], F32, "cntbase")
        RS["idx_w"] = sbr([128, NBLK], I32, "idx_w")
        RS["idx_b"] = sbr([128, NBLK], I32, "idx_b")
        RS["idx_w8"] = sbr([128, NBLK, 8], I32, "idx_w8")
        RS["tokc"] = sbr([128, NTILE, 2], I32, "tokc")
    T_lg, T_m8, T_pos, T_w4, T_dest, T_cnt = [TT(n) for n in "lg m8 pos w4 dest cnt".split()]

    def load_bc(sb, src_row_ap, n, T, reads=(), name="bc"):
        t = sb([128, n], F32, name)
        P.add("sp", lambda e: e.dma_start(out=t[:], in_=src_row_ap.to_broadcast([128, n])), reads=list(reads), writes=[T], dma=True)
        return t

    def phase_C(l):
        lg_all, m8_all, pos_all, w4_all, cntbase = RS["lg_all"], RS["m8_all"], RS["pos_all"], RS["w4_all"], RS["cntbase"]
        with contextlib.ExitStack() as st:
            sb = mk_sb(st)
            T_w = TT("wC")
            oa = sb([128, 8, D], BF16, "oa")
            ob = sb([128, 4, D], BF16, "ob")
            oc_ = sb([128, 4, D], BF16, "oc")
            wo = sb([128, 8, D], BF16, "wo")
            pw = sb([128, 4, 128], BF16, "pw")
            rw = sb([128, 8, NE], BF16, "rw")
            for t, src in ((oa, out_a[l]), (ob, out_b[l]), (oc_, out_c[l]), (wo, w_o[l]), (rw, router_w[l])):
                P.add("pool", lambda e, t=t, src=src: e.dma_start(out=t[:], in_=src.rearrange("(kc p) n -> p kc n", p=128)), writes=[T_w], dma=True)
            P.add("pool", lambda e: e.dma_start(out=pw[:], in_=pool_w[l].rearrange("g i j -> i g j")), writes=[T_w], dma=True)
            pT_ = sb([128, 4, 2], F32, "poolT")
            P.add("sp", lambda e: e.dma_start(out=pT_[:].rearrange("p a b -> p (a b)"), in_=poolT[l]), writes=[T_w], dma=True)
            T_bc = TT("bcC")
            bo_bc = load_bc(sb, b_o[l:l + 1, :], D, T_bc, name="bo")
            n2g_bc = load_bc(sb, n2g[l:l + 1, :], D, T_bc, name="n2g")
            rb_bc = load_bc(sb, router_b[l:l + 1, :], NE, T_bc, name="rb")
            g1s, bog1s, gm2s, sh2s, T_rows = [], [], [], [], []
            for k in range(2):
                g1s.append(sb([128, D], F32, "g1"))
                gm2s.append(sb([128, D], F32, "gm2"))
                sh2s.append(sb([128, D], F32, "sh2"))
                T_rows.append(TT("rows%d" % k))

            def load_rows(k, r):
                T = T_rows[k]
                P.add("sp", lambda e: e.dma_start(out=g1s[k][:], in_=mod_d[r:r + 1, 2 * D:3 * D].to_broadcast([128, D])), reads=[T_mod], writes=[T], dma=True)
                P.add("sp", lambda e: e.dma_start(out=sh2s[k][:], in_=mod_d[r:r + 1, 3 * D:4 * D].to_broadcast([128, D])), reads=[T_mod], writes=[T], dma=True)
                P.add("sp", lambda e: e.dma_start(out=gm2s[k][:], in_=mod_d[r:r + 1, 4 * D:5 * D].to_broadcast([128, D])), reads=[T_mod], writes=[T], dma=True)
                P.add("dve", lambda e: e.scalar_tensor_tensor(out=gm2s[k][:], in0=gm2s[k][:], scalar=1.0, in1=n2g_bc[:], op0=ALU.add, op1=ALU.mult),
                      reads=[T, T_bc], writes=[T])
            load_rows(0, 2)
            cur_s = [-1]
            IN_UA = Ring([(sb([128, 8, 512], BF16, "uaT"), TT("uaT%d" % i)) for i in range(1)])
            IN_PM = Ring([(sb([128, 4, 512], BF16, "pmT"), TT("pmT%d" % i)) for i in range(1)])
            IN_UC = Ring([(sb([128, 4, 512], BF16, "ucTi"), TT("ucTi%d" % i)) for i in range(1)])
            IN_G = Ring([(sb([128, 24, 512], BF16, "gTi"), TT("gTi%d" % i)) for i in range(1)])
            YB = Ring([(sb([128, 4, 512], BF16, "ybin"), TT("ybin%d" % i)) for i in range(1)])
            MG = Ring([(sb([128, 8, 512], BF16, "mg"), TT("mg%d" % i)) for i in range(1)])
            TM = Ring([(sb([128, 512], F32, "tm"), TT("tm%d" % i)) for i in range(6)])
            XT = Ring([(sb([128, D], F32, "xtC"), TT("xtC%d" % i)) for i in range(3)])
            XF = Ring([(sb([128, D], F32, "xf"), TT("xf%d" % i)) for i in range(2)])
            H2 = Ring([(sb([128, D], BF16, "h2"), TT("h2%d" % i)) for i in range(2)])
            H2T = Ring([(sb([128, 8, 128], BF16, "h2T"), TT("h2T%d" % i)) for i in range(2)])
            junk = sb([128, D], BF16, "junkC")
            T_junk = TT("junkC")
            stat = Ring([(sb([128, 8], F32, "statC"), TT("statC%d" % i)) for i in range(4)])
            mk = Ring([(sb([128, NE], BF16, "mk"), TT("mk%d" % i)) for i in range(2)])
            P.add("dve", lambda e: e.memset(cntbase[:], 0.0), writes=[T_cnt])

            def loads_C(sti):
                tok = slice(sti * 512, (sti + 1) * 512)
                uaT, T_ua = IN_UA.next()
                pmT, T_pm = IN_PM.next()
                ucT, T_uc = IN_UC.next()
                gT, T_g = IN_G.next()
                P.add("sp", lambda e, tok=tok, uaT=uaT: e.dma_start(out=uaT[:], in_=uaT_d.rearrange("(c p) t -> p c t", p=128)[:, :, tok]), reads=[T_uaT], writes=[T_ua], dma=True)
                P.add("sp", lambda e, tok=tok, pmT=pmT: e.dma_start(out=pmT[:], in_=pmT_d.rearrange("(c p) t -> p c t", p=128)[:, :, tok]), reads=[T_pmT], writes=[T_pm], dma=True)
                P.add("sp", lambda e, tok=tok, ucT=ucT: e.dma_start(out=ucT[:], in_=ucT_d.rearrange("(c p) t -> p c t", p=128)[:, :, tok]), reads=[T_ucT], writes=[T_uc], dma=True)
                P.add("sp", lambda e, tok=tok, gT=gT: e.dma_start(out=gT[:], in_=gT_d.rearrange("(c p) t -> p c t", p=128)[:, :, tok]), reads=[T_gT], writes=[T_g], dma=True)
                return uaT, T_ua, pmT, T_pm, ucT, T_uc, gT, T_g
            nxt_in = loads_C(0)
            for sti in range(NST):
                tok = slice(sti * 512, (sti + 1) * 512)
                uaT, T_ua, pmT, T_pm, ucT, T_uc, gT, T_g = nxt_in
                ybin, T_yb = YB.next()
                for g in range(4):
                    ps, Tp = PSF.next()
                    P.add("pe", lambda e, ps=ps, g=g, pmT=pmT: e.matmul(ps[:], lhsT=pw[:, g, :], rhs=pmT[:, g, :], start=True, stop=True), reads=[T_w, T_pm], writes=[Tp])
                    P.add("dve", lambda e, ps=ps, g=g, ybin=ybin: e.tensor_scalar(out=ybin[:, g, :], in0=ps[:], scalar1=pT_[:, g, 0:1], scalar2=pT_[:, g, 1:2],
                                                                                op0=ALU.add, op1=ALU.mult), reads=[Tp, T_w], writes=[T_yb])
                mg, T_mg = MG.next()
                for oc in range(8):
                    pa, Tpa = PSF.next()
                    pb, Tpb = PSF.next()
                    pc, Tpc = PSF.next()
                    for kc in range(8):
                        P.add("pe", lambda e, pa=pa, kc=kc, oc=oc, uaT=uaT: e.matmul(pa[:], lhsT=oa[:, kc, sl(oc)], rhs=uaT[:, kc, :], start=(kc == 0), stop=(kc == 7)),
                              reads=[T_w, T_ua], writes=[Tpa])
                    for kc in range(4):
                        P.add("pe", lambda e, pb=pb, kc=kc, oc=oc, ybin=ybin: e.matmul(pb[:], lhsT=ob[:, kc, sl(oc)], rhs=ybin[:, kc, :], start=(kc == 0), stop=(kc == 3)),
                              reads=[T_w, T_yb], writes=[Tpb])
                    for kc in range(4):
                        P.add("pe", lambda e, pc=pc, kc=kc, oc=oc, ucT=ucT: e.matmul(pc[:], lhsT=oc_[:, kc, sl(oc)], rhs=ucT[:, kc, :], start=(kc == 0), stop=(kc == 3)),
                              reads=[T_w, T_uc], writes=[Tpc])
                    (t1, T1), (t2, T2), (t3, T3) = TM.next(), TM.next(), TM.next()
                    P.add("dve", lambda e, t1=t1, pa=pa, gT=gT, oc=oc: e.tensor_tensor(out=t1[:], in0=pa[:], in1=gT[:, oc, :], op=ALU.mult), reads=[Tpa, T_g], writes=[T1])
                    P.add("dve", lambda e, t2=t2, pb=pb, gT=gT, oc=oc: e.tensor_tensor(out=t2[:], in0=pb[:], in1=gT[:, 8 + oc, :], op=ALU.mult), reads=[Tpb, T_g], writes=[T2])
                    P.add("dve", lambda e, t3=t3, pc=pc, gT=gT, oc=oc: e.tensor_tensor(out=t3[:], in0=pc[:], in1=gT[:, 16 + oc, :], op=ALU.mult), reads=[Tpc, T_g], writes=[T3])
                    P.add("pool", lambda e, t1=t1, t2=t2: e.tensor_tensor(out=t1[:], in0=t1[:], in1=t2[:], op=ALU.add), reads=[T1, T2], writes=[T1])
                    P.add("pool", lambda e, t1=t1, t3=t3, mg=mg, oc=oc: e.tensor_tensor(out=mg[:, oc, :], in0=t1[:], in1=t3[:], op=ALU.add), reads=[T1, T3], writes=[T_mg])
                if sti + 1 < NST:
                    nxt_in = loads_C(sti + 1)
                for j in range(4):
                    ti = 4 * sti + j
                    r = cfg.tile_r(ti)
                    if r == 2:
                        k = 0
                    else:
                        k = 1
                        if cur_s[0] != r:
                            load_rows(1, r)
                            cur_s[0] = r
                    xt, T_xt = XT.next()
                    P.add("sp", lambda e, xt=xt, ti=ti: e.dma_start(out=xt[:], in_=xres[sl(ti), :]), reads=[T_xres[ti]], writes=[T_xt], dma=True)
                    for nb in range(2):
                        ps, Tp = PSF.next()
                        for kc in range(8):
                            P.add("pe", lambda e, ps=ps, kc=kc, j=j, nb=nb, mg=mg: e.matmul(ps[:], lhsT=mg[:, kc, sl(j)], rhs=wo[:, kc, sl(nb, 512)], start=(kc == 0), stop=(kc == 7)),
                                  reads=[T_w, T_mg], writes=[Tp])
                        t1, T1 = TM.next()
                        P.add("dve", lambda e, t1=t1, ps=ps, nb=nb: e.tensor_tensor(out=t1[:], in0=ps[:], in1=bo_bc[:, sl(nb, 512)], op=ALU.add), reads=[Tp, T_bc], writes=[T1])
                        P.add("pool", lambda e, t1=t1, k=k, nb=nb: e.tensor_tensor(out=t1[:], in0=t1[:], in1=g1s[k][:, sl(nb, 512)], op=ALU.mult), reads=[T1, T_rows[k]], writes=[T1])
                        P.add("dve", lambda e, t1=t1, xt=xt, nb=nb: e.tensor_tensor(out=xt[:, sl(nb, 512)], in0=xt[:, sl(nb, 512)], in1=t1[:], op=ALU.add), reads=[T1, T_xt], writes=[T_xt])
                    P.add("sp", lambda e, xt=xt, ti=ti: e.dma_start(out=xres[sl(ti), :], in_=xt[:]), reads=[T_xt], writes=[T_xres[ti]], dma=True)
                    sq, T_sq = stat.next()
                    xf, T_xf = XF.next()
                    h2, T_h2t = H2.next()
                    P.add("act", lambda e, xt=xt, sq=sq: e.activation(out=junk[:], in_=xt[:], func=AF.Square, accum_out=sq[:, 0:1]), reads=[T_xt], writes=[T_junk, T_sq])
                    P.add("act", lambda e, sq=sq: e.activation(out=sq[:, 1:2], in_=sq[:, 0:1], func=AF.Sqrt, scale=1.0 / D, bias=EPS), reads=[T_sq], writes=[T_sq])
                    P.add("dve", lambda e, sq=sq: e.reciprocal(out=sq[:, 2:3], in_=sq[:, 1:2]), reads=[T_sq], writes=[T_sq])
                    P.add("act", lambda e, xt=xt, xf=xf, sq=sq: e.activation(out=xf[:], in_=xt[:], func=AF.Copy, scale=sq[:, 2:3]), reads=[T_xt, T_sq], writes=[T_xf])
                    P.add("pool", lambda e, xf=xf, k=k: e.tensor_tensor(out=xf[:], in0=xf[:], in1=gm2s[k][:], op=ALU.mult), reads=[T_xf, T_rows[k]], writes=[T_xf])
                    P.add("pool", lambda e, xf=xf, h2=h2, k=k: e.tensor_tensor(out=h2[:], in0=xf[:], in1=sh2s[k][:], op=ALU.add), reads=[T_xf, T_rows[k]], writes=[T_h2t])
                    P.add("sp", lambda e, h2=h2, ti=ti: e.dma_start(out=h2_d[sl(ti), :], in_=h2[:]), reads=[T_h2t], writes=[TT()], dma=True)
                    h2T, T_h2T = H2T.next()
                    pT, T_pT = PSB.next()
                    for kc in range(8):
                        P.add("pe", lambda e, pT=pT, h2=h2, kc=kc: e.transpose(out=pT[:, sl(kc)], in_=h2[:, sl(kc)], identity=ident[:]), reads=[T_h2t, T_const], writes=[T_pT])
                    P.add("act", lambda e, pT=pT, h2T=h2T: e.activation(out=h2T[:].rearrange("p a b -> p (a b)"), in_=pT[:], func=AF.Copy), reads=[T_pT], writes=[T_h2T])
                    ps, Tp = PSF.next()
                    for kc in range(8):
                        P.add("pe", lambda e, ps=ps, kc=kc, h2T=h2T: e.matmul(ps[:, 0:NE], lhsT=h2T[:, kc, :], rhs=rw[:, kc, :], start=(kc == 0), stop=(kc == 7)),
                              reads=[T_w, T_h2T], writes=[Tp])
                    lgt = lg_all[:, ti, :]
                    m8 = m8_all[:, ti, :]
                    P.add("dve", lambda e, ps=ps, lgt=lgt: e.tensor_tensor(out=lgt, in0=ps[:, 0:NE], in1=rb_bc[:], op=ALU.add), reads=[Tp, T_bc], writes=[T_lg])
                    P.add("dve", lambda e, lgt=lgt, m8=m8: e.max(out=m8, in_=lgt), reads=[T_lg], writes=[T_m8])
                    P.add("dve", lambda e, sq=sq, m8=m8: e.tensor_scalar(out=sq[:, 3:4], in0=m8[:, 0:1], scalar1=-1.0, scalar2=None, op0=ALU.mult), reads=[T_m8], writes=[T_sq])
                    w4 = w4_all[:, ti, :]
                    P.add("act", lambda e, sq=sq, m8=m8, w4=w4: e.activation(out=w4, in_=m8[:, 0:4], func=AF.Exp, bias=sq[:, 3:4], accum_out=sq[:, 4:5]),
                          reads=[T_m8, T_sq], writes=[T_w4, T_sq])
                    P.add("dve", lambda e, sq=sq: e.reciprocal(out=sq[:, 5:6], in_=sq[:, 4:5]), reads=[T_sq], writes=[T_sq])
                    P.add("dve", lambda e, sq=sq, w4=w4: e.tensor_scalar(out=w4, in0=w4, scalar1=sq[:, 5:6], scalar2=None, op0=ALU.mult), reads=[T_w4, T_sq], writes=[T_w4])
                    mkt, T_mk = mk.next()
                    P.add("dve", lambda e, mkt=mkt, lgt=lgt, m8=m8: e.tensor_scalar(out=mkt[:], in0=lgt, scalar1=m8[:, 3:4], scalar2=None, op0=ALU.is_ge), reads=[T_lg, T_m8], writes=[T_mk])
                    pp, Tpp = PSF.next()
                    P.add("pe", lambda e, pp=pp, mkt=mkt: e.matmul(pp[:, 0:NE], lhsT=ltri[:], rhs=mkt[:], start=True, stop=True), reads=[T_const, T_mk], writes=[Tpp])
                    P.add("pe", lambda e, pp=pp, mkt=mkt: e.matmul(pp[:, NE:2 * NE], lhsT=ones[:], rhs=mkt[:], start=True, stop=True), reads=[T_const, T_mk], writes=[Tpp])
                    P.add("dve", lambda e, pp=pp, ti=ti: e.tensor_tensor(out=pos_all[:, ti, :], in0=pp[:, 0:NE], in1=cntbase[:], op=ALU.add), reads=[Tpp, T_cnt], writes=[T_pos])
                    P.add("dve", lambda e, pp=pp: e.tensor_tensor(out=cntbase[:], in0=pp[:, NE:2 * NE], in1=cntbase[:], op=ALU.add), reads=[Tpp, T_cnt], writes=[T_cnt])
        P.barrier()

    T_be = TT("be")

    def phase_D(l):
        lg_all, m8_all, pos_all, cntbase = RS["lg_all"], RS["m8_all"], RS["pos_all"], RS["cntbase"]
        dest_i, idx_w, idx_b, tokc = RS["dest_i"], RS["idx_w"], RS["idx_b"], RS["tokc"]
        with contextlib.ExitStack() as st:
            sb = mk_sb(st)
            T_d = TT("D")
            padded = sb([128, NE], F32, "padded")
            pend = sb([128, NE], F32, "pend")
            pstart = sb([128, NE], F32, "pstart")
            onesf = sb([128, NE], F32, "onesf")
            blk = sb([128, NBLK], F32, "blk")
            cmp2 = sb([128, NE, NST + 1], F32, "cmp2")
            cmp_ = sb([128, NBLK, NE], F32, "cmp")
            bef = sb([128, NBLK], F32, "bef")
            dp = sb([128, NE], F32, "dp")
            junk = sb([128, NE], F32, "junkD")
            dest_f = sb([128, NTILE * 4], F32, "dest_f")
            meta0 = sb([128, 2 * (NROWS // 128)], I32, "meta0")
            P.add("sp", lambda e: e.dma_start(out=blk[:], in_=c_blk.to_broadcast([128, NBLK])), writes=[T_d], dma=True)
            P.add("sp", lambda e: e.dma_start(out=tokc[:].rearrange("p a b -> p (a b)"), in_=c_tok), writes=[T_d], dma=True)
            P.add("sp", lambda e: e.dma_start(out=meta0[:], in_=c_meta0), writes=[T_d], dma=True)
            P.add("sp", lambda e: e.dma_start(out=meta_d.rearrange("(p j) c -> p (j c)", p=128), in_=meta0[:]), reads=[T_d], writes=[T_meta], dma=True)
            NJ = NST + 1
            P.add("dve", lambda e: e.tensor_tensor(out=cmp2[:], in0=cntbase[:].rearrange("p (n o) -> p n o", o=1).to_broadcast([128, NE, NJ]),
                                                   in1=blk[:, 0:NJ].rearrange("p (o n) -> p o n", o=1).to_broadcast([128, NE, NJ]), op=ALU.is_gt), reads=[T_cnt, T_d], writes=[T_d])
            P.add("dve", lambda e: e.tensor_reduce(out=padded[:], in_=cmp2[:], axis=AX.X, op=ALU.add), reads=[T_d], writes=[T_d])
            P.add("dve", lambda e: e.tensor_scalar(out=padded[:], in0=padded[:], scalar1=float(MOE_BLOCK), scalar2=None, op0=ALU.mult), reads=[T_d], writes=[T_d])
            P.add("dve", lambda e: e.memset(onesf[:], 1.0), writes=[T_d])
            P.add("dve", lambda e: e.tensor_tensor_scan(out=pend[:], data0=onesf[:], data1=padded[:], initial=0.0, op0=ALU.mult, op1=ALU.add), reads=[T_d], writes=[T_d])
            P.add("dve", lambda e: e.tensor_tensor(out=pstart[:], in0=pend[:], in1=padded[:], op=ALU.subtract), reads=[T_d], writes=[T_d])
            P.add("dve", lambda e: e.tensor_tensor(out=cmp_[:], in0=pend[:].rearrange("p (o n) -> p o n", o=1).to_broadcast([128, NBLK, NE]),
                                                   in1=blk[:].rearrange("p (n o) -> p n o", o=1).to_broadcast([128, NBLK, NE]), op=ALU.is_le), reads=[T_d], writes=[T_d])
            P.add("dve", lambda e: e.tensor_reduce(out=bef[:], in_=cmp_[:], axis=AX.X, op=ALU.add), reads=[T_d], writes=[T_d])
            P.add("dve", lambda e: e.tensor_scalar(out=bef[:], in0=bef[:], scalar1=float(NE - 1), scalar2=None, op0=ALU.min), reads=[T_d], writes=[T_d])
            pidx = sb([128, 1], F32, "pidx")
            P.add("sp", lambda e: e.dma_start(out=pidx[:], in_=c_pidx), writes=[T_d], dma=True)
            P.add("dve", lambda e: e.tensor_scalar(out=bef[:], in0=bef[:], scalar1=float(l * NE), scalar2=None, op0=ALU.add), reads=[T_d], writes=[T_d])
            P.add("dve", lambda e: e.tensor_copy(out=idx_b[:], in_=bef[:]), reads=[T_d], writes=[T_be])
            bw = sb([128, NBLK], F32, "bw")
            P.add("dve", lambda e: e.tensor_scalar(out=bw[:], in0=bef[:], scalar1=128.0, scalar2=pidx[:, 0:1], op0=ALU.mult, op1=ALU.add), reads=[T_d, T_be], writes=[T_d])
            P.add("dve", lambda e: e.tensor_copy(out=idx_w[:], in_=bw[:]), reads=[T_d], writes=[T_be])
            plc = sb([128, 8], F32, "plc")
            i8f = sb([128, NBLK, 8], F32, "i8f")
            P.add("sp", lambda e: e.dma_start(out=plc[:], in_=c_pl.to_broadcast([128, 8])), writes=[T_d], dma=True)
            P.add("dve", lambda e: e.tensor_scalar(out=bw[:], in0=bef[:], scalar1=1024.0, scalar2=pidx[:, 0:1], op0=ALU.mult, op1=ALU.add), reads=[T_d, T_be], writes=[T_d])
            P.add("dve", lambda e: e.tensor_tensor(out=i8f[:], in0=bw[:].rearrange("p (n o) -> p n o", o=1).to_broadcast([128, NBLK, 8]),
                                                   in1=plc[:].rearrange("p (o n) -> p o n", o=1).to_broadcast([128, NBLK, 8]), op=ALU.add), reads=[T_d], writes=[T_d])
            P.add("dve", lambda e: e.tensor_copy(out=RS["idx_w8"][:], in_=i8f[:]), reads=[T_d], writes=[T_be])
            for ti in range(NTILE):
                P.add("dve", lambda e, ti=ti: e.tensor_tensor(out=dp[:], in0=pos_all[:, ti, :], in1=pstart[:], op=ALU.add), reads=[T_pos, T_d], writes=[T_d])
                for k in range(4):
                    P.add("dve", lambda e, ti=ti, k=k: e.scalar_tensor_tensor(out=junk[:], in0=lg_all[:, ti, :], scalar=m8_all[:, ti, k:k + 1], in1=dp[:],
                                                                               op0=ALU.is_equal, op1=ALU.mult, accum_out=dest_f[:, ti * 4 + k:ti * 4 + k + 1]),
                          reads=[T_lg, T_m8, T_d], writes=[T_d])
            P.add("dve", lambda e: e.tensor_copy(out=dest_i[:], in_=dest_f[:]), reads=[T_d], writes=[T_dest])
            if cfg.debug:
                dbg = nc.dram_tensor("dbg_d%d" % l, [128, NTILE * 4 + 2 * NBLK], I32, kind="ExternalOutput").ap()
                dbgf = nc.dram_tensor("dbg_f%d" % l, [128, NE * 3], F32, kind="ExternalOutput").ap()
                T_dbg = TT("dbg")
                P.add("sp", lambda e: e.dma_start(out=dbg[:, 0:NTILE * 4], in_=dest_i[:]), reads=[T_dest], writes=[T_dbg], dma=True)
                P.add("sp", lambda e: e.dma_start(out=dbg[:, NTILE * 4:NTILE * 4 + NBLK], in_=idx_w[:]), reads=[T_be], writes=[T_dbg], dma=True)
                P.add("sp", lambda e: e.dma_start(out=dbg[:, NTILE * 4 + NBLK:], in_=idx_b[:]), reads=[T_be], writes=[T_dbg], dma=True)
                P.add("sp", lambda e: e.dma_start(out=dbgf[:, 0:NE], in_=cntbase[:]), reads=[T_cnt], writes=[T_dbg], dma=True)
                P.add("sp", lambda e: e.dma_start(out=dbgf[:, NE:2 * NE], in_=pend[:]), reads=[T_d], writes=[T_dbg], dma=True)
                P.add("sp", lambda e: e.dma_start(out=dbgf[:, 2 * NE:3 * NE], in_=pstart[:]), reads=[T_d], writes=[T_dbg], dma=True)
                if cfg.stop == "Dpre":
                    P.barrier()
                    return
            for ti in range(NTILE):
                for k in range(4):
                    P.add("pool", lambda e, ti=ti, k=k: e.indirect_dma_start(
                        out=meta_d, out_offset=bass.IndirectOffsetOnAxis(ap=dest_i[:, ti * 4 + k:ti * 4 + k + 1], axis=0),
                        in_=tokc[:, ti, :], in_offset=None), reads=[T_dest, T_d, T_meta], writes=[TT("sc")], dma=True)
        P.barrier()

    def phase_E(l):
        idx_w, idx_b, idx_w8 = RS["idx_w"], RS["idx_b"], RS["idx_w8"]
        with contextlib.ExitStack() as st:
            sb = mk_sb(st)
            WR = Ring([(sb([128, 8, D], BF16, "wexp"), [TT("wexp%d_%d" % (i, q)) for q in range(8)]) for i in range(6)])
            BG = Ring([(sb([128, 16], F32, "bgu"), TT("bgu%d" % i)) for i in range(2)])
            BD = Ring([(sb([128, D], F32, "bd"), TT("bd%d" % i)) for i in range(2)])
            MT = Ring([(sb([128, 4, 2], I32, "mt"), TT("mt%d" % i)) for i in range(2)])
            XG = Ring([(sb([128, 4, D], BF16, "xg"), TT("xg%d" % i)) for i in range(2)])
            XGT = Ring([(sb([128, 8, 512], BF16, "xgT"), TT("xgT%d" % i)) for i in range(1)])
            ACT_ = Ring([(sb([128, 8, 512], BF16, "actT"), TT("actT%d" % i)) for i in range(1)])
            TM = Ring([(sb([128, 512], F32, "tmE"), TT("tmE%d" % i)) for i in range(6)])
            YP = Ring([(sb([128, D], F32, "ypt"), TT("ypt%d" % i)) for i in range(2)])
            zt = sb([128, D], BF16, "zrow")
            T_z = TT("z")
            P.add("pool", lambda e: e.memset(zt[:], 0.0), writes=[T_z])
            P.add("sp", lambda e: e.dma_start(out=h2_d[NT:NT + 128, :], in_=zt[:]), reads=[T_z], writes=[T_h2], dma=True)

            def loads(j):
                d = {}
                mt, T_mt = MT.next()
                P.add("sp", lambda e: e.dma_start(out=mt[:], in_=meta_d[j * 512:(j + 1) * 512, :].rearrange("(jj p) c -> p jj c", p=128)), reads=[T_meta], writes=[T_mt], dma=True)
                xg, T_xg = XG.next()
                for jj in range(4):
                    P.add("pool", lambda e, jj=jj: e.indirect_dma_start(out=xg[:, jj, :], out_offset=None, in_=h2_d,
                                                                        in_offset=bass.IndirectOffsetOnAxis(ap=mt[:, jj, 0:1], axis=0)),
                          reads=[T_mt, T_h2], writes=[T_xg], dma=True)
                ws = []
                for wi, wsrc in enumerate((w_gate, w_up, w_down)):
                    wt, T_wt = WR.next()
                    src = wsrc.rearrange("l e k n -> (l e k) n")
                    for pl in range(8):
                        P.add("pool", lambda e, wt=wt, src=src, pl=pl: e.indirect_dma_start(
                            out=wt[:, pl, :], out_offset=None, in_=src, in_offset=bass.IndirectOffsetOnAxis(ap=idx_w8[:, j, pl:pl + 1], axis=0)),
                            reads=[T_be], writes=[T_wt[pl]], dma=True)
                    ws.append((wt, T_wt))
                bg, T_bg = BG.next()
                bd, T_bd = BD.next()
                P.add("pool", lambda e: e.indirect_dma_start(out=bg[:, 0:8], out_offset=None, in_=b_gateT,
                                                             in_offset=bass.IndirectOffsetOnAxis(ap=idx_w[:, j:j + 1], axis=0)), reads=[T_be], writes=[T_bg], dma=True)
                P.add("pool", lambda e: e.indirect_dma_start(out=bg[:, 8:16], out_offset=None, in_=b_upT,
                                                             in_offset=bass.IndirectOffsetOnAxis(ap=idx_w[:, j:j + 1], axis=0)), reads=[T_be], writes=[T_bg], dma=True)
                P.add("pool", lambda e: e.indirect_dma_start(out=bd[:], out_offset=None, in_=b_down,
                                                             in_offset=bass.IndirectOffsetOnAxis(ap=idx_b[:, j:j + 1], axis=0)), reads=[T_be], writes=[T_bd], dma=True)
                return dict(mt=(mt, T_mt), xg=(xg, T_xg), ws=ws, bg=(bg, T_bg), bd=(bd, T_bd))

            def compute(j, dd):
                mt, T_mt = dd["mt"]
                xg, T_xg = dd["xg"]
                (wg, T_wg), (wu, T_wu), (wd, T_wd) = dd["ws"]
                bg, T_bg = dd["bg"]
                bd, T_bd = dd["bd"]
                xgT, T_xgT = XGT.next()
                for kc in range(8):
                    pT, T_pT = PSB.next()
                    for jj in range(4):
                        P.add("pe", lambda e, pT=pT, jj=jj, kc=kc: e.transpose(out=pT[:, sl(jj)], in_=xg[:, jj, sl(kc)], identity=ident[:]), reads=[T_xg, T_const], writes=[T_pT])
                    P.add("act", lambda e, pT=pT, kc=kc: e.activation(out=xgT[:, kc, :], in_=pT[:, 0:512], func=AF.Copy), reads=[T_pT], writes=[T_xgT])
                actT, T_act = ACT_.next()
                for fc in range(8):
                    pg, Tpg = PSF.next()
                    pu, Tpu = PSF.next()
                    for kc in range(8):
                        P.add("pe", lambda e, pg=pg, kc=kc, fc=fc: e.matmul(pg[:], lhsT=wg[:, kc, sl(fc)], rhs=xgT[:, kc, :], start=(kc == 0), stop=(kc == 7)), reads=[T_wg[kc], T_xgT], writes=[Tpg])
                    for kc in range(8):
                        P.add("pe", lambda e, pu=pu, kc=kc, fc=fc: e.matmul(pu[:], lhsT=wu[:, kc, sl(fc)], rhs=xgT[:, kc, :], start=(kc == 0), stop=(kc == 7)), reads=[T_wu[kc], T_xgT], writes=[Tpu])
                    (gt, Tgt), (sg, Tsg), (up, Tup) = TM.next(), TM.next(), TM.next()
                    P.add("dve", lambda e, gt=gt, pg=pg, fc=fc: e.tensor_scalar(out=gt[:], in0=pg[:], scalar1=bg[:, fc:fc + 1], scalar2=7.0, op0=ALU.add, op1=ALU.min), reads=[Tpg, T_bg], writes=[Tgt])
                    P.add("act", lambda e, gt=gt, sg=sg: e.activation(out=sg[:], in_=gt[:], func=AF.Sigmoid, scale=1.702), reads=[Tgt], writes=[Tsg])
                    P.add("dve", lambda e, up=up, pu=pu, fc=fc: e.tensor_scalar(out=up[:], in0=pu[:], scalar1=bg[:, 8 + fc:9 + fc], scalar2=7.0, op0=ALU.add, op1=ALU.min), reads=[Tpu, T_bg], writes=[Tup])
                    P.add("dve", lambda e, up=up: e.tensor_scalar(out=up[:], in0=up[:], scalar1=-7.0, scalar2=1.0, op0=ALU.max, op1=ALU.add), reads=[Tup], writes=[Tup])
                    P.add("dve", lambda e, gt=gt, sg=sg: e.tensor_tensor(out=gt[:], in0=gt[:], in1=sg[:], op=ALU.mult), reads=[Tgt, Tsg], writes=[Tgt])
                    P.add("dve", lambda e, gt=gt, up=up, fc=fc: e.tensor_tensor(out=actT[:, fc, :], in0=gt[:], in1=up[:], op=ALU.mult), reads=[Tgt, Tup], writes=[T_act])
                if cfg.debug and j == 0:
                    dx = nc.dram_tensor("dbg_x%d" % l, [2, 128, 8, 512], BF16, kind="ExternalOutput").ap()
                    T_dx = TT("dbgx")
                    P.add("sp", lambda e: e.dma_start(out=dx[0], in_=xgT[:]), reads=[T_xgT], writes=[T_dx], dma=True)
                    P.add("sp", lambda e: e.dma_start(out=dx[1], in_=actT[:]), reads=[T_act], writes=[T_dx], dma=True)
                for jj in range(4):
                    ypt, T_ypt = YP.next()
                    for nb in range(2):
                        ps, Tp = PSF.next()
                        for fc in range(8):
                            P.add("pe", lambda e, ps=ps, fc=fc, jj=jj, nb=nb: e.matmul(ps[:], lhsT=actT[:, fc, sl(jj)], rhs=wd[:, fc, sl(nb, 512)], start=(fc == 0), stop=(fc == 7)),
                                  reads=[T_wd[fc], T_act], writes=[Tp])
                        P.add("dve", lambda e, ypt=ypt, ps=ps, nb=nb: e.tensor_tensor(out=ypt[:, sl(nb, 512)], in0=ps[:], in1=bd[:, sl(nb, 512)], op=ALU.add), reads=[Tp, T_bd], writes=[T_ypt])
                    P.add("sp", lambda e, ypt=ypt, jj=jj: e.dma_start(out=yp_d[j * 512 + jj * 128:j * 512 + (jj + 1) * 128, :], in_=ypt[:]), reads=[T_ypt], writes=[TT()], dma=True)

            pend = loads(0)
            if cfg.debug:
                dw = nc.dram_tensor("dbg_w%d" % l, [3, 128, 8, D], BF16, kind="ExternalOutput").ap()
                db = nc.dram_tensor("dbg_b%d" % l, [128, 16 + D], F32, kind="ExternalOutput").ap()
                T_dbg = TT("dbgE")
                for wi in range(3):
                    P.add("sp", lambda e, wi=wi, pd=pend: e.dma_start(out=dw[wi], in_=pd["ws"][wi][0][:]), reads=pend["ws"][wi][1], writes=[T_dbg], dma=True)
                P.add("sp", lambda e, pd=pend: e.dma_start(out=db[:, 0:16], in_=pd["bg"][0][:]), reads=[pend["bg"][1]], writes=[T_dbg], dma=True)
                P.add("sp", lambda e, pd=pend: e.dma_start(out=db[:, 16:], in_=pd["bd"][0][:]), reads=[pend["bd"][1]], writes=[T_dbg], dma=True)
            for j in range(NBLK):
                nxt = loads(j + 1) if j + 1 < NBLK else None
                compute(j, pend)
                pend = nxt
        P.barrier()

    def phase_F(l, last):
        dest_i, w4_all = RS["dest_i"], RS["w4_all"]
        with contextlib.ExitStack() as st:
            sb = mk_sb(st)
            T_bc = TT("bcF")
            g2s = [sb([128, D], F32, "g2") for _ in range(2)]
            T_rows = [TT("rowsF%d" % k) for k in range(2)]
            fg = load_bc(sb, final_g[0:1, :], D, T_bc, name="fg") if last else None

            def load_rows(k, r):
                P.add("sp", lambda e: e.dma_start(out=g2s[k][:], in_=mod_d[r:r + 1, 5 * D:6 * D].to_broadcast([128, D])), reads=[T_mod], writes=[T_rows[k]], dma=True)
            load_rows(0, 2)
            cur_s = [-1]
            YK = Ring([tuple((sb([128, D], F32, "yk"), TT("yk%d_%d" % (i, q))) for q in range(4)) for i in range(4)])
            XT = Ring([(sb([128, D], F32, "xtF"), TT("xtF%d" % i)) for i in range(4)])
            junk = sb([128, D], BF16, "junkF")
            T_junk = TT("junkF")
            stat = Ring([(sb([128, 4], F32, "statF"), TT("statF%d" % i)) for i in range(3)])
            tlist = [ti for ti in range(NTILE) if not (last and cfg.tile_r(ti) == 2)]

            def loads(ti):
                yk = YK.next()
                for q in range(4):
                    P.add("pool", lambda e, q=q, yk=yk, ti=ti: e.indirect_dma_start(out=yk[q][0][:], out_offset=None, in_=yp_d,
                                                                                  in_offset=bass.IndirectOffsetOnAxis(ap=dest_i[:, ti * 4 + q:ti * 4 + q + 1], axis=0)),
                          reads=[T_dest, T_yp], writes=[yk[q][1]], dma=True)
                xt, T_xt = XT.next()
                P.add("sp", lambda e, xt=xt, ti=ti: e.dma_start(out=xt[:], in_=xres[sl(ti), :]), reads=[T_xres[ti]], writes=[T_xt], dma=True)
                return yk, xt, T_xt
            AHEAD = 2
            pend = [loads(ti) for ti in tlist[:AHEAD]]
            for n, ti in enumerate(tlist):
                if n + AHEAD < len(tlist):
                    pend.append(loads(tlist[n + AHEAD]))
                yk, xt, T_xt = pend.pop(0)
                r = cfg.tile_r(ti)
                if r == 2:
                    k = 0
                else:
                    k = 1
                    if cur_s[0] != r:
                        load_rows(1, r)
                        cur_s[0] = r
                P.add("act", lambda e, yk=yk, ti=ti: e.activation(out=yk[0][0][:], in_=yk[0][0][:], func=AF.Copy, scale=w4_all[:, ti, 0:1]),
                      reads=[yk[0][1], T_w4], writes=[yk[0][1]])
                for q in (1, 2, 3):
                    P.add("dve", lambda e, yk=yk, ti=ti, q=q: e.scalar_tensor_tensor(out=yk[0][0][:], in0=yk[q][0][:], scalar=w4_all[:, ti, q:q + 1], in1=yk[0][0][:],
                                                                                    op0=ALU.mult, op1=ALU.add), reads=[yk[0][1], yk[q][1], T_w4], writes=[yk[0][1]])
                P.add("dve", lambda e, yk=yk, k=k: e.tensor_tensor(out=yk[0][0][:], in0=yk[0][0][:], in1=g2s[k][:], op=ALU.mult), reads=[yk[0][1], T_rows[k]], writes=[yk[0][1]])
                P.add("dve", lambda e, yk=yk, xt=xt: e.tensor_tensor(out=xt[:], in0=xt[:], in1=yk[0][0][:], op=ALU.add), reads=[yk[0][1], T_xt], writes=[T_xt])
                if not last:
                    P.add("sp", lambda e, xt=xt, ti=ti: e.dma_start(out=xres[sl(ti), :], in_=xt[:]), reads=[T_xt], writes=[T_xres[ti]], dma=True)
                else:
                    sq, T_sq = stat.next()
                    P.add("act", lambda e, xt=xt, sq=sq: e.activation(out=junk[:], in_=xt[:], func=AF.Square, accum_out=sq[:, 0:1]), reads=[T_xt], writes=[T_junk, T_sq])
                    P.add("act", lambda e, sq=sq: e.activation(out=sq[:, 1:2], in_=sq[:, 0:1], func=AF.Sqrt, scale=1.0 / D, bias=EPS), reads=[T_sq], writes=[T_sq])
                    P.add("dve", lambda e, sq=sq: e.reciprocal(out=sq[:, 2:3], in_=sq[:, 1:2]), reads=[T_sq], writes=[T_sq])
                    P.add("act", lambda e, xt=xt, sq=sq: e.activation(out=xt[:], in_=xt[:], func=AF.Copy, scale=sq[:, 2:3]), reads=[T_xt, T_sq], writes=[T_xt])
                    P.add("pool", lambda e, xt=xt: e.tensor_tensor(out=xt[:], in0=xt[:], in1=fg[:], op=ALU.mult), reads=[T_xt, T_bc], writes=[T_xt])
                    s = (ti * 128) // TS
                    orow = s * TL + (ti * 128 - s * TS - TC)
                    P.add("sp", lambda e, xt=xt, orow=orow: e.dma_start(out=out[orow:orow + 128, :], in_=xt[:]), reads=[T_xt], writes=[TT()], dma=True)
        P.barrier()

    P.barrier()
    stop = cfg.stop
    for l in range(L):
        P.phase = "mod"
        phase_mod(l)
        P.phase = "A"
        phase_A(l)
        if stop == "A":
            break
        P.phase = "B"
        phase_B(l)
        if stop == "B":
            break
        P.phase = "B2"
        phase_B2(l)
        if stop == "B2":
            break
        with contextlib.ExitStack() as rst:
            alloc_routing(rst)
            P.phase = "C"
            phase_C(l)
            if stop == "C":
                break
            P.phase = "D"
            phase_D(l)
            if stop in ("D", "Dpre"):
                break
            P.phase = "E"
            phase_E(l)
            if stop == "E":
                break
            P.phase = "F"
            phase_F(l, l == L - 1)
    P.add("sp", lambda e: e.nop(), reads=[T_meta, T_h2, T_mod] + T_xres)
    P.emit()
    top.close()
    return nc, P


def host_consts(cfg):
    bf = ml_dtypes.bfloat16
    ident = np.eye(128, dtype=np.float32).astype(bf)
    ltri = np.triu(np.ones((128, 128), np.float32), 1).astype(bf)
    ones = np.ones((128, 128), np.float32).astype(bf)

    def inv_counts(T, k):
        pos = np.arange(T)
        lo = np.clip(pos - k // 2, 0, T)
        hi = np.clip(pos + (k - k // 2), 0, T)
        return (1.0 / (hi - lo)).astype(np.float32)
    pinv = np.zeros((4, 128 + cfg.TC), np.float32)
    for g, k in enumerate(POOL_K):
        pinv[g, 0:64] = inv_counts(GRID_W, k)
        pinv[g, 64:128] = 0
        ir = inv_counts(cfg.R, k)
        pinv[g, 64:64 + min(64, cfg.R)] = ir[:64]
        pinv[g, 128:] = inv_counts(cfg.TC, k)
    blk = (np.arange(cfg.NBLK, dtype=np.float32) * MOE_BLOCK).reshape(1, -1)
    tok = np.zeros((128, cfg.NTILE, 2), np.int32)
    tok[:, :, 0] = np.arange(cfg.NTILE, dtype=np.int32)[None, :] * 128 + np.arange(128, dtype=np.int32)[:, None]
    tok = tok.reshape(128, -1)
    meta0 = np.zeros((128, cfg.NROWS // 128, 2), np.int32)
    meta0[:, :, 0] = cfg.NT
    return dict(c_pidx=np.arange(128, dtype=np.float32).reshape(128, 1), c_pl=(128.0 * np.arange(8, dtype=np.float32)).reshape(1, 8), c_ident=ident, c_ltri=ltri, c_ones=ones, c_pinv=pinv, c_blk=blk, c_tok=tok,
                c_meta0=meta0.reshape(128, -1))


def host_params(inp, L):
    f = np.float32
    g = lambda k: np.asarray(inp[k], dtype=f)
    d = {}
    d["ada_w"] = g("ada_w")
    d["ada_b"] = g("ada_b")
    d["n1gT"] = np.ascontiguousarray(g("norm1_g").reshape(L, 8, 128).transpose(0, 2, 1))
    d["n2g"] = g("norm2_g")
    d["final_g"] = g("final_g").reshape(1, D)
    d["w_in"] = g("w_in")
    d["b_inT"] = np.ascontiguousarray(g("b_in").reshape(L, 52, 128).transpose(0, 2, 1))
    d["b_in"] = g("b_in")
    cw = g("conv_w").reshape(L, 4, 8, 128)
    cb = g("conv_b").reshape(L, 1, 8, 128)
    d["convT"] = np.ascontiguousarray(np.concatenate([cw, cb], axis=1).transpose(0, 3, 2, 1)).reshape(L, 128, 40)
    lr = np.stack([g("lru_ba"), g("lru_bx"), g("lru_lambda")], axis=-1).reshape(L, 2, 8, 128, 3)
    d["lruT"] = np.ascontiguousarray(lr.transpose(0, 3, 1, 2, 4)).reshape(L, 128, 48)
    d["lru_wa"] = g("lru_wa")
    d["lru_wx"] = g("lru_wx")
    for k in ("out_a", "out_b", "out_c", "w_o", "b_o", "pool_w", "router_w", "router_b", "w_gate", "w_up", "w_down", "b_down"):
        d[k] = g(k)
    pt = np.stack([g("pool_b"), g("pool_scale")], axis=-1).reshape(L, 4, 128, 2)
    d["poolT"] = np.ascontiguousarray(pt.transpose(0, 2, 1, 3)).reshape(L, 128, 8)
    d["sg_ln"] = np.stack([g("sg_ln_g"), g("sg_ln_b")], axis=1)
    d["sg_wT"] = np.ascontiguousarray(g("sg_w").transpose(0, 1, 3, 2))
    d["sg_b"] = g("sg_b").reshape(L, 512)
    d["b_down"] = g("b_down").reshape(L * NE, D)
    d["b_gateT"] = np.ascontiguousarray(g("b_gate").reshape(L, NE, 8, 128).transpose(0, 1, 3, 2)).reshape(L * NE * 128, 8)
    d["b_upT"] = np.ascontiguousarray(g("b_up").reshape(L, NE, 8, 128).transpose(0, 1, 3, 2)).reshape(L * NE * 128, 8)
    return d


def core_inputs(inp, cfg, core):
    f = np.float32
    x = np.asarray(inp["x"], dtype=f)
    ctx = np.asarray(inp["ctx"], dtype=f)
    c = np.asarray(inp["c"], dtype=f)
    cc = np.asarray(inp["c_ctx"], dtype=f)
    rows = []
    for s in range(cfg.NS):
        b = core * cfg.NS + s
        rows.append(ctx[b])
        rows.append(x[b])
    xin = np.ascontiguousarray(np.concatenate(rows, axis=0))
    cv = np.stack([c[core * cfg.NS + s] for s in range(cfg.NS)] + [cc], axis=0)
    cT = np.ascontiguousarray(cv.reshape(3, 8, 128).transpose(2, 1, 0)).reshape(128, 24)
    return dict(xin=xin, cT=cT)


_CACHE = {}


def kernel(**inputs):
    cfg = Cfg()
    n_cores = 8
    if "nc" not in _CACHE:
        _CACHE["nc"] = build(cfg)[0]
    nc = _CACHE["nc"]
    shared = host_params(inputs, cfg.L)
    shared.update(host_consts(cfg))
    in_maps = []
    for core in range(n_cores):
        m = dict(shared)
        m.update(core_inputs(inputs, cfg, core))
        in_maps.append(m)
    res = run_bass_kernel_spmd(nc, in_maps, core_ids=list(range(n_cores)))
    outs = [np.asarray(r["out"]).reshape(cfg.NS, cfg.TL, D) for r in res.results]
    return np.concatenate(outs, axis=0).astype(np.float32)
```
